# Optimizing a Trainium2 kernel written in Bass

```python
import math
import jax, jax.numpy as jnp
from jax import lax
import numpy as np

D_MODEL = 1024
BATCH = 2
SEQ = 8192
DEPTH = 1

D_MIX = D_MODEL
N_DIFF_HEADS = 4
DIFF_HEAD_DIM = 64
DIFF_V_DIM = 2 * DIFF_HEAD_DIM
ATTN_WIDTH = N_DIFF_HEADS * DIFF_V_DIM
QK_WIDTH = N_DIFF_HEADS * 2 * DIFF_HEAD_DIM
POOL_WINDOWS = (2, 4, 8, 16)
POOL_GROUPS = len(POOL_WINDOWS)
POOL_WIDTH = D_MIX - ATTN_WIDTH
POOL_GROUP_DIM = POOL_WIDTH // POOL_GROUPS
IN_COLS = 2 * QK_WIDTH + ATTN_WIDTH + POOL_WIDTH
Q_BLOCK = 128
N_GROUPS = 4
E_PER_GROUP = 8
N_EXPERTS = N_GROUPS * E_PER_GROUP
TOP_K = 2
D_EXPERT = D_MODEL // 2
ROW_BLOCK = 128
EPS = 1e-6

kernel_name = "hybrid_diffattn_pool_hmoe_encoder"


def rmsnorm(x, g):
    xf = x.astype(jnp.float32)
    y = xf * lax.rsqrt(jnp.mean(xf * xf, axis=-1, keepdims=True) + EPS)
    return (y * g.astype(jnp.float32)).astype(x.dtype)


def diff_attention(q, k, v, lam, lam_init, subln_g):
    B, S = q.shape[:2]
    H, Dh = N_DIFF_HEADS, DIFF_HEAD_DIM
    nqb = S // Q_BLOCK
    scale = 1.0 / math.sqrt(Dh)
    slopes = 2.0 ** (-8.0 * jnp.arange(1, H + 1, dtype=jnp.float32) / H)
    kpos = jnp.arange(S, dtype=jnp.int32)
    q_blocks = q.reshape(B, nqb, Q_BLOCK, H, 2, Dh).transpose(1, 0, 2, 3, 4, 5)

    def block(args):
        q_blk, i = args
        s = jnp.einsum('bqhcd,bkhcd->bhcqk', q_blk, k,
                       preferred_element_type=jnp.float32) * scale
        qpos = i * Q_BLOCK + jnp.arange(Q_BLOCK, dtype=jnp.int32)
        dist = jnp.abs(qpos[:, None] - kpos[None, :]).astype(jnp.float32)
        s = s - slopes[None, :, None, None, None] * dist
        p = jax.nn.softmax(s, axis=-1)
        a = p[:, :, 0] - lam * p[:, :, 1]
        return jnp.einsum('bhqk,bkhe->bqhe', a.astype(v.dtype), v)

    o = lax.map(block, (q_blocks, jnp.arange(nqb, dtype=jnp.int32)))
    o = o.transpose(1, 0, 2, 3, 4).reshape(B, S, H, DIFF_V_DIM)
    o = rmsnorm(o, subln_g) * (1.0 - lam_init)
    return o.reshape(B, S, ATTN_WIDTH)


def multiscale_pool(u, w_pool, pool_scale):
    B, S, _ = u.shape
    uf = u.astype(jnp.float32).reshape(B, S, POOL_GROUPS, POOL_GROUP_DIM)
    csum = jnp.concatenate([jnp.zeros((B, 1, POOL_GROUPS, POOL_GROUP_DIM), jnp.float32),
                            jnp.cumsum(uf, axis=1)], axis=1)
    t = jnp.arange(S, dtype=jnp.int32)
    means = []
    for g, w in enumerate(POOL_WINDOWS):
        lo = jnp.clip(t - w // 2, 0, S)
        hi = jnp.clip(t + w // 2, 0, S)
        cnt = (hi - lo).astype(jnp.float32)
        sg = csum[:, hi, g] - csum[:, lo, g]
        means.append(sg / cnt[None, :, None])
    pooled = jnp.stack(means, axis=2)
    d = (pooled - uf).astype(u.dtype)
    y = jnp.einsum('bsgc,gcd->bsgd', d, w_pool).reshape(B, S, POOL_WIDTH)
    return y * pool_scale


def hierarchical_moe(xn, w_gr, b_gr, w_er, b_er, w_gate, w_up, w_down):
    B, S, D = xn.shape
    T = B * S
    xt = xn.reshape(T, D)
    g_logits = (xt @ w_gr).astype(jnp.float32) + b_gr.astype(jnp.float32)
    p_group = jax.nn.softmax(g_logits, axis=-1)
    gsel = jnp.argmax(g_logits, axis=-1).astype(jnp.int32)
    pg = jnp.take_along_axis(p_group, gsel[:, None], axis=-1)
    e_logits = (xt @ w_er.reshape(D, N_EXPERTS)).astype(jnp.float32).reshape(T, N_GROUPS, E_PER_GROUP)
    e_logits = e_logits + b_er.astype(jnp.float32)
    e_logits = jnp.take_along_axis(e_logits, gsel[:, None, None], axis=1)[:, 0]
    p_e = jax.nn.softmax(e_logits, axis=-1)
    top_p, top_i = lax.top_k(p_e, TOP_K)
    top_p = top_p / jnp.sum(top_p, axis=-1, keepdims=True)
    gates = pg * top_p
    expert_ids = gsel[:, None] * E_PER_GROUP + top_i.astype(jnp.int32)

    A = T * TOP_K
    nb = (A + ROW_BLOCK - 1) // ROW_BLOCK + N_EXPERTS
    P = nb * ROW_BLOCK
    flat_e = expert_ids.reshape(-1)
    flat_tok = jnp.repeat(jnp.arange(T, dtype=jnp.int32), TOP_K)
    flat_g = gates.reshape(-1)
    order = jnp.argsort(flat_e)
    se = flat_e[order]
    counts = jnp.bincount(flat_e, length=N_EXPERTS)
    starts = jnp.cumsum(counts) - counts
    padded = (counts + ROW_BLOCK - 1) // ROW_BLOCK * ROW_BLOCK
    pad_ends = jnp.cumsum(padded)
    pad_starts = pad_ends - padded
    dest = pad_starts[se] + (jnp.arange(A, dtype=jnp.int32) - starts[se])
    buf_tok = jnp.full((P,), T, jnp.int32).at[dest].set(flat_tok[order])
    buf_gate = jnp.zeros((P,), jnp.float32).at[dest].set(flat_g[order])
    block_e = jnp.minimum(jnp.searchsorted(pad_ends, jnp.arange(nb, dtype=jnp.int32) * ROW_BLOCK,
                                           side='right'), N_EXPERTS - 1).astype(jnp.int32)
    xpad = jnp.concatenate([xt, jnp.zeros((1, D), xt.dtype)], axis=0)
    xs = xpad[buf_tok].reshape(nb, ROW_BLOCK, D)

    def expert_block(args):
        xb, e = args
        hdn = jax.nn.silu(xb @ w_gate[e]) * (xb @ w_up[e])
        return hdn @ w_down[e]

    ys = lax.map(expert_block, (xs, block_e)).reshape(P, D)
    ys = ys * buf_gate[:, None].astype(ys.dtype)
    out = jnp.zeros((T + 1, D), ys.dtype).at[buf_tok].add(ys)[:T]
    return out.reshape(B, S, D)


def setup_inputs(seed: int = 0) -> dict:
    key = jax.random.key(seed)
    ks = jax.random.split(key, 20)
    f32 = jnp.float32
    L, D = DEPTH, D_MODEL
    nrm = lambda k, shape, s: jax.random.normal(k, shape, f32) * s
    return {
        "x": jax.random.normal(ks[0], (BATCH, SEQ, D), f32),
        "norm1_g": 1.0 + nrm(ks[1], (L, D), 0.02),
        "w_in": nrm(ks[2], (L, D, IN_COLS), D ** -0.5),
        "lambda_q1": nrm(ks[3], (L, DIFF_HEAD_DIM), 0.1),
        "lambda_k1": nrm(ks[4], (L, DIFF_HEAD_DIM), 0.1),
        "lambda_q2": nrm(ks[5], (L, DIFF_HEAD_DIM), 0.1),
        "lambda_k2": nrm(ks[6], (L, DIFF_HEAD_DIM), 0.1),
        "subln_g": 1.0 + nrm(ks[7], (L, DIFF_V_DIM), 0.02),
        "w_pool": nrm(ks[8], (L, POOL_GROUPS, POOL_GROUP_DIM, POOL_GROUP_DIM), POOL_GROUP_DIM ** -0.5),
        "pool_scale": 1.0 + nrm(ks[9], (L, POOL_WIDTH), 0.02),
        "w_out": nrm(ks[10], (L, D_MIX, D), D_MIX ** -0.5),
        "norm2_g": 1.0 + nrm(ks[11], (L, D), 0.02),
        "w_group_router": nrm(ks[12], (L, D, N_GROUPS), D ** -0.5),
        "b_group_router": nrm(ks[13], (L, N_GROUPS), 0.01),
        "w_expert_router": nrm(ks[14], (L, D, N_GROUPS, E_PER_GROUP), D ** -0.5),
        "b_expert_router": nrm(ks[15], (L, N_GROUPS, E_PER_GROUP), 0.01),
        "w_gate": nrm(ks[16], (L, N_EXPERTS, D, D_EXPERT), D ** -0.5),
        "w_up": nrm(ks[17], (L, N_EXPERTS, D, D_EXPERT), D ** -0.5),
        "w_down": nrm(ks[18], (L, N_EXPERTS, D_EXPERT, D), D_EXPERT ** -0.5),
        "final_g": 1.0 + nrm(ks[19], (D,), 0.02),
    }


def reference(x, norm1_g, w_in, lambda_q1, lambda_k1, lambda_q2, lambda_k2, subln_g,
              w_pool, pool_scale, w_out, norm2_g, w_group_router, b_group_router,
              w_expert_router, b_expert_router, w_gate, w_up, w_down, final_g):
    B, S, _ = x.shape
    H, Dh = N_DIFF_HEADS, DIFF_HEAD_DIM
    h = x
    for l in range(DEPTH):
        hn = rmsnorm(h, norm1_g[l])
        proj = hn @ w_in[l]
        q = proj[..., :QK_WIDTH].reshape(B, S, H, 2, Dh)
        k = proj[..., QK_WIDTH:2 * QK_WIDTH].reshape(B, S, H, 2, Dh)
        v = proj[..., 2 * QK_WIDTH:2 * QK_WIDTH + ATTN_WIDTH].reshape(B, S, H, DIFF_V_DIM)
        u = proj[..., 2 * QK_WIDTH + ATTN_WIDTH:]
        lam_init = 0.8 - 0.6 * math.exp(-0.3 * l)
        lam = (jnp.exp(jnp.sum(lambda_q1[l].astype(jnp.float32) * lambda_k1[l].astype(jnp.float32)))
               - jnp.exp(jnp.sum(lambda_q2[l].astype(jnp.float32) * lambda_k2[l].astype(jnp.float32)))
               + lam_init)
        a = diff_attention(q, k, v, lam, lam_init, subln_g[l])
        p = multiscale_pool(u, w_pool[l], pool_scale[l])
        h = h + jnp.concatenate([a, p], axis=-1) @ w_out[l]
        h = h + hierarchical_moe(rmsnorm(h, norm2_g[l]), w_group_router[l], b_group_router[l],
                                 w_expert_router[l], b_expert_router[l],
                                 w_gate[l], w_up[l], w_down[l])
    return rmsnorm(h, final_g)
```

```python
import contextlib
import numpy as np
import concourse.bass as bass
import concourse.mybir as mybir
from concourse.bass_utils import run_bass_kernel_spmd

F32 = mybir.dt.float32
BF16 = mybir.dt.bfloat16
AF = mybir.ActivationFunctionType
ALU = mybir.AluOpType
AX = mybir.AxisListType

S = 8192
DM = 1024
NQ = 2048
NT = S // 128
EPS = 1e-6
SLOPES = [2.0 ** (-8.0 * (h + 1) / 4) for h in range(4)]
WINS = (2, 4, 8, 16)
ENGS = ("pe", "act", "dve", "pool", "sp")
CAP = 256
SKIP_T = 64.0
NR = 32 * CAP + 2048


class Prog:
    def __init__(self, nc):
        self.nc = nc
        self.ops = {e: [] for e in ENGS}
        self.cnt = {}
        self.sem_names = []
        for e in ENGS:
            self._newsem(e)

    def _newsem(self, key):
        self.cnt[key] = 0
        self.sem_names.append(key)

    def op(self, eng, fn, deps=(), inc=True, dr=None):
        d = tuple(x for x in deps if x is not None)
        tok = None
        if inc:
            self.cnt[eng] += 1
            tok = (eng, self.cnt[eng])
        if dr is None:
            dr = eng in ("dve", "act", "pool")
        self.ops[eng].append((fn, d, (eng, 1) if inc else None, dr))
        return tok

    def dma(self, queue, fn, semkey, deps=()):
        if semkey not in self.cnt:
            self._newsem(semkey)
        self.cnt[semkey] += 16
        tok = (semkey, self.cnt[semkey])
        d = tuple(x for x in deps if x is not None)
        self.ops[queue].append((fn, d, (semkey, 16), False))
        return tok

    def wait(self, eng, deps):
        d = tuple(x for x in deps if x is not None)
        self.ops[eng].append((None, d, None, False))

    def emit(self):
        nc = self.nc
        with contextlib.ExitStack() as st:
            sems = {k: st.enter_context(nc.semaphore("s_" + k)) for k in self.sem_names}
            block = st.enter_context(nc.Block())
            prog = self

            def run(engname, eng):
                waited = {}
                for fn, deps, inc, dr in prog.ops[engname]:
                    for (k, v) in deps:
                        if k == engname:
                            continue
                        if waited.get(k, 0) < v:
                            eng.wait_ge(sems[k], v)
                            waited[k] = v
                    if fn is None:
                        continue
                    if dr:
                        eng.drain()
                    ins = fn(eng)
                    if inc is not None:
                        ins.then_inc(sems[inc[0]], inc[1])

            @block.tensor
            def _(e):
                run("pe", e)

            @block.scalar
            def _(e):
                run("act", e)

            @block.vector
            def _(e):
                run("dve", e)

            @block.gpsimd
            def _(e):
                run("pool", e)

            @block.sync
            def _(e):
                run("sp", e)


class Buf:
    def __init__(self):
        self.w = None
        self.rs = {}

    def rd(self):
        return [self.w]

    def wr(self):
        return [self.w] + list(self.rs.values())

    def did_r(self, tok):
        if tok is None:
            return
        k, v = tok
        if k not in self.rs or self.rs[k][1] < v:
            self.rs[k] = tok

    def did_w(self, tok):
        if tok is None:
            return
        self.w = tok
        self.rs = {}


def build(stage=99, dbg=False, noattn=False):
    nc = bass.Bass("TRN2", target_bir_lowering=False)

    def DI(name, shape, dt=F32):
        return nc.dram_tensor(name, list(shape), dt, kind="ExternalInput").ap()

    xr = DI("xr", [S, DM])
    w_in = DI("w_in", [DM, 2048])
    w_out = DI("w_out", [DM, DM])
    w_pool = DI("w_pool", [4, 128, 128])
    w_gate = DI("w_gate", [32, DM, 512])
    w_up = DI("w_up", [32, DM, 512])
    w_down = DI("w_down", [32, 512, DM])
    wr_d = DI("wr", [DM, 36])
    vecs = DI("vecs", [3, DM])
    rbias = DI("rbias", [36])
    lams = DI("lams", [4, 64])
    subg = DI("subg", [128])
    pscale = DI("pscale", [128, 4])
    ident = DI("ident", [128, 128])
    kb_d = DI("kb", [128, 3 * 4 * 64])
    csg_d = DI("csg", [2, 4 * 66])
    qpos_d = DI("qpos", [2, NQ])
    dband_d = DI("dband", [128, 2 * 4 * 512])
    sid_d = DI("sid", [128, 4 * 128])
    hv_d = DI("hv", [128, 16])
    rcorr_d = DI("rcorr", [128, 4 * 16])
    ltri_d = DI("ltri", [128, 128])
    eoff_d = DI("eoff", [128, 512])
    out = nc.dram_tensor("out", [NQ, DM], F32, kind="ExternalOutput").ap()
    Xbuf = nc.dram_tensor("Xbuf", [NR, DM], BF16).ap()
    Ybuf = nc.dram_tensor("Ybuf", [NR, DM], F32).ap()
    XTs = nc.dram_tensor("XTs", [16, 128, 4096], BF16).ap()
    Wg_bf = nc.dram_tensor("Wg_bf", [32, 128, 4096], BF16).ap()
    Wu_bf = nc.dram_tensor("Wu_bf", [32, 128, 4096], BF16).ap()
    Wd_bf = nc.dram_tensor("Wd_bf", [32, 128, 4096], BF16).ap()
    dbg_outs = {}

    with contextlib.ExitStack() as st:
        def sb(name, shape, dt):
            return st.enter_context(nc.sbuf_tensor("sb_" + name, list(shape), dt))

        def psb(name, shape, dt):
            return st.enter_context(nc.psum_tensor("ps_" + name, list(shape), dt))

        P = Prog(nc)

        X1 = sb("X1", [128, 37632], BF16)
        X2 = sb("X2", [128, 24576], BF16)
        X3 = sb("X3", [128, 16384], BF16)
        kb = sb("kb", [128, 3, 4, 64], F32)
        csg = sb("csg", [34, 4, 66], BF16)
        qpos = sb("qpos", [34, NQ], BF16)
        dband = sb("dband", [128, 2, 4, 512], BF16)
        sid = sb("sid", [128, 4, 128], BF16)
        idb = sb("idb", [128, 128], BF16)
        hv = sb("hv", [128, 16], F32)
        rcorr = sb("rcorr", [128, 4, 16], F32)
        gvb = sb("gvb", [128, 3, DM], F32)
        rbb = sb("rbb", [128, 36], F32)
        lamb = sb("lamb", [128, 4, 64], F32)
        sg8 = sb("sg8", [128, 128], F32)
        psc = sb("psc", [128, 4], F32)
        wpb = sb("wpb", [128, 4, 128], BF16)
        wrtb = sb("wrtb", [128, 8, 36], BF16)
        epsb = sb("epsb", [128, 1], F32)
        small = sb("small", [128, 64], F32)
        junk = sb("junk", [128, DM], BF16)
        PTb = [sb(f"PT{i}", [128, 512], BF16) for i in range(4)]
        I32 = mybir.dt.int32
        ltri = sb("ltri", [128, 128], BF16)
        onesb = sb("onesb", [128, 128], BF16)
        eoff = sb("eoff", [128, 512], F32)
        Mb_all = sb("Mb_all", [128, 16, 32], BF16)
        idxAll = sb("idxAll", [128, 16, 2], I32)
        gatesAll = sb("gatesAll", [128, 16, 2], F32)
        zt = sb("zt", [128, 1024], BF16)
        RW = sb("RW", [128, 2304], F32)
        tmpA = RW[:, 0:1024].rearrange("p (a b) -> p a b", b=512)
        accS = RW[:, 1024:2304].rearrange("p (b c) -> p b c", c=320)
        L_all = RW[:, 0:576].rearrange("p (t k) -> p t k", k=36)
        gmask = RW[:, 576:640].rearrange("p (t k) -> p t k", k=4)
        gdiff = RW[:, 640:704].rearrange("p (t k) -> p t k", k=4)
        esel = RW[:, 704:832].rearrange("p (t k) -> p t k", k=8)
        mask1 = RW[:, 832:960].rearrange("p (t k) -> p t k", k=8)
        e2 = RW[:, 960:1088].rearrange("p (t k) -> p t k", k=8)
        mask2 = RW[:, 1088:1216].rearrange("p (t k) -> p t k", k=8)
        msum = RW[:, 1216:1344].rearrange("p (t k) -> p t k", k=8)
        Rr = RW[:, 1344:1472].rearrange("p (t k) -> p t k", k=8)
        tmp32 = RW[:, 1472:1984].rearrange("p (t k) -> p t k", k=32)
        V_gmax, V_gsum, V_m1, V_m2, V_ex, V_den = [RW[:, 1984 + 16 * q: 2000 + 16 * q] for q in range(6)]
        slf = RW[:, 2080:2112].rearrange("p (t k) -> p t k", k=2)
        ss_t = sb("ss_t", [128, 4], F32)
        rs_t = sb("rs_t", [128, 4], F32)

        banks = [psb(f"bank{i}", [128, 512], F32) for i in range(8)]
        bankB = [Buf() for _ in range(8)]

        a_tok = X3[:, 0:8192].rearrange("p (t c) -> p t c", c=512)
        pT = X3[:, 8192:16384].rearrange("p (g t) -> p g t", t=NQ)
        xn2T = X3[:, :].rearrange("p (c t) -> p c t", t=NQ)
        KT = X1[:, 0:16384].rearrange("p (h t) -> p h t", t=S)
        VA = X1[:, 16384:16384 + 64 * 2 * 130].rearrange("p (m h e) -> p m h e", h=2, e=130)
        QT = X1[:, 33024:33024 + 4096].rearrange("p (h t) -> p h t", t=NQ)
        UT = X1[:, 0:4 * 2064 * 2].bitcast(F32).rearrange("p (g t) -> p g t", t=2064)
        Sa = X1[:, 16512:16512 + 4128].bitcast(F32)
        Sb = X1[:, 20640:20640 + 4128].bitcast(F32)
        dpool = X1[:, 24768:24768 + NQ]
        h1 = X1[:, 0:32768].bitcast(F32).rearrange("p (t d) -> p t d", d=DM)
        Wk2 = X2[:, 0:2048].rearrange("p (c n) -> p c n", n=256)
        Wv2 = X2[:, 2048:4096].rearrange("p (c n) -> p c n", n=256)
        Wq2 = X2[:, 4096:6144].rearrange("p (c n) -> p c n", n=256)
        Wu4 = X2[:, 0:4096].rearrange("p (c n) -> p c n", n=512)
        xtb = [X2[:, 6144 + i * 2048: 6144 + (i + 1) * 2048].bitcast(F32) for i in range(3)]
        xsb = [X2[:, 12288 + i * 1024: 12288 + (i + 1) * 1024] for i in range(3)]
        xTc = [X2[:, 15360 + i * 4096: 15360 + (i + 1) * 4096].rearrange("p (c t) -> p c t", t=512) for i in range(2)]
        xTc_flat = [X2[:, 15360 + i * 4096: 15360 + (i + 1) * 4096] for i in range(2)]
        Wo = X2[:, 12288:20480].rearrange("p (c n) -> p c n", n=DM)
        aTt = [X2[:, 20480 + i * 512: 20480 + (i + 1) * 512].rearrange("p (c t) -> p c t", t=128) for i in range(2)]
        xnAll = X3[:, :].rearrange("p (t d) -> p t d", d=DM)
        xnT2 = [X1[:, 32768 + i * 1024: 32768 + (i + 1) * 1024].rearrange("p (c t) -> p c t", t=128) for i in range(2)]
        XeT = [X3[:, 2048 + i * 2048: 2048 + (i + 1) * 2048].rearrange("p (c t) -> p c t", t=256) for i in range(2)]
        xe = [X3[:, 6144 + i * 2048: 6144 + (i + 1) * 2048].rearrange("p (s d) -> p s d", d=DM) for i in range(2)]
        ye = [X3[:, 10240 + i * 2048: 10240 + (i + 1) * 2048].bitcast(F32) for i in range(2)]
        HT = [X3[:, 14336 + i * 1024: 14336 + (i + 1) * 1024].rearrange("p (c t) -> p c t", t=256) for i in range(2)]
        gb = [X2[:, i * 2048:(i + 1) * 2048].bitcast(F32) for i in range(8)]
        wg_flat = [X2[:, i * 12288: i * 12288 + 4096] for i in range(2)]
        wu_flat = [X2[:, i * 12288 + 4096: i * 12288 + 8192] for i in range(2)]
        wd_flat = [X2[:, i * 12288 + 8192: i * 12288 + 12288] for i in range(2)]
        wgb = [X2[:, i * 12288: i * 12288 + 4096].rearrange("p (c f) -> p c f", f=512) for i in range(2)]
        wub = [X2[:, i * 12288 + 4096: i * 12288 + 8192].rearrange("p (c f) -> p c f", f=512) for i in range(2)]
        wdb = [X2[:, i * 12288 + 8192: i * 12288 + 12288].rearrange("p (c n) -> p c n", n=DM) for i in range(2)]
        HTb = [X1[:, 32768 + i * 2048: 32768 + (i + 1) * 2048].rearrange("p (c t) -> p c t", t=512) for i in range(2)]

        B = {}

        def buf(name):
            if name not in B:
                B[name] = Buf()
            return B[name]

        def ld(queue, dst, src, name):
            t = P.dma(queue, lambda e: e.dma_start(out=dst, in_=src), "ld_" + name)
            buf(name).did_w(t)
            return t

        ld("sp", kb[:].rearrange("p a b c -> p (a b c)"), kb_d[:, :], "kb")
        ld("pool", csg[0:2].rearrange("p a b -> p (a b)"), csg_d[:, :], "csg")
        ld("pool", qpos[0:2], qpos_d[:, :], "qpos")
        ld("pool", csg[32:34].rearrange("p a b -> p (a b)"), csg_d[:, :], "csg2")
        ld("pool", qpos[32:34], qpos_d[:, :], "qpos2")
        ld("pool", dband[:].rearrange("p a b c -> p (a b c)"), dband_d[:, :], "dband")
        ld("pool", sid[:].rearrange("p a b -> p (a b)"), sid_d[:, :], "sid")
        ld("pool", idb[:], ident[:, :], "idb")
        ld("sp", hv[:], hv_d[:, :], "hv")
        ld("sp", rcorr[:].rearrange("p a b -> p (a b)"), rcorr_d[:, :], "rcorr")
        for i in range(3):
            ld("sp", gvb[:, i, :], vecs[i, :].partition_broadcast(128), f"gv{i}")
        ld("sp", rbb[:], rbias.partition_broadcast(128), "rbb")
        ld("sp", lamb[:].rearrange("p a b -> p (a b)"), lams.rearrange("a b -> (a b)").partition_broadcast(128), "lamb")
        ld("sp", sg8[:], subg.partition_broadcast(128), "sg8")
        ld("sp", psc[:], pscale[:, :], "psc")
        ld("pool", wpb[:], w_pool.rearrange("g c d -> c g d"), "wpb")
        ld("pool", wrtb[:], wr_d.rearrange("(c p) n -> p c n", p=128), "wrtb")
        ld("pool", ltri[:], ltri_d[:, :], "ltri")
        ld("sp", eoff[:], eoff_d[:, :], "eoff")

        t = P.op("dve", lambda e: e.memset(epsb[:], EPS))
        buf("epsb").did_w(t)
        tz0 = P.op("dve", lambda e: e.memset(zt[:], 0.0))

        def zero_fill():
            tzf = None
            for r0 in range(0, NR, 128):
                tzf = P.dma("pool", (lambda r0: lambda e: e.dma_start(out=Xbuf[r0:r0 + 128, :], in_=zt[:]))(r0), "zf", [tz0])
            for r0 in range(32 * CAP, NR, 128):
                for h0 in (0, 512):
                    tzf = P.dma("pool", (lambda r0, h0: lambda e: e.dma_start(out=Ybuf[r0:r0 + 128, h0:h0 + 512], in_=zt[:].bitcast(F32)))(r0, h0), "zf", [tz0])
            buf("zf").did_w(tzf)

        pc_last = {}

        def precast_weights(lo, hi, deps):
            jobs = []
            for ex in range(lo, hi):
                for r0 in (0, 512):
                    c0 = r0 // 128
                    jobs.append((Wg_bf[ex].rearrange("p (c f) -> c p f", f=512)[c0:c0 + 4],
                                 w_gate[ex, r0:r0 + 512, :].rearrange("(c p) f -> c p f", p=128)))
                    jobs.append((Wu_bf[ex].rearrange("p (c f) -> c p f", f=512)[c0:c0 + 4],
                                 w_up[ex, r0:r0 + 512, :].rearrange("(c p) f -> c p f", p=128)))
                for r0 in (0, 256):
                    c0 = r0 // 128
                    jobs.append((Wd_bf[ex].rearrange("p (c n) -> c p n", n=DM)[c0:c0 + 2],
                                 w_down[ex, r0:r0 + 256, :].rearrange("(c p) n -> c p n", p=128)))
            for n_, (dst_, src_) in enumerate(jobs):
                key = f"pc{n_ % 2}"
                prev = [pc_last[key]] if key in pc_last else []
                pc_last[key] = P.dma("pool", (lambda dst_, src_: lambda e: e.dma_start(out=dst_, in_=src_))(dst_, src_), key, list(deps) + prev)
        t = P.op("dve", lambda e: e.tensor_scalar(out=sg8[:], in0=sg8[:], scalar1=0.8, scalar2=None, op0=ALU.mult),
                 buf("sg8").rd())
        buf("sg8").did_w(t)
        t = P.op("dve", lambda e: e.tensor_tensor(out=lamb[:, 0, :], in0=lamb[:, 0, :], in1=lamb[:, 1, :], op=ALU.mult),
                 buf("lamb").rd())
        t = P.op("dve", lambda e: e.tensor_tensor(out=lamb[:, 2, :], in0=lamb[:, 2, :], in1=lamb[:, 3, :], op=ALU.mult))
        t = P.op("dve", lambda e: e.reduce_sum(out=small[:, 1:2], in_=lamb[:, 0, :], axis=AX.X))
        t = P.op("dve", lambda e: e.reduce_sum(out=small[:, 2:3], in_=lamb[:, 2, :], axis=AX.X))
        t = P.op("act", lambda e: e.activation(out=small[:, 1:3], in_=small[:, 1:3], func=AF.Exp), [t])
        t = P.op("dve", lambda e: e.tensor_tensor(out=small[:, 0:1], in0=small[:, 2:3], in1=small[:, 1:2], op=ALU.subtract), [t])
        t = P.op("dve", lambda e: e.tensor_scalar(out=small[:, 0:1], in0=small[:, 0:1], scalar1=-0.2, scalar2=None, op0=ALU.add))
        buf("neglam").did_w(t)

        fe_cnt = [0]
        pTr_banks = [6, 7]

        def fe_A(m, gidx=0):
            i = fe_cnt[0] % 3
            ib = fe_cnt[0] % 2
            fe_cnt[0] += 1
            bx, bs = buf(f"xt{i}"), buf(f"xs{i}")
            tx = P.dma("sp", lambda e: e.dma_start(out=xtb[i], in_=xr[m * 128:(m + 1) * 128, :]), f"ld_xt{i}", bx.wr())
            bx.did_w(tx)
            col = (fe_cnt[0] % 4)
            t1 = P.op("act", lambda e: e.activation(out=junk[:], in_=xtb[i], func=AF.Square, accum_out=ss_t[:, col:col + 1]),
                      bx.rd() + buf("junk").wr() + buf(f"ss{col}").wr(), dr=False)
            buf("junk").did_w(t1)
            bx.did_r(t1)
            t2 = P.op("act", lambda e: e.activation(out=rs_t[:, col:col + 1], in_=ss_t[:, col:col + 1], func=AF.Sqrt,
                                                    bias=epsb[:, 0:1], scale=1.0 / DM), buf("epsb").rd() + buf(f"rs{col}").wr())
            t3 = P.op("dve", lambda e: e.reciprocal(out=rs_t[:, col:col + 1], in_=rs_t[:, col:col + 1]), [t2], dr=False)
            buf(f"ss{col}").did_r(t2)
            t4 = P.op("dve", lambda e: e.scalar_tensor_tensor(out=xsb[i], in0=xtb[i], scalar=rs_t[:, col:col + 1],
                                                               in1=gvb[:, gidx, :], op0=ALU.mult, op1=ALU.mult),
                      bx.rd() + bs.wr() + buf(f"gv{gidx}").rd())
            buf(f"ss{col}").did_w(t1)
            buf(f"rs{col}").did_w(t3)
            buf(f"rs{col}").did_r(t4)
            bx.did_r(t4)
            bs.did_w(t4)
            return (i, ib)

        def fe_B(ctx, dst, dstB):
            i, ib = ctx
            bs = buf(f"xs{i}")
            bk = pTr_banks[ib]
            pv = banks[bk][:].bitcast(BF16).rearrange("p (c t) -> p c t", t=128)
            tl = None
            for c in range(8):
                tl = P.op("pe", (lambda c: lambda e: e.transpose(out=pv[:, c, :], in_=xsb[i][:, c * 128:(c + 1) * 128],
                                                                 identity=idb[:]))(c),
                          bs.rd() + bankB[bk].wr() + buf("idb").rd(), inc=(c == 7))
            bs.did_r(tl)
            bankB[bk].did_w(tl)
            t5 = P.op("act", lambda e: e.copy(out=dst, in_=pv), bankB[bk].rd() + dstB.wr(), dr=False)
            bankB[bk].did_r(t5)
            dstB.did_w(t5)
            return t5

        def front_end(m, dst, dstB, gidx=0):
            return fe_B(fe_A(m, gidx), dst, dstB)

        if stage >= 1:
            tw = P.dma("pool", lambda e: e.dma_start(out=Wu4, in_=w_in[:, 1536:2048].rearrange("(c p) n -> p c n", p=128)), "ld_w0")
            buf("W").did_w(tw)
            tiles = [63] + list(range(16)) + [16]
            pk = 0
            ctxP = {0: fe_A(tiles[0]), 1: fe_A(tiles[1])}
            for idx, m in enumerate(tiles):
                i = idx % 2
                dst = xTc[i][:, :, 0:128]
                dB = buf(f"xTc{i}")
                if idx + 2 < len(tiles):
                    ctxP[idx + 2] = fe_A(tiles[idx + 2])
                t5p = fe_B(ctxP[idx], dst, dB)
                if 1 <= idx <= 16 and stage >= 2:
                    tsx = P.dma("pool", (lambda m, i: lambda e: e.dma_start(
                        out=XTs[m // 4].rearrange("p (c t) -> p c t", t=512)[:, :, (m % 4) * 128:(m % 4 + 1) * 128],
                        in_=xTc[i][:, :, 0:128]))(m, i), "st_xts", [t5p])
                    dB.did_r(tsx)
                    buf("xts").did_w(tsx)
                if idx == 0:
                    c0, n, uo = 120, 8, 0
                elif idx == 17:
                    c0, n, uo = 0, 8, 8 + NQ
                else:
                    c0, n, uo = 0, 128, 8 + (idx - 1) * 128
                bk = pk % 4
                pk += 1
                pu = banks[bk][:].rearrange("p (g t) -> p g t", t=128)
                tl = None
                for g in range(4):
                    for c in range(8):
                        tl = P.op("pe", (lambda g, c, c0, n, i, pu: lambda e: e.matmul(
                            pu[:, g, 0:n], lhsT=Wu4[:, c, g * 128:(g + 1) * 128], rhs=xTc[i][:, c, c0:c0 + n],
                            start=(c == 0), stop=(c == 7)))(g, c, c0, n, i, pu),
                            dB.rd() + buf("W").rd() + bankB[bk].wr(), inc=(g == 3 and c == 7))
                dB.did_r(tl)
                bankB[bk].did_w(tl)
                t = P.op("dve", (lambda n, uo, pu: lambda e: e.tensor_copy(out=UT[:, :, uo:uo + n], in_=pu[:, :, 0:n]))(n, uo, pu),
                         bankB[bk].rd() + buf("UT").wr(), dr=False)
                bankB[bk].did_r(t)
                buf("UTw").did_w(t)
            buf("W").did_r(tl)
            for g in range(4):
                t = P.op("dve", (lambda g: lambda e: e.tensor_tensor(out=UT[:, g, 0:8], in0=UT[:, g, 0:8], in1=hv[:, 0:8], op=ALU.mult))(g),
                         buf("hv").rd() + buf("UTw").rd())
                t = P.op("dve", (lambda g: lambda e: e.tensor_tensor(out=UT[:, g, 8 + NQ:16 + NQ], in0=UT[:, g, 8 + NQ:16 + NQ],
                                                                     in1=hv[:, 8:16], op=ALU.mult))(g))
            L = 2064
            for g in range(4):
                w = WINS[g]
                U = UT[:, g, :]
                t = P.op("dve", (lambda U: lambda e: e.tensor_tensor(out=Sa[:, 1:L], in0=U[:, 0:L - 1], in1=U[:, 1:L], op=ALU.add))(U),
                         buf("dpool").rd())
                cur, oth = Sa, Sb
                if w >= 4:
                    t = P.op("dve", (lambda cur, oth: lambda e: e.tensor_tensor(out=oth[:, 2:L - 1], in0=cur[:, 1:L - 2], in1=cur[:, 3:L], op=ALU.add))(cur, oth))
                    cur, oth = oth, cur
                if w >= 8:
                    t = P.op("dve", (lambda cur, oth: lambda e: e.tensor_tensor(out=oth[:, 4:L - 3], in0=cur[:, 2:L - 5], in1=cur[:, 6:L - 1], op=ALU.add))(cur, oth))
                    cur, oth = oth, cur
                if w >= 16:
                    t = P.op("dve", (lambda cur, oth: lambda e: e.tensor_tensor(out=oth[:, 8:L - 7], in0=cur[:, 4:L - 11], in1=cur[:, 12:L - 3], op=ALU.add))(cur, oth))
                    cur, oth = oth, cur
                t = P.op("dve", (lambda cur, U, w: lambda e: e.scalar_tensor_tensor(
                    out=dpool, in0=cur[:, 8:8 + NQ], scalar=1.0 / w, in1=U[:, 8:8 + NQ], op0=ALU.mult, op1=ALU.subtract))(cur, U, w),
                    buf("dpool").wr())
                for (lo, ro) in ((0, 0), (NQ - 8, 8)):
                    t = P.op("dve", (lambda cur, oth, g, lo, ro: lambda e: e.tensor_tensor(
                        out=oth[:, 0:8], in0=cur[:, 8 + lo:16 + lo], in1=rcorr[:, g, ro:ro + 8], op=ALU.mult))(cur, oth, g, lo, ro),
                        buf("rcorr").rd())
                    t = P.op("dve", (lambda U, oth, lo: lambda e: e.tensor_tensor(
                        out=dpool[:, lo:lo + 8], in0=oth[:, 0:8], in1=U[:, 8 + lo:16 + lo], op=ALU.subtract))(U, oth, lo))
                buf("dpool").did_w(t)
                for ch in range(4):
                    bk = pk % 4
                    pk += 1
                    tm = P.op("pe", (lambda g, ch, bk: lambda e: e.matmul(banks[bk][:], lhsT=wpb[:, g, :], rhs=dpool[:, ch * 512:(ch + 1) * 512],
                                                                          start=True, stop=True))(g, ch, bk),
                              buf("dpool").rd() + buf("wpb").rd() + bankB[bk].wr())
                    bankB[bk].did_w(tm)
                    buf("dpool").did_r(tm)
                    t = P.op("act", (lambda g, ch, bk: lambda e: e.activation(out=pT[:, g, ch * 512:(ch + 1) * 512], in_=banks[bk][:],
                                                                              func=AF.Copy, scale=psc[:, g:g + 1]))(g, ch, bk),
                             bankB[bk].rd() + buf("psc").rd(), dr=False)
                    bankB[bk].did_r(t)
                    buf("pT").did_w(t)
            buf("X1").did_r(tm)
            buf("X1").did_r(t)

        acc_loc = {}
        k = 0
        for c in range(2):
            for qt in range(4):
                acc_loc[(c, qt)] = (4 + k // 2, (k % 2) * 160)
                k += 1
        sc_cnt = [0]
        pt_cnt = [0]

        att_state = {"zeroed": False, "deferred": None}

        def attention(hp):
            att_state["zeroed"] = False
            for hh in range(2):
                h = 2 * hp + hh
                for j in range(4):
                    pend = None
                    if not att_state["zeroed"]:
                        for bk_ in (4, 5, 6, 7):
                            tz = P.op("dve", (lambda bk_: lambda e: e.memset(banks[bk_][:, 0:320], 0.0))(bk_), bankB[bk_].wr(), dr=False)
                            bankB[bk_].did_w(tz)
                    att_state["zeroed"] = False
                    sl_ = SLOPES[h]
                    mlist = []
                    for m_ in range(NT):
                        if m_ < 16:
                            if m_ < 4 * j:
                                dmin = 512 * j - (128 * m_ + 127)
                            elif m_ > 4 * j + 3:
                                dmin = 128 * m_ - (512 * j + 511)
                            else:
                                dmin = 0
                        else:
                            dmin = min(128 * m_ - (512 * j + 511), 512 * j + S - (128 * m_ + 127))
                        if sl_ * dmin >= SKIP_T:
                            continue
                        mlist.append(m_)
                    for mi in range(len(mlist) + 1):
                        if mi == 3 and att_state["deferred"] is not None:
                            att_state["deferred"]()
                            att_state["deferred"] = None
                        cur = None
                        m = mlist[mi] if mi < len(mlist) else NT
                        if mi < len(mlist):
                            if m < 16:
                                i = m - 4 * j
                                if i < 0:
                                    kind, col, band = 0, 65, None
                                elif i > 3:
                                    kind, col, band = 1, 64, None
                                else:
                                    kind, col, band = None, None, i
                            else:
                                kind, col, band = 2, m, None
                            cur = []
                            bks = []
                            for c in range(2):
                                bk = sc_cnt[0] % 4
                                sc_cnt[0] += 1
                                bks.append(bk)
                                P.op("pe", (lambda c, hh, m, j, bk: lambda e: e.matmul(
                                    banks[bk][:], lhsT=KT[64 * c:64 * c + 64, hh, m * 128:(m + 1) * 128],
                                    rhs=QT[64 * c:64 * c + 64, hh, j * 512:(j + 1) * 512], start=True, stop=False))(c, hh, m, j, bk),
                                    bankB[bk].wr() + buf("KT").rd() + buf("QT").rd(), inc=False)
                            tqs = []
                            for c in range(2):
                                bk = bks[c]
                                if band is None:
                                    p0 = 32 * c
                                    tq = P.op("pe", (lambda h, col, j, bk, p0: lambda e: e.matmul(
                                        banks[bk][:], lhsT=csg[p0:p0 + 2, h, col:col + 1].to_broadcast([2, 128]),
                                        rhs=qpos[p0:p0 + 2, j * 512:(j + 1) * 512], start=False, stop=True))(h, col, j, bk, p0),
                                        buf("csg").rd() + buf("qpos").rd() + buf("csg2").rd() + buf("qpos2").rd())
                                else:
                                    P.op("pe", (lambda h, band, bk: lambda e: e.matmul(
                                        banks[bk][:], lhsT=sid[:, h, :], rhs=dband[:, 0, band, :], start=False, stop=False))(h, band, bk),
                                        buf("sid").rd() + buf("dband").rd(), inc=False)
                                    tq = P.op("pe", (lambda h, band, bk: lambda e: e.matmul(
                                        banks[bk][:], lhsT=sid[:, h, :], rhs=dband[:, 1, band, :], start=False, stop=True))(h, band, bk))
                                bankB[bk].did_w(tq)
                            for c in range(2):
                                bk = bks[c]
                                pi = pt_cnt[0] % 4
                                pt_cnt[0] += 1
                                pb = buf(f"PT{pi}")
                                if kind is None:
                                    te = P.op("act", (lambda bk, pi: lambda e: e.activation(
                                        out=PTb[pi][:], in_=banks[bk][:], func=AF.Exp, scale=0.125))(bk, pi),
                                        bankB[bk].rd() + pb.wr(), dr=False)
                                else:
                                    te = P.op("act", (lambda bk, pi, kind, h, m: lambda e: e.activation(
                                        out=PTb[pi][:], in_=banks[bk][:], func=AF.Exp, bias=kb[:, kind, h, m:m + 1], scale=0.125))(bk, pi, kind, h, m),
                                        bankB[bk].rd() + pb.wr() + buf("kb").rd(), dr=False)
                                bankB[bk].did_r(te)
                                pb.did_w(te)
                                cur.append((c, pi, m, mi == 0, mi == len(mlist) - 1))
                        if pend is not None:
                            for (c, pi, mm_, first_, last_) in pend:
                                pb = buf(f"PT{pi}")
                                tl = None
                                for qt in range(4):
                                    bk, off = acc_loc[(c, qt)]
                                    tl = P.op("pe", (lambda pi, qt, bk, off, mm_, hh, last_: lambda e: e.matmul(
                                        banks[bk][:, off:off + 129], lhsT=PTb[pi][:, qt * 128:(qt + 1) * 128],
                                        rhs=VA[:, mm_, hh, 0:129], start=False, stop=last_,
                                        skip_group_check=True))(pi, qt, bk, off, mm_, hh, last_),
                                        pb.rd() + buf("VA").rd() + (bankB[bk].wr() if first_ else []), inc=(qt == 3))
                                pb.did_r(tl)
                                if last_:
                                    for bk in (4, 5, 6, 7):
                                        bankB[bk].did_w(tl)
                        pend = cur
                    if att_state["deferred"] is not None:
                        att_state["deferred"]()
                        att_state["deferred"] = None
                    cps = []
                    for bi, bk in enumerate((4, 5, 6, 7)):
                        eng_ = "act" if bi % 2 == 0 else "dve"
                        if eng_ == "act":
                            tcp = P.op("act", (lambda bi, bk: lambda e: e.copy(out=accS[:, bi, 0:289], in_=banks[bk][:, 0:289]))(bi, bk),
                                       bankB[bk].rd() + buf("accS").wr(), dr=False)
                        else:
                            tcp = P.op("dve", (lambda bi, bk: lambda e: e.tensor_copy(out=accS[:, bi, 0:289], in_=banks[bk][:, 0:289]))(bi, bk),
                                       bankB[bk].rd() + buf("accS").wr(), dr=False)
                        bankB[bk].did_r(tcp)
                        cps.append(tcp)
                    for bk_ in (4, 5, 6, 7):
                        tz = P.op("dve", (lambda bk_: lambda e: e.memset(banks[bk_][:, 0:320], 0.0))(bk_), bankB[bk_].wr(), dr=False)
                        bankB[bk_].did_w(tz)
                    att_state["zeroed"] = True
                    td1 = None
                    for qt in range(4):
                        b0, o0 = acc_loc[(0, qt)]
                        b1, o1 = acc_loc[(1, qt)]
                        sc0 = 16 + 8 * qt
                        P.op("dve", (lambda b0, o0, sc0: lambda e: e.reciprocal(out=small[:, sc0:sc0 + 1], in_=accS[:, b0 - 4, o0 + 128:o0 + 129]))(b0, o0, sc0),
                             cps + buf("nrm").wr() + buf("tmpA").wr())
                        P.op("dve", (lambda b1, o1, sc0: lambda e: e.reciprocal(out=small[:, sc0 + 1:sc0 + 2], in_=accS[:, b1 - 4, o1 + 128:o1 + 129]))(b1, o1, sc0))
                        P.op("dve", (lambda sc0: lambda e: e.tensor_tensor(out=small[:, sc0 + 2:sc0 + 3], in0=small[:, sc0 + 1:sc0 + 2], in1=small[:, 0:1], op=ALU.mult))(sc0),
                             buf("neglam").rd())
                        P.op("dve", (lambda b0, o0, sc0, qt: lambda e: e.tensor_scalar(out=tmpA[:, 0, qt * 128:(qt + 1) * 128], in0=accS[:, b0 - 4, o0:o0 + 128],
                                                                                         scalar1=small[:, sc0:sc0 + 1], scalar2=None, op0=ALU.mult))(b0, o0, sc0, qt))
                        td1 = P.op("dve", (lambda b1, o1, sc0, qt: lambda e: e.scalar_tensor_tensor(
                            out=tmpA[:, 0, qt * 128:(qt + 1) * 128], in0=accS[:, b1 - 4, o1:o1 + 128], scalar=small[:, sc0 + 2:sc0 + 3],
                            in1=tmpA[:, 0, qt * 128:(qt + 1) * 128], op0=ALU.mult, op1=ALU.add))(b1, o1, sc0, qt))
                    buf("accS").did_w(cps[0])
                    buf("accS").did_r(cps[1])
                    buf("accS").did_r(cps[2])
                    buf("accS").did_r(cps[3])
                    buf("accS").did_r(td1)
                    buf("tmpA").did_w(td1)

                    def norm_tail(j=j, h=h, td1=td1):
                        ta1 = None
                        for qt in range(4):
                            sc0 = 16 + 8 * qt
                            P.op("act", (lambda qt, sc0: lambda e: e.activation(out=tmpA[:, 1, 0:128], in_=tmpA[:, 0, qt * 128:(qt + 1) * 128], func=AF.Square,
                                                                                accum_out=small[:, sc0 + 3:sc0 + 4]))(qt, sc0), [td1], dr=False)
                            ta1 = P.op("act", (lambda sc0: lambda e: e.activation(out=small[:, sc0 + 4:sc0 + 5], in_=small[:, sc0 + 3:sc0 + 4], func=AF.Sqrt,
                                                                                  bias=epsb[:, 0:1], scale=1.0 / 128))(sc0))
                        t4 = None
                        for qt in range(4):
                            sc0 = 16 + 8 * qt
                            P.op("dve", (lambda sc0: lambda e: e.reciprocal(out=small[:, sc0 + 4:sc0 + 5], in_=small[:, sc0 + 4:sc0 + 5]))(sc0), [ta1])
                            t4 = P.op("dve", (lambda j, qt, h, sc0: lambda e: e.scalar_tensor_tensor(
                                out=a_tok[:, 4 * j + qt, h * 128:(h + 1) * 128], in0=tmpA[:, 0, qt * 128:(qt + 1) * 128], scalar=small[:, sc0 + 4:sc0 + 5],
                                in1=sg8[:], op0=ALU.mult, op1=ALU.mult))(j, qt, h, sc0), buf("sg8").rd())
                        buf("tmpA").did_r(t4)
                        buf("tmpA").did_r(ta1)
                        buf("nrm").did_r(t4)
                        buf("nrm").did_w(t4)
                        buf("a_tok").did_w(t4)

                    att_state["deferred"] = norm_tail
            if att_state["deferred"] is not None:
                att_state["deferred"]()
                att_state["deferred"] = None

        if noattn:
            t = P.op("dve", lambda e: e.memset(X3[:, 0:8192], 0.0))
            buf("a_tok").did_w(t)
        if stage >= 2 and not noattn:
            for hp in range(2):
                dW = buf("W").wr() + buf("X1").wr()
                c0 = 2 * hp * 128
                tw = P.dma("pool", lambda e, c0=c0: e.dma_start(out=Wq2, in_=w_in[:, c0:c0 + 256].rearrange("(c p) n -> p c n", p=128)), "ld_w0", dW)
                tw = P.dma("pool", lambda e, c0=c0: e.dma_start(out=Wk2, in_=w_in[:, 512 + c0:512 + c0 + 256].rearrange("(c p) n -> p c n", p=128)), "ld_w0", dW)
                tw = P.dma("pool", lambda e, c0=c0: e.dma_start(out=Wv2, in_=w_in[:, 1024 + c0:1024 + c0 + 256].rearrange("(c p) n -> p c n", p=128)), "ld_w0", dW)
                buf("W").did_w(tw)
                t = P.op("dve", lambda e: e.memset(VA[:, :, :, 128:130], 1.0), buf("X1").wr() + buf("VA").wr() + buf("KT").wr())
                buf("VA").did_w(t)
                buf("KT").did_w(t)
                buf("QT").did_w(t)
                pk = 0
                last = None
                st_ = {"pk": 0, "last": None, "lastv": None, "tl": None}

                def proj_part(ch, part):
                    i = ch % 2
                    dB = buf(f"xTc{i}")
                    groups = [("k", 0), ("k", 1)] + ([("q", 0), ("q", 1)] if ch < 4 else [])
                    for gi_, (kind, hh) in enumerate(groups):
                        if gi_ % 4 != part:
                            continue
                        bk = st_["pk"] % 4
                        st_["pk"] += 1
                        Wt = Wk2 if kind == "k" else Wq2
                        tl = None
                        for c in range(8):
                            tl = P.op("pe", (lambda Wt, c, hh, i, bk: lambda e: e.matmul(
                                banks[bk][:], lhsT=Wt[:, c, hh * 128:(hh + 1) * 128], rhs=xTc[i][:, c, :],
                                start=(c == 0), stop=(c == 7)))(Wt, c, hh, i, bk),
                                dB.rd() + buf("W").rd() + bankB[bk].wr(), inc=(c == 7))
                        bankB[bk].did_w(tl)
                        dstT = KT if kind == "k" else QT
                        t = P.op("dve", (lambda dstT, hh, ch, bk: lambda e: e.tensor_copy(
                            out=dstT[:, hh, ch * 512:(ch + 1) * 512], in_=banks[bk][:]))(dstT, hh, ch, bk),
                            bankB[bk].rd() + buf("KT").rd(), dr=False)
                        bankB[bk].did_r(t)
                        st_["last"] = t
                        dB.did_r(tl)
                    tt = part
                    bk = st_["pk"] % 4
                    st_["pk"] += 1
                    m = 4 * ch + tt
                    tl = None
                    for c in range(8):
                        tl = P.op("pe", (lambda c, tt, i, bk: lambda e: e.matmul(
                            banks[bk][:, 0:256], lhsT=xTc[i][:, c, tt * 128:(tt + 1) * 128], rhs=Wv2[:, c, :],
                            start=(c == 0), stop=(c == 7)))(c, tt, i, bk),
                            dB.rd() + buf("W").rd() + bankB[bk].wr(), inc=(c == 7))
                    bankB[bk].did_w(tl)
                    t = P.op("act", (lambda m, bk: lambda e: e.copy(
                        out=VA[:, m, :, 0:128], in_=banks[bk][:, 0:256].rearrange("p (h e) -> p h e", e=128)))(m, bk),
                        bankB[bk].rd() + buf("VA").rd(), dr=False)
                    bankB[bk].did_r(t)
                    st_["lastv"] = t
                    st_["tl"] = tl
                    dB.did_r(tl)

                def load_chunk(ch_):
                    i2 = ch_ % 2
                    dB2 = buf(f"xTc{i2}")
                    tlx = P.dma("sp", (lambda ch_, i2: lambda e: e.dma_start(out=xTc_flat[i2], in_=XTs[ch_]))(ch_, i2), f"ld_xc{i2}",
                                dB2.wr() + buf("xts").rd())
                    dB2.did_w(tlx)

                if hp == 0:
                    load_chunk(0)
                    load_chunk(1)
                    ctxA = {16: fe_A(16), 17: fe_A(17)}

                    def feB_tile(n):
                        if n + 2 < 64:
                            ctxA[n + 2] = fe_A(n + 2)
                        i2 = (n // 4) % 2
                        dB2 = buf(f"xTc{i2}")
                        t5 = fe_B(ctxA.pop(n), xTc[i2][:, :, (n % 4) * 128:(n % 4 + 1) * 128], dB2)
                        if n % 4 == 3:
                            ch_ = n // 4
                            tsx = P.dma("pool", (lambda ch_, i2: lambda e: e.dma_start(out=XTs[ch_], in_=xTc_flat[i2]))(ch_, i2), "st_xts", [t5])
                            dB2.did_r(tsx)
                            buf("xts").did_w(tsx)

                    for ch in range(16):
                        for tt in range(4):
                            if 4 <= ch + 1 < 16:
                                feB_tile(4 * (ch + 1) + tt)
                            proj_part(ch, tt)
                        if ch + 2 < 4:
                            load_chunk(ch + 2)
                else:
                    load_chunk(0)
                    load_chunk(1)
                    for ch in range(16):
                        for tt in range(4):
                            proj_part(ch, tt)
                        if ch + 2 < 16:
                            load_chunk(ch + 2)
                tl, last, lastv = st_["tl"], st_["last"], st_["lastv"]
                buf("W").did_r(tl)
                buf("KT").did_w(last)
                buf("QT").did_w(last)
                buf("VA").did_w(lastv)
                if stage >= 6:
                    pdeps = [last, lastv]
                    if hp == 0:
                        zero_fill()
                        precast_weights(0, 8, pdeps)
                    else:
                        precast_weights(8, 32, pdeps)
                if stage >= 3:
                    attention(hp)
                    tk = (("pe", P.cnt["pe"]))
                    buf("X1").did_r(tk)
                    buf("KT").did_r(tk)
                    buf("VA").did_r(tk)
                    buf("QT").did_r(tk)

        if stage >= 4:
            dW = buf("W").wr() + [("pe", P.cnt["pe"])]
            for c_ in range(8):
                tw = P.dma("pool", (lambda c_: lambda e: e.dma_start(out=Wo[:, c_, :], in_=w_out[c_ * 128:(c_ + 1) * 128, :]))(c_), "ld_w0", dW)
            buf("W").did_w(tw)
            for t_ in range(16):
                i = t_ % 2
                bx = buf(f"xt{i}")
                tx = P.dma("sp", (lambda t_, i: lambda e: e.dma_start(out=xtb[i], in_=xr[t_ * 128:(t_ + 1) * 128, :]))(t_, i), f"ld_xt{i}", bx.wr())
                bx.did_w(tx)
                bk = 6 + i
                pv = banks[bk][:].bitcast(BF16).rearrange("p (c t) -> p c t", t=128)
                tl = None
                for c in range(4):
                    tl = P.op("pe", (lambda c, t_, pv: lambda e: e.transpose(out=pv[:, c, :], in_=a_tok[:, t_, c * 128:(c + 1) * 128], identity=idb[:]))(c, t_, pv),
                              buf("a_tok").rd() + bankB[bk].wr(), inc=(c == 3))
                bankB[bk].did_w(tl)
                ba = buf(f"aT{i}")
                t5 = P.op("act", (lambda i, pv: lambda e: e.copy(out=aTt[i], in_=pv[:, 0:4, :]))(i, pv), bankB[bk].rd() + ba.wr(), dr=False)
                bankB[bk].did_r(t5)
                ba.did_w(t5)
                for half in range(2):
                    bk2 = 2 * i + half
                    tl = None
                    for c in range(8):
                        lhs = aTt[i][:, c, :] if c < 4 else pT[:, c - 4, t_ * 128:(t_ + 1) * 128]
                        tl = P.op("pe", (lambda lhs, c, half, bk2: lambda e: e.matmul(
                            banks[bk2][:], lhsT=lhs, rhs=Wo[:, c, half * 512:(half + 1) * 512], start=(c == 0), stop=(c == 7)))(lhs, c, half, bk2),
                            ba.rd() + buf("pT").rd() + buf("W").rd() + bankB[bk2].wr(), inc=(c == 7))
                    bankB[bk2].did_w(tl)
                    t = P.op("dve", (lambda t_, half, bk2, i: lambda e: e.tensor_tensor(
                        out=h1[:, t_, half * 512:(half + 1) * 512], in0=banks[bk2][:], in1=xtb[i][:, half * 512:(half + 1) * 512], op=ALU.add))(t_, half, bk2, i),
                        bankB[bk2].rd() + bx.rd() + buf("X1").wr(), dr=False)
                    bankB[bk2].did_r(t)
                    bx.did_r(t)
                ba.did_r(tl)
                buf("h1").did_w(t)
            buf("W").did_r(tl)
            buf("X3").did_r(tl)

        def load_weights(ex):
            i = ex % 2
            bw = buf(f"we{i}")
            dW = bw.wr() + buf("W").wr() + buf("xt0").wr() + buf("xt1").wr() + buf("xt2").wr() + buf("xs0").wr() + buf("xs1").wr() + buf("xs2").wr()
            dW = dW + list(pc_last.values())
            tw = P.dma("pool", (lambda ex, i: lambda e: e.dma_start(out=wg_flat[i], in_=Wg_bf[ex]))(ex, i), f"ld_we{i}", dW)
            tw = P.dma("pool", (lambda ex, i: lambda e: e.dma_start(out=wu_flat[i], in_=Wu_bf[ex]))(ex, i), f"ld_we{i}", dW)
            tw = P.dma("pool", (lambda ex, i: lambda e: e.dma_start(out=wd_flat[i], in_=Wd_bf[ex]))(ex, i), f"ld_we{i}", dW)
            bw.did_w(tw)

        if stage >= 5:
            if stage >= 6 and buf("zf").w is None:
                zero_fill()
                precast_weights(0, 32, [])
            if stage >= 6:
                load_weights(0)
                load_weights(1)
            t = P.op("dve", lambda e: e.memset(onesb[:], 1.0))
            buf("onesb").did_w(t)

            def prepA(t_):
                col = t_ % 4
                t1 = P.op("act", (lambda t_, col: lambda e: e.activation(out=junk[:], in_=h1[:, t_, :], func=AF.Square, accum_out=ss_t[:, col:col + 1]))(t_, col),
                          buf("h1").rd() + buf("junk").wr() + buf(f"ss{col}").wr(), dr=False)
                buf("junk").did_w(t1)
                t2 = P.op("act", (lambda col: lambda e: e.activation(out=rs_t[:, col:col + 1], in_=ss_t[:, col:col + 1], func=AF.Sqrt,
                                                                     bias=epsb[:, 0:1], scale=1.0 / DM))(col), buf(f"rs{col}").wr())
                t3 = P.op("dve", (lambda col: lambda e: e.reciprocal(out=rs_t[:, col:col + 1], in_=rs_t[:, col:col + 1]))(col), [t2], dr=False)
                buf(f"ss{col}").did_r(t2)
                t4 = P.op("dve", (lambda t_, col: lambda e: e.scalar_tensor_tensor(
                    out=xnAll[:, t_, :], in0=h1[:, t_, :], scalar=rs_t[:, col:col + 1], in1=gvb[:, 1, :], op0=ALU.mult, op1=ALU.mult))(t_, col),
                    buf("gv1").rd() + buf("X3").wr())
                buf(f"ss{col}").did_w(t1)
                buf(f"rs{col}").did_w(t3)
                buf(f"rs{col}").did_r(t4)
                buf(f"xn{t_}").did_w(t4)

            def prepB(t_):
                i = t_ % 2
                bn = buf(f"xn{t_}")
                bk = 0 + i
                pv = banks[bk][:].bitcast(BF16).rearrange("p (c t) -> p c t", t=128)
                tl = None
                for c in range(8):
                    tl = P.op("pe", (lambda c, pv, t_: lambda e: e.transpose(out=pv[:, c, :], in_=xnAll[:, t_, c * 128:(c + 1) * 128], identity=idb[:]))(c, pv, t_),
                              bn.rd() + bankB[bk].wr() + buf("idb").rd(), inc=(c == 7))
                bankB[bk].did_w(tl)
                bt = buf(f"xnT2{i}")
                ta = P.op("act", (lambda pv, i: lambda e: e.copy(out=xnT2[i], in_=pv))(pv, i),
                          bankB[bk].rd() + buf("X1").wr() + bt.wr(), dr=False)
                bankB[bk].did_r(ta)
                bt.did_w(ta)
                bk = 4 + i
                tl = None
                for c in range(8):
                    tl = P.op("pe", (lambda c, bk, i: lambda e: e.matmul(banks[bk][:, 0:36], lhsT=xnT2[i][:, c, :], rhs=wrtb[:, c, :],
                                                                         start=(c == 0), stop=(c == 7)))(c, bk, i),
                              bt.rd() + buf("wrtb").rd() + bankB[bk].wr(), inc=(c == 7))
                bt.did_r(tl)
                bankB[bk].did_w(tl)
                t = P.op("dve", (lambda bk, t_: lambda e: e.tensor_tensor(out=L_all[:, t_, :], in0=banks[bk][:, 0:36], in1=rbb[:], op=ALU.add))(bk, t_),
                         bankB[bk].rd() + buf("rbb").rd(), dr=False)
                bankB[bk].did_r(t)
                return t

            prepA(0)
            prepA(1)
            tL = None
            for t_ in range(16):
                if t_ + 2 < 16:
                    prepA(t_ + 2)
                tL = prepB(t_)

            def bc(v, n):
                return v.unsqueeze(2).to_broadcast([128, 16, n])

            D = lambda fn, deps=(): P.op("dve", fn, deps)
            GL = L_all[:, :, 0:4]
            D(lambda e: e.tensor_reduce(out=V_gmax, in_=GL, axis=AX.X, op=ALU.max), [tL])
            D(lambda e: e.tensor_tensor(out=gmask, in0=GL, in1=bc(V_gmax, 4), op=ALU.is_equal))
            t = D(lambda e: e.tensor_tensor(out=gdiff, in0=GL, in1=bc(V_gmax, 4), op=ALU.subtract))
            t = P.op("act", lambda e: e.activation(out=gdiff, in_=gdiff, func=AF.Exp), [t])
            D(lambda e: e.tensor_reduce(out=V_gsum, in_=gdiff, axis=AX.X, op=ALU.add), [t])
            D(lambda e: e.tensor_tensor(out=esel, in0=L_all[:, :, 4:12], in1=bc(gmask[:, :, 0], 8), op=ALU.mult))
            for g in range(1, 4):
                D((lambda g: lambda e: e.tensor_tensor(out=e2, in0=L_all[:, :, 4 + 8 * g:12 + 8 * g], in1=bc(gmask[:, :, g], 8), op=ALU.mult))(g))
                D(lambda e: e.tensor_tensor(out=esel, in0=esel, in1=e2, op=ALU.add))
            D(lambda e: e.tensor_reduce(out=V_m1, in_=esel, axis=AX.X, op=ALU.max))
            D(lambda e: e.tensor_tensor(out=mask1, in0=esel, in1=bc(V_m1, 8), op=ALU.is_equal))
            D(lambda e: e.scalar_tensor_tensor(out=e2, in0=mask1, scalar=-1.0e30, in1=esel, op0=ALU.mult, op1=ALU.add))
            D(lambda e: e.tensor_reduce(out=V_m2, in_=e2, axis=AX.X, op=ALU.max))
            D(lambda e: e.tensor_tensor(out=mask2, in0=e2, in1=bc(V_m2, 8), op=ALU.is_equal))
            t = D(lambda e: e.tensor_tensor(out=V_ex, in0=V_m2, in1=V_m1, op=ALU.subtract))
            t = P.op("act", lambda e: e.activation(out=V_ex, in_=V_ex, func=AF.Exp), [t])
            D(lambda e: e.scalar_tensor_tensor(out=V_den, in0=V_ex, scalar=1.0, in1=V_gsum, op0=ALU.add, op1=ALU.mult), [t])
            D(lambda e: e.reciprocal(out=gatesAll[:, :, 0], in_=V_den))
            D(lambda e: e.tensor_tensor(out=gatesAll[:, :, 1], in0=gatesAll[:, :, 0], in1=V_ex, op=ALU.mult))
            D(lambda e: e.tensor_tensor(out=msum, in0=mask1, in1=mask2, op=ALU.add))
            tmb = None
            for g in range(4):
                tmb = D((lambda g: lambda e: e.tensor_tensor(out=Mb_all[:, :, 8 * g:8 * g + 8], in0=msum, in1=bc(gmask[:, :, g], 8), op=ALU.mult))(g))
            prk = banks[2][:].rearrange("p (t e) -> p t e", e=32)
            tr_ = None
            for t_ in range(16):
                for tp_ in range(t_):
                    P.op("pe", (lambda t_, tp_: lambda e: e.matmul(prk[:, t_, :], lhsT=onesb[:], rhs=Mb_all[:, tp_, :], start=(tp_ == 0), stop=False))(t_, tp_),
                         [tmb] + buf("onesb").rd() + bankB[2].wr(), inc=False)
                tr_ = P.op("pe", (lambda t_: lambda e: e.matmul(prk[:, t_, :], lhsT=ltri[:], rhs=Mb_all[:, t_, :], start=(t_ == 0), stop=True))(t_),
                           [tmb] + buf("ltri").rd() + bankB[2].wr())
            bankB[2].did_w(tr_)
            D(lambda e: e.tensor_tensor(out=tmp32, in0=prk, in1=eoff[:].rearrange("p (t e) -> p t e", e=32), op=ALU.add), [tr_] + buf("eoff").rd())
            bankB[2].did_r(("dve", P.cnt["dve"]))
            D(lambda e: e.tensor_tensor(out=Rr, in0=tmp32[:, :, 0:8], in1=bc(gmask[:, :, 0], 8), op=ALU.mult))
            for g in range(1, 4):
                D((lambda g: lambda e: e.tensor_tensor(out=e2, in0=tmp32[:, :, 8 * g:8 * g + 8], in1=bc(gmask[:, :, g], 8), op=ALU.mult))(g))
                D(lambda e: e.tensor_tensor(out=Rr, in0=Rr, in1=e2, op=ALU.add))
            D(lambda e: e.tensor_tensor(out=e2, in0=Rr, in1=mask1, op=ALU.mult))
            D(lambda e: e.tensor_reduce(out=slf[:, :, 0], in_=e2, axis=AX.X, op=ALU.add))
            D(lambda e: e.tensor_tensor(out=e2, in0=Rr, in1=mask2, op=ALU.mult))
            D(lambda e: e.tensor_reduce(out=slf[:, :, 1], in_=e2, axis=AX.X, op=ALU.add))
            ti = D(lambda e: e.tensor_copy(out=idxAll[:], in_=slf))
            buf("idx").did_w(ti)
            sc_toks = []
            if stage >= 6:
                for t_ in range(16):
                    i = t_ % 2
                    tsc = None
                    for k_ in range(2):
                        tsc = P.dma("pool", (lambda t_, k_: lambda e: e.indirect_dma_start(
                            out=Xbuf[:, :], out_offset=bass.IndirectOffsetOnAxis(ap=idxAll[:, t_, k_:k_ + 1], axis=0),
                            in_=xnAll[:, t_, :], in_offset=None, bounds_check=None, oob_is_err=False))(t_, k_),
                            f"sc_x{i}", [ti] + buf("zf").rd())
                    sc_toks = [x for x in sc_toks if x[0] != f"sc_x{i}"] + [tsc]
            buf("X3").did_r(("pe", P.cnt["pe"]))
            for tk in sc_toks:
                buf("X3").did_r(tk)

        if stage >= 6:
            y_toks = {}

            def load_xe(ex):
                i = ex % 2
                bxe = buf(f"xe{i}")
                tld = P.dma("sp", (lambda ex, i: lambda e: e.dma_start(
                    out=xe[i], in_=Xbuf[ex * CAP:(ex + 1) * CAP, :].rearrange("(s p) d -> p s d", p=128)))(ex, i),
                    f"ld_xe{i}", sc_toks + bxe.wr() + buf("X3").wr())
                bxe.did_w(tld)

            def partA(ex):
                i = ex % 2
                bxe, bxt = buf(f"xe{i}"), buf(f"XeT{i}")
                tl = tcp = None
                for st_ in range(2):
                    bk = st_
                    pv = banks[bk][:].bitcast(BF16).rearrange("p (c t) -> p c t", t=128)
                    for c in range(8):
                        tl = P.op("pe", (lambda c, pv, i, st_: lambda e: e.transpose(out=pv[:, c, :], in_=xe[i][:, st_, c * 128:(c + 1) * 128], identity=idb[:]))(c, pv, i, st_),
                                  bxe.rd() + bankB[bk].wr(), inc=(c == 7))
                    bankB[bk].did_w(tl)
                    tcp = P.op("act", (lambda pv, i, st_: lambda e: e.copy(out=XeT[i][:, :, st_ * 128:(st_ + 1) * 128], in_=pv))(pv, i, st_),
                               bankB[bk].rd() + bxt.wr(), dr=False)
                    bankB[bk].did_r(tcp)
                bxe.did_r(tl)
                bxt.did_w(tcp)

            def gateup(ex):
                i = ex % 2
                bw = buf(f"we{i}")
                bxt, bh = buf(f"XeT{i}"), buf(f"HT{i}")
                tu = tm = None
                for fc in range(4):
                    bg, bu = (2, 3) if fc % 2 == 0 else (4, 5)
                    tg = None
                    for c in range(8):
                        tg = P.op("pe", (lambda c, fc, i, bg: lambda e: e.matmul(
                            banks[bg][:, 0:CAP], lhsT=wgb[i][:, c, fc * 128:(fc + 1) * 128], rhs=XeT[i][:, c, :],
                            start=(c == 0), stop=(c == 7)))(c, fc, i, bg),
                            bw.rd() + bxt.rd() + bankB[bg].wr(), inc=(c == 7))
                    bankB[bg].did_w(tg)
                    for c in range(8):
                        tu = P.op("pe", (lambda c, fc, i, bu: lambda e: e.matmul(
                            banks[bu][:, 0:CAP], lhsT=wub[i][:, c, fc * 128:(fc + 1) * 128], rhs=XeT[i][:, c, :],
                            start=(c == 0), stop=(c == 7)))(c, fc, i, bu),
                            bankB[bu].wr(), inc=(c == 7))
                    bankB[bu].did_w(tu)
                    sgi = fc % 2
                    ts = P.op("act", (lambda bg, sgi: lambda e: e.activation(out=tmpA[:, sgi, 0:CAP], in_=banks[bg][:, 0:CAP], func=AF.Silu))(bg, sgi),
                              bankB[bg].rd() + buf(f"sg{sgi}").wr(), dr=False)
                    bankB[bg].did_r(ts)
                    buf(f"sg{sgi}").did_w(ts)
                    tm = P.op("dve", (lambda i, fc, sgi, bu: lambda e: e.tensor_tensor(out=HT[i][:, fc, :], in0=banks[bu][:, 0:CAP], in1=tmpA[:, sgi, 0:CAP],
                                                                                       op=ALU.mult))(i, fc, sgi, bu),
                              bankB[bu].rd() + buf(f"sg{sgi}").rd() + bh.wr(), dr=False)
                    bankB[bu].did_r(tm)
                    buf(f"sg{sgi}").did_r(tm)
                bxt.did_r(tu)
                bh.did_w(tm)

            def down(ex):
                i = ex % 2
                bw, bh = buf(f"we{i}"), buf(f"HT{i}")
                tl = None
                for st_ in range(2):
                    yi = st_
                    by = buf(f"ye{yi}")
                    tcy_a = tcy = None
                    for half in range(2):
                        bk = 6 + half
                        for fc in range(4):
                            tl = P.op("pe", (lambda fc, st_, half, i, bk: lambda e: e.matmul(
                                banks[bk][:], lhsT=HT[i][:, fc, st_ * 128:(st_ + 1) * 128], rhs=wdb[i][:, fc, half * 512:(half + 1) * 512],
                                start=(fc == 0), stop=(fc == 3)))(fc, st_, half, i, bk),
                                bh.rd() + bw.rd() + bankB[bk].wr(), inc=(fc == 3))
                        bankB[bk].did_w(tl)
                        if half == 0:
                            tcy = P.op("act", (lambda yi, bk: lambda e: e.copy(out=ye[yi][:, 0:512], in_=banks[bk][:]))(yi, bk),
                                       bankB[bk].rd() + by.wr(), dr=False)
                            tcy_a = tcy
                        else:
                            tcy = P.op("dve", (lambda yi, bk: lambda e: e.tensor_copy(out=ye[yi][:, 512:1024], in_=banks[bk][:]))(yi, bk),
                                       bankB[bk].rd() + by.wr(), dr=False)
                        bankB[bk].did_r(tcy)
                    tst = P.dma("sp", (lambda ex, st_, yi: lambda e: e.dma_start(
                        out=Ybuf[ex * CAP + st_ * 128: ex * CAP + (st_ + 1) * 128, :], in_=ye[yi]))(ex, st_, yi),
                        f"st_ye{yi}", [tcy_a, tcy])
                    by.did_w(tcy)
                    by.did_r(tcy_a)
                    by.did_r(tst)
                    y_toks[yi] = tst
                bh.did_r(tl)
                bw.did_r(tl)

            load_xe(0)
            load_xe(1)
            partA(0)
            for ex in range(32):
                gateup(ex)
                if ex + 1 < 32:
                    partA(ex + 1)
                if ex + 2 < 32:
                    load_xe(ex + 2)
                down(ex)
                if ex + 2 < 32:
                    load_weights(ex + 2)
            allw = [("pe", P.cnt["pe"])] + buf("we0").wr() + buf("we1").wr()
            for t_ in range(16):
                gs = []
                for k_ in range(2):
                    gi = (2 * t_ + k_) % 8
                    bgb = buf(f"gb{gi}")
                    tg_ = P.dma("pool", (lambda t_, k_, gi: lambda e: e.indirect_dma_start(
                        out=gb[gi], out_offset=None, in_=Ybuf[:, :],
                        in_offset=bass.IndirectOffsetOnAxis(ap=idxAll[:, t_, k_:k_ + 1], axis=0),
                        bounds_check=None, oob_is_err=False))(t_, k_, gi),
                        f"ga{gi}", list(y_toks.values()) + bgb.wr() + allw + buf("idx").rd())
                    bgb.did_w(tg_)
                    gs.append((gi, bgb))
                for k_, (gi, bgb) in enumerate(gs):
                    ta = P.op("dve", (lambda t_, k_, gi: lambda e: e.scalar_tensor_tensor(
                        out=h1[:, t_, :], in0=gb[gi], scalar=gatesAll[:, t_, k_:k_ + 1], in1=h1[:, t_, :],
                        op0=ALU.mult, op1=ALU.add))(t_, k_, gi), bgb.rd() + buf("h1").rd())
                    bgb.did_r(ta)
            buf("h1").did_w(ta)

        if stage >= 4:
            for t_ in range(16):
                col = t_ % 4
                i = t_ % 2
                t1 = P.op("act", (lambda t_, col: lambda e: e.activation(out=junk[:], in_=h1[:, t_, :], func=AF.Square, accum_out=ss_t[:, col:col + 1]))(t_, col),
                          buf("h1").rd() + buf("junk").wr() + buf(f"ss{col}").wr(), dr=False)
                buf("junk").did_w(t1)
                t2 = P.op("act", (lambda col: lambda e: e.activation(out=rs_t[:, col:col + 1], in_=ss_t[:, col:col + 1], func=AF.Sqrt,
                                                                     bias=epsb[:, 0:1], scale=1.0 / DM))(col), buf(f"rs{col}").wr())
                t3 = P.op("dve", (lambda col: lambda e: e.reciprocal(out=rs_t[:, col:col + 1], in_=rs_t[:, col:col + 1]))(col), [t2], dr=False)
                buf(f"ss{col}").did_r(t2)
                t4 = P.op("dve", (lambda t_, col: lambda e: e.scalar_tensor_tensor(
                    out=h1[:, t_, :], in0=h1[:, t_, :], scalar=rs_t[:, col:col + 1], in1=gvb[:, 2, :], op0=ALU.mult, op1=ALU.mult))(t_, col),
                    buf("gv2").rd() + [t1])
                buf(f"ss{col}").did_w(t1)
                buf(f"rs{col}").did_w(t3)
                buf(f"rs{col}").did_r(t4)
                to = P.dma("sp", (lambda t_: lambda e: e.dma_start(out=out[t_ * 128:(t_ + 1) * 128, :], in_=h1[:, t_, :]))(t_), "st_out", [t4])
            P.wait("sp", [to])

        if dbg:
            deps_all = [(k, P.cnt[k]) for k in ("pe", "act", "dve")]
            def dump(name, ap_sb, shape, dt):
                d = nc.dram_tensor(name, list(shape), dt, kind="ExternalOutput").ap()
                tk = P.dma("sp", lambda e: e.dma_start(out=d, in_=ap_sb), "st_dbg", deps_all)
                P.wait("sp", [tk])
            if stage == 1:
                dump("d_pT", pT, [128, 4, NQ], BF16)
                dump("d_UT", UT, [128, 4, 2064], F32)
            if stage == 2:
                dump("d_KT", KT, [128, 2, S], BF16)
                dump("d_QT", QT, [128, 2, NQ], BF16)
                dump("d_VA", X1[:, 16384:16384 + 16640], [128, 16640], BF16)
            if stage == 3:
                dump("d_a", a_tok, [128, 16, 512], BF16)
            if stage == 5:
                dump("d_G", gatesAll[:], [128, 16, 2], F32)
                dump("d_idx", idxAll[:], [128, 16, 2], I32)
        P.emit()
    return nc


def host_consts(qs):
    T0 = NQ * qs
    r = np.arange(S)
    wrapped = (r + T0) >= S
    krel = np.where(wrapped, r - S, r).astype(np.float64)
    sig_tile = np.where(wrapped.reshape(NT, 128)[:, 0], -1.0, 1.0)
    kb = np.zeros((128, 3, 4, 64), np.float32)
    csg = np.zeros((2, 4, 66), np.float32)
    for h in range(4):
        sl = SLOPES[h]
        for m in range(64):
            kpos = 128 * m + np.arange(128)
            kb[:, 0, h, m] = sl * kpos
            kb[:, 1, h, m] = -sl * kpos
            kb[:, 2, h, m] = -sig_tile[m] * sl * krel[128 * m:128 * m + 128]
            csg[:, h, m] = 8 * sl * sig_tile[m]
        csg[:, h, 64] = 8 * sl
        csg[:, h, 65] = -8 * sl
    qpos = np.stack([128.0 * (np.arange(NQ) // 128), (np.arange(NQ) % 128).astype(np.float64)]).astype(np.float32)
    dband = np.zeros((128, 2, 4, 512), np.float32)
    kk = np.arange(128)[:, None]
    qq = np.arange(512)[None, :]
    for i in range(4):
        v = np.abs(qq - kk - 128 * i)
        dband[:, 0, i, :] = 256 * (v // 256)
        dband[:, 1, i, :] = v % 256
    sid = np.zeros((128, 4, 128), np.float32)
    for h in range(4):
        sid[:, h, :] = -8 * SLOPES[h] * np.eye(128)
    hv = np.zeros((128, 16), np.float32)
    hv[:, 0:8] = 1.0 if T0 > 0 else 0.0
    hv[:, 8:16] = 1.0 if T0 + NQ < S else 0.0
    rcorr = np.zeros((128, 4, 16), np.float32)
    for g, w in enumerate(WINS):
        for k_, rr in enumerate(list(range(8)) + list(range(NQ - 8, NQ))):
            t = T0 + rr
            lo = max(t - w // 2, 0)
            hi = min(t + w // 2, S)
            rcorr[:, g, k_] = 1.0 / (hi - lo)
    ltri = np.triu(np.ones((128, 128), np.float32), k=1)
    eoff = np.broadcast_to(np.tile((np.arange(32) * CAP).astype(np.float32), 16)[None, :], (128, 512)).copy()
    return dict(ltri=ltri, eoff=eoff, kb=kb.reshape(128, -1), csg=csg.reshape(2, -1), qpos=qpos, dband=dband.reshape(128, -1),
                sid=sid.reshape(128, -1), hv=hv, rcorr=rcorr.reshape(128, -1))


def make_in_maps(inputs, cores=range(8)):
    f = lambda a: np.ascontiguousarray(np.asarray(a, dtype=np.float32))
    x = f(inputs["x"])
    shared = dict(
        w_in=f(inputs["w_in"])[0], w_out=f(inputs["w_out"])[0], w_pool=f(inputs["w_pool"])[0],
        w_gate=f(inputs["w_gate"])[0], w_up=f(inputs["w_up"])[0], w_down=f(inputs["w_down"])[0],
        wr=np.ascontiguousarray(np.concatenate([f(inputs["w_group_router"])[0], f(inputs["w_expert_router"])[0].reshape(DM, 32)], axis=1)),
        vecs=np.ascontiguousarray(np.stack([f(inputs["norm1_g"])[0], f(inputs["norm2_g"])[0], f(inputs["final_g"])])),
        rbias=np.ascontiguousarray(np.concatenate([f(inputs["b_group_router"])[0], f(inputs["b_expert_router"])[0].reshape(32)])),
        lams=np.ascontiguousarray(np.stack([f(inputs["lambda_q1"])[0], f(inputs["lambda_k1"])[0], f(inputs["lambda_q2"])[0], f(inputs["lambda_k2"])[0]])),
        subg=f(inputs["subln_g"])[0],
        pscale=np.ascontiguousarray(f(inputs["pool_scale"])[0].reshape(4, 128).T),
        ident=np.eye(128, dtype=np.float32),
    )
    maps = []
    for c in cores:
        b, qs = c // 4, c % 4
        m = dict(shared)
        m["xr"] = np.ascontiguousarray(np.roll(x[b], -NQ * qs, axis=0))
        m.update(host_consts(qs))
        maps.append(m)
    return maps


_NC = None


def kernel(**inputs):
    global _NC
    if _NC is None:
        _NC = build()
    in_maps = make_in_maps(inputs)
    res = run_bass_kernel_spmd(_NC, in_maps, core_ids=list(range(8)))
    out = np.zeros((2, S, DM), np.float32)
    for c in range(8):
        b, qs = c // 4, c % 4
        out[b, NQ * qs:NQ * (qs + 1)] = res.results[c]["out"]
    return out
```

```python
import contextlib
import numpy as np
import concourse.bass as bass
import concourse.mybir as mybir
from concourse.bass_utils import run_bass_kernel_spmd

F32 = mybir.dt.float32
BF16 = mybir.dt.bfloat16
AF = mybir.ActivationFunctionType
ALU = mybir.AluOpType
AX = mybir.AxisListType

S = 8192
DM = 1024
NQ = 2048
NT = S // 128
EPS = 1e-6
SLOPES = [2.0 ** (-8.0 * (h + 1) / 4) for h in range(4)]
WINS = (2, 4, 8, 16)
ENGS = ("pe", "act", "dve", "pool", "sp")
CAP = 256
SKIP_T = 64.0
NR = 32 * CAP + 2048


class Prog:
    def __init__(self, nc):
        self.nc = nc
        self.ops = {e: [] for e in ENGS}
        self.cnt = {}
        self.sem_names = []
        for e in ENGS:
            self._newsem(e)

    def _newsem(self, key):
        self.cnt[key] = 0
        self.sem_names.append(key)

    def op(self, eng, fn, deps=(), inc=True, dr=None):
        d = tuple(x for x in deps if x is not None)
        tok = None
        if inc:
            self.cnt[eng] += 1
            tok = (eng, self.cnt[eng])
        if dr is None:
            dr = eng in ("dve", "act", "pool")
        self.ops[eng].append((fn, d, (eng, 1) if inc else None, dr))
        return tok

    def dma(self, queue, fn, semkey, deps=()):
        if semkey not in self.cnt:
            self._newsem(semkey)
        self.cnt[semkey] += 16
        tok = (semkey, self.cnt[semkey])
        d = tuple(x for x in deps if x is not None)
        self.ops[queue].append((fn, d, (semkey, 16), False))
        return tok

    def wait(self, eng, deps):
        d = tuple(x for x in deps if x is not None)
        self.ops[eng].append((None, d, None, False))

    def emit(self):
        nc = self.nc
        with contextlib.ExitStack() as st:
            sems = {k: st.enter_context(nc.semaphore("s_" + k)) for k in self.sem_names}
            block = st.enter_context(nc.Block())
            prog = self

            def run(engname, eng):
                waited = {}
                for fn, deps, inc, dr in prog.ops[engname]:
                    for (k, v) in deps:
                        if k == engname:
                            continue
                        if waited.get(k, 0) < v:
                            eng.wait_ge(sems[k], v)
                            waited[k] = v
                    if fn is None:
                        continue
                    if dr:
                        eng.drain()
                    ins = fn(eng)
                    if inc is not None:
                        ins.then_inc(sems[inc[0]], inc[1])

            @block.tensor
            def _(e):
                run("pe", e)

            @block.scalar
            def _(e):
                run("act", e)

            @block.vector
            def _(e):
                run("dve", e)

            @block.gpsimd
            def _(e):
                run("pool", e)

            @block.sync
            def _(e):
                run("sp", e)


class Buf:
    def __init__(self):
        self.w = None
        self.rs = {}

    def rd(self):
        return [self.w]

    def wr(self):
        return [self.w] + list(self.rs.values())

    def did_r(self, tok):
        if tok is None:
            return
        k, v = tok
        if k not in self.rs or self.rs[k][1] < v:
            self.rs[k] = tok

    def did_w(self, tok):
        if tok is None:
            return
        self.w = tok
        self.rs = {}


def build(stage=99, dbg=False, noattn=False):
    nc = bass.Bass("TRN2", target_bir_lowering=False)

    def DI(name, shape, dt=F32):
        return nc.dram_tensor(name, list(shape), dt, kind="ExternalInput").ap()

    xr = DI("xr", [S, DM])
    w_in = DI("w_in", [DM, 2048])
    w_out = DI("w_out", [DM, DM])
    w_pool = DI("w_pool", [4, 128, 128])
    w_gate = DI("w_gate", [32, DM, 512])
    w_up = DI("w_up", [32, DM, 512])
    w_down = DI("w_down", [32, 512, DM])
    wr_d = DI("wr", [DM, 36])
    vecs = DI("vecs", [3, DM])
    rbias = DI("rbias", [36])
    lams = DI("lams", [4, 64])
    subg = DI("subg", [128])
    pscale = DI("pscale", [128, 4])
    ident = DI("ident", [128, 128])
    kb_d = DI("kb", [128, 3 * 4 * 64])
    csg_d = DI("csg", [2, 4 * 66])
    qpos_d = DI("qpos", [2, NQ])
    dband_d = DI("dband", [128, 2 * 4 * 512])
    sid_d = DI("sid", [128, 4 * 128])
    hv_d = DI("hv", [128, 16])
    rcorr_d = DI("rcorr", [128, 4 * 16])
    ltri_d = DI("ltri", [128, 128])
    eoff_d = DI("eoff", [128, 512])
    out = nc.dram_tensor("out", [NQ, DM], F32, kind="ExternalOutput").ap()
    Xbuf = nc.dram_tensor("Xbuf", [NR, DM], BF16).ap()
    Ybuf = nc.dram_tensor("Ybuf", [NR, DM], F32).ap()
    XTs = nc.dram_tensor("XTs", [16, 128, 4096], BF16).ap()
    Wg_bf = nc.dram_tensor("Wg_bf", [32, DM, 512], BF16).ap()
    Wu_bf = nc.dram_tensor("Wu_bf", [32, DM, 512], BF16).ap()
    Wd_bf = nc.dram_tensor("Wd_bf", [32, 512, DM], BF16).ap()
    dbg_outs = {}

    with contextlib.ExitStack() as st:
        def sb(name, shape, dt):
            return st.enter_context(nc.sbuf_tensor("sb_" + name, list(shape), dt))

        def psb(name, shape, dt):
            return st.enter_context(nc.psum_tensor("ps_" + name, list(shape), dt))

        P = Prog(nc)

        X1 = sb("X1", [128, 37632], BF16)
        X2 = sb("X2", [128, 24576], BF16)
        X3 = sb("X3", [128, 16384], BF16)
        kb = sb("kb", [128, 3, 4, 64], F32)
        csg = sb("csg", [34, 4, 66], BF16)
        qpos = sb("qpos", [34, NQ], BF16)
        dband = sb("dband", [128, 2, 4, 512], BF16)
        sid = sb("sid", [128, 4, 128], BF16)
        idb = sb("idb", [128, 128], BF16)
        hv = sb("hv", [128, 16], F32)
        rcorr = sb("rcorr", [128, 4, 16], F32)
        gvb = sb("gvb", [128, 3, DM], F32)
        rbb = sb("rbb", [128, 36], F32)
        lamb = sb("lamb", [128, 4, 64], F32)
        sg8 = sb("sg8", [128, 128], F32)
        psc = sb("psc", [128, 4], F32)
        wpb = sb("wpb", [128, 4, 128], BF16)
        wrtb = sb("wrtb", [128, 8, 36], BF16)
        epsb = sb("epsb", [128, 1], F32)
        small = sb("small", [128, 64], F32)
        junk = sb("junk", [128, DM], BF16)
        PTb = [sb(f"PT{i}", [128, 512], BF16) for i in range(4)]
        I32 = mybir.dt.int32
        ltri = sb("ltri", [128, 128], BF16)
        onesb = sb("onesb", [128, 128], BF16)
        eoff = sb("eoff", [128, 512], F32)
        Mb_all = sb("Mb_all", [128, 16, 32], BF16)
        idxAll = sb("idxAll", [128, 16, 2], I32)
        gatesAll = sb("gatesAll", [128, 16, 2], F32)
        zt = sb("zt", [128, 1024], BF16)
        RW = sb("RW", [128, 2304], F32)
        tmpA = RW[:, 0:1024].rearrange("p (a b) -> p a b", b=512)
        accS = RW[:, 1024:2304].rearrange("p (b c) -> p b c", c=320)
        L_all = RW[:, 0:576].rearrange("p (t k) -> p t k", k=36)
        gmask = RW[:, 576:640].rearrange("p (t k) -> p t k", k=4)
        gdiff = RW[:, 640:704].rearrange("p (t k) -> p t k", k=4)
        esel = RW[:, 704:832].rearrange("p (t k) -> p t k", k=8)
        mask1 = RW[:, 832:960].rearrange("p (t k) -> p t k", k=8)
        e2 = RW[:, 960:1088].rearrange("p (t k) -> p t k", k=8)
        mask2 = RW[:, 1088:1216].rearrange("p (t k) -> p t k", k=8)
        msum = RW[:, 1216:1344].rearrange("p (t k) -> p t k", k=8)
        Rr = RW[:, 1344:1472].rearrange("p (t k) -> p t k", k=8)
        tmp32 = RW[:, 1472:1984].rearrange("p (t k) -> p t k", k=32)
        V_gmax, V_gsum, V_m1, V_m2, V_ex, V_den = [RW[:, 1984 + 16 * q: 2000 + 16 * q] for q in range(6)]
        slf = RW[:, 2080:2112].rearrange("p (t k) -> p t k", k=2)
        ss_t = sb("ss_t", [128, 4], F32)
        rs_t = sb("rs_t", [128, 4], F32)

        banks = [psb(f"bank{i}", [128, 512], F32) for i in range(8)]
        bankB = [Buf() for _ in range(8)]

        a_tok = X3[:, 0:8192].rearrange("p (t c) -> p t c", c=512)
        pT = X3[:, 8192:16384].rearrange("p (g t) -> p g t", t=NQ)
        xn2T = X3[:, :].rearrange("p (c t) -> p c t", t=NQ)
        KT = X1[:, 0:16384].rearrange("p (h t) -> p h t", t=S)
        VA = X1[:, 16384:16384 + 64 * 2 * 130].rearrange("p (m h e) -> p m h e", h=2, e=130)
        QT = X1[:, 33024:33024 + 4096].rearrange("p (h t) -> p h t", t=NQ)
        UT = X1[:, 0:4 * 2064 * 2].bitcast(F32).rearrange("p (g t) -> p g t", t=2064)
        Sa = X1[:, 16512:16512 + 4128].bitcast(F32)
        Sb = X1[:, 20640:20640 + 4128].bitcast(F32)
        dpool = X1[:, 24768:24768 + NQ]
        h1 = X1[:, 0:32768].bitcast(F32).rearrange("p (t d) -> p t d", d=DM)
        Wk2 = X2[:, 0:2048].rearrange("p (c n) -> p c n", n=256)
        Wv2 = X2[:, 2048:4096].rearrange("p (c n) -> p c n", n=256)
        Wq2 = X2[:, 4096:6144].rearrange("p (c n) -> p c n", n=256)
        Wu4 = X2[:, 0:4096].rearrange("p (c n) -> p c n", n=512)
        xtb = [X2[:, 6144 + i * 2048: 6144 + (i + 1) * 2048].bitcast(F32) for i in range(3)]
        xsb = [X2[:, 12288 + i * 1024: 12288 + (i + 1) * 1024] for i in range(3)]
        xTc = [X2[:, 15360 + i * 4096: 15360 + (i + 1) * 4096].rearrange("p (c t) -> p c t", t=512) for i in range(2)]
        xTc_flat = [X2[:, 15360 + i * 4096: 15360 + (i + 1) * 4096] for i in range(2)]
        Wo = X2[:, 12288:20480].rearrange("p (c n) -> p c n", n=DM)
        aTt = [X2[:, 20480 + i * 512: 20480 + (i + 1) * 512].rearrange("p (c t) -> p c t", t=128) for i in range(2)]
        xnAll = X3[:, :].rearrange("p (t d) -> p t d", d=DM)
        xnT2 = [X1[:, 32768 + i * 1024: 32768 + (i + 1) * 1024].rearrange("p (c t) -> p c t", t=128) for i in range(2)]
        XeT = [X3[:, 2048 + i * 2048: 2048 + (i + 1) * 2048].rearrange("p (c t) -> p c t", t=256) for i in range(2)]
        xe = [X3[:, 6144 + i * 2048: 6144 + (i + 1) * 2048].rearrange("p (s d) -> p s d", d=DM) for i in range(2)]
        ye = [X3[:, 10240 + i * 2048: 10240 + (i + 1) * 2048].bitcast(F32) for i in range(2)]
        HT = [X3[:, 14336 + i * 1024: 14336 + (i + 1) * 1024].rearrange("p (c t) -> p c t", t=256) for i in range(2)]
        gb = [X2[:, i * 2048:(i + 1) * 2048].bitcast(F32) for i in range(8)]
        wg_flat = [X2[:, i * 12288: i * 12288 + 4096] for i in range(2)]
        wu_flat = [X2[:, i * 12288 + 4096: i * 12288 + 8192] for i in range(2)]
        wgb = [X2[:, i * 12288: i * 12288 + 4096].rearrange("p (c f) -> p c f", f=512) for i in range(2)]
        wub = [X2[:, i * 12288 + 4096: i * 12288 + 8192].rearrange("p (c f) -> p c f", f=512) for i in range(2)]
        wdb = [X2[:, i * 12288 + 8192: i * 12288 + 12288].rearrange("p (c n) -> p c n", n=DM) for i in range(2)]
        HTb = [X1[:, 32768 + i * 2048: 32768 + (i + 1) * 2048].rearrange("p (c t) -> p c t", t=512) for i in range(2)]

        B = {}

        def buf(name):
            if name not in B:
                B[name] = Buf()
            return B[name]

        def ld(queue, dst, src, name):
            t = P.dma(queue, lambda e: e.dma_start(out=dst, in_=src), "ld_" + name)
            buf(name).did_w(t)
            return t

        ld("sp", kb[:].rearrange("p a b c -> p (a b c)"), kb_d[:, :], "kb")
        ld("pool", csg[0:2].rearrange("p a b -> p (a b)"), csg_d[:, :], "csg")
        ld("pool", qpos[0:2], qpos_d[:, :], "qpos")
        ld("pool", csg[32:34].rearrange("p a b -> p (a b)"), csg_d[:, :], "csg2")
        ld("pool", qpos[32:34], qpos_d[:, :], "qpos2")
        ld("pool", dband[:].rearrange("p a b c -> p (a b c)"), dband_d[:, :], "dband")
        ld("pool", sid[:].rearrange("p a b -> p (a b)"), sid_d[:, :], "sid")
        ld("pool", idb[:], ident[:, :], "idb")
        ld("sp", hv[:], hv_d[:, :], "hv")
        ld("sp", rcorr[:].rearrange("p a b -> p (a b)"), rcorr_d[:, :], "rcorr")
        for i in range(3):
            ld("sp", gvb[:, i, :], vecs[i, :].partition_broadcast(128), f"gv{i}")
        ld("sp", rbb[:], rbias.partition_broadcast(128), "rbb")
        ld("sp", lamb[:].rearrange("p a b -> p (a b)"), lams.rearrange("a b -> (a b)").partition_broadcast(128), "lamb")
        ld("sp", sg8[:], subg.partition_broadcast(128), "sg8")
        ld("sp", psc[:], pscale[:, :], "psc")
        ld("pool", wpb[:], w_pool.rearrange("g c d -> c g d"), "wpb")
        ld("pool", wrtb[:], wr_d.rearrange("(c p) n -> p c n", p=128), "wrtb")
        ld("pool", ltri[:], ltri_d[:, :], "ltri")
        ld("sp", eoff[:], eoff_d[:, :], "eoff")

        t = P.op("dve", lambda e: e.memset(epsb[:], EPS))
        buf("epsb").did_w(t)
        tz0 = P.op("dve", lambda e: e.memset(zt[:], 0.0))

        def zero_fill():
            tzf = None
            for r0 in range(0, NR, 128):
                tzf = P.dma("pool", (lambda r0: lambda e: e.dma_start(out=Xbuf[r0:r0 + 128, :], in_=zt[:]))(r0), "zf", [tz0])
            for r0 in range(32 * CAP, NR, 128):
                for h0 in (0, 512):
                    tzf = P.dma("pool", (lambda r0, h0: lambda e: e.dma_start(out=Ybuf[r0:r0 + 128, h0:h0 + 512], in_=zt[:].bitcast(F32)))(r0, h0), "zf", [tz0])
            buf("zf").did_w(tzf)

        pc_last = {}

        def precast_weights(lo, hi, deps):
            jobs = []
            for ex in range(lo, hi):
                for r0 in (0, 512):
                    jobs.append((Wg_bf[ex, r0:r0 + 512, :], w_gate[ex, r0:r0 + 512, :]))
                    jobs.append((Wu_bf[ex, r0:r0 + 512, :], w_up[ex, r0:r0 + 512, :]))
                for r0 in (0, 256):
                    jobs.append((Wd_bf[ex, r0:r0 + 256, :], w_down[ex, r0:r0 + 256, :]))
            for n_, (dst_, src_) in enumerate(jobs):
                key = f"pc{n_ % 2}"
                prev = [pc_last[key]] if key in pc_last else []
                pc_last[key] = P.dma("pool", (lambda dst_, src_: lambda e: e.dma_start(out=dst_, in_=src_))(dst_, src_), key, list(deps) + prev)
        t = P.op("dve", lambda e: e.tensor_scalar(out=sg8[:], in0=sg8[:], scalar1=0.8, scalar2=None, op0=ALU.mult),
                 buf("sg8").rd())
        buf("sg8").did_w(t)
        t = P.op("dve", lambda e: e.tensor_tensor(out=lamb[:, 0, :], in0=lamb[:, 0, :], in1=lamb[:, 1, :], op=ALU.mult),
                 buf("lamb").rd())
        t = P.op("dve", lambda e: e.tensor_tensor(out=lamb[:, 2, :], in0=lamb[:, 2, :], in1=lamb[:, 3, :], op=ALU.mult))
        t = P.op("dve", lambda e: e.reduce_sum(out=small[:, 1:2], in_=lamb[:, 0, :], axis=AX.X))
        t = P.op("dve", lambda e: e.reduce_sum(out=small[:, 2:3], in_=lamb[:, 2, :], axis=AX.X))
        t = P.op("act", lambda e: e.activation(out=small[:, 1:3], in_=small[:, 1:3], func=AF.Exp), [t])
        t = P.op("dve", lambda e: e.tensor_tensor(out=small[:, 0:1], in0=small[:, 2:3], in1=small[:, 1:2], op=ALU.subtract), [t])
        t = P.op("dve", lambda e: e.tensor_scalar(out=small[:, 0:1], in0=small[:, 0:1], scalar1=-0.2, scalar2=None, op0=ALU.add))
        buf("neglam").did_w(t)

        fe_cnt = [0]
        pTr_banks = [6, 7]

        def fe_A(m, gidx=0):
            i = fe_cnt[0] % 3
            ib = fe_cnt[0] % 2
            fe_cnt[0] += 1
            bx, bs = buf(f"xt{i}"), buf(f"xs{i}")
            tx = P.dma("sp", lambda e: e.dma_start(out=xtb[i], in_=xr[m * 128:(m + 1) * 128, :]), f"ld_xt{i}", bx.wr())
            bx.did_w(tx)
            col = (fe_cnt[0] % 4)
            t1 = P.op("act", lambda e: e.activation(out=junk[:], in_=xtb[i], func=AF.Square, accum_out=ss_t[:, col:col + 1]),
                      bx.rd() + buf("junk").wr() + buf(f"ss{col}").wr(), dr=False)
            buf("junk").did_w(t1)
            bx.did_r(t1)
            t2 = P.op("act", lambda e: e.activation(out=rs_t[:, col:col + 1], in_=ss_t[:, col:col + 1], func=AF.Sqrt,
                                                    bias=epsb[:, 0:1], scale=1.0 / DM), buf("epsb").rd() + buf(f"rs{col}").wr())
            t3 = P.op("dve", lambda e: e.reciprocal(out=rs_t[:, col:col + 1], in_=rs_t[:, col:col + 1]), [t2], dr=False)
            buf(f"ss{col}").did_r(t2)
            t4 = P.op("dve", lambda e: e.scalar_tensor_tensor(out=xsb[i], in0=xtb[i], scalar=rs_t[:, col:col + 1],
                                                               in1=gvb[:, gidx, :], op0=ALU.mult, op1=ALU.mult),
                      bx.rd() + bs.wr() + buf(f"gv{gidx}").rd())
            buf(f"ss{col}").did_w(t1)
            buf(f"rs{col}").did_w(t3)
            buf(f"rs{col}").did_r(t4)
            bx.did_r(t4)
            bs.did_w(t4)
            return (i, ib)

        def fe_B(ctx, dst, dstB):
            i, ib = ctx
            bs = buf(f"xs{i}")
            bk = pTr_banks[ib]
            pv = banks[bk][:].bitcast(BF16).rearrange("p (c t) -> p c t", t=128)
            tl = None
            for c in range(8):
                tl = P.op("pe", (lambda c: lambda e: e.transpose(out=pv[:, c, :], in_=xsb[i][:, c * 128:(c + 1) * 128],
                                                                 identity=idb[:]))(c),
                          bs.rd() + bankB[bk].wr() + buf("idb").rd(), inc=(c == 7))
            bs.did_r(tl)
            bankB[bk].did_w(tl)
            t5 = P.op("act", lambda e: e.copy(out=dst, in_=pv), bankB[bk].rd() + dstB.wr(), dr=False)
            bankB[bk].did_r(t5)
            dstB.did_w(t5)
            return t5

        def front_end(m, dst, dstB, gidx=0):
            return fe_B(fe_A(m, gidx), dst, dstB)

        if stage >= 1:
            tw = P.dma("pool", lambda e: e.dma_start(out=Wu4, in_=w_in[:, 1536:2048].rearrange("(c p) n -> p c n", p=128)), "ld_w0")
            buf("W").did_w(tw)
            tiles = [63] + list(range(16)) + [16]
            pk = 0
            ctxP = {0: fe_A(tiles[0]), 1: fe_A(tiles[1])}
            for idx, m in enumerate(tiles):
                i = idx % 2
                dst = xTc[i][:, :, 0:128]
                dB = buf(f"xTc{i}")
                if idx + 2 < len(tiles):
                    ctxP[idx + 2] = fe_A(tiles[idx + 2])
                t5p = fe_B(ctxP[idx], dst, dB)
                if 1 <= idx <= 16 and stage >= 2:
                    tsx = P.dma("pool", (lambda m, i: lambda e: e.dma_start(
                        out=XTs[m // 4].rearrange("p (c t) -> p c t", t=512)[:, :, (m % 4) * 128:(m % 4 + 1) * 128],
                        in_=xTc[i][:, :, 0:128]))(m, i), "st_xts", [t5p])
                    dB.did_r(tsx)
                    buf("xts").did_w(tsx)
                if idx == 0:
                    c0, n, uo = 120, 8, 0
                elif idx == 17:
                    c0, n, uo = 0, 8, 8 + NQ
                else:
                    c0, n, uo = 0, 128, 8 + (idx - 1) * 128
                bk = pk % 4
                pk += 1
                pu = banks[bk][:].rearrange("p (g t) -> p g t", t=128)
                tl = None
                for g in range(4):
                    for c in range(8):
                        tl = P.op("pe", (lambda g, c, c0, n, i, pu: lambda e: e.matmul(
                            pu[:, g, 0:n], lhsT=Wu4[:, c, g * 128:(g + 1) * 128], rhs=xTc[i][:, c, c0:c0 + n],
                            start=(c == 0), stop=(c == 7)))(g, c, c0, n, i, pu),
                            dB.rd() + buf("W").rd() + bankB[bk].wr(), inc=(g == 3 and c == 7))
                dB.did_r(tl)
                bankB[bk].did_w(tl)
                t = P.op("dve", (lambda n, uo, pu: lambda e: e.tensor_copy(out=UT[:, :, uo:uo + n], in_=pu[:, :, 0:n]))(n, uo, pu),
                         bankB[bk].rd() + buf("UT").wr(), dr=False)
                bankB[bk].did_r(t)
                buf("UTw").did_w(t)
            buf("W").did_r(tl)
            for g in range(4):
                t = P.op("dve", (lambda g: lambda e: e.tensor_tensor(out=UT[:, g, 0:8], in0=UT[:, g, 0:8], in1=hv[:, 0:8], op=ALU.mult))(g),
                         buf("hv").rd() + buf("UTw").rd())
                t = P.op("dve", (lambda g: lambda e: e.tensor_tensor(out=UT[:, g, 8 + NQ:16 + NQ], in0=UT[:, g, 8 + NQ:16 + NQ],
                                                                     in1=hv[:, 8:16], op=ALU.mult))(g))
            L = 2064
            for g in range(4):
                w = WINS[g]
                U = UT[:, g, :]
                t = P.op("dve", (lambda U: lambda e: e.tensor_tensor(out=Sa[:, 1:L], in0=U[:, 0:L - 1], in1=U[:, 1:L], op=ALU.add))(U),
                         buf("dpool").rd())
                cur, oth = Sa, Sb
                if w >= 4:
                    t = P.op("dve", (lambda cur, oth: lambda e: e.tensor_tensor(out=oth[:, 2:L - 1], in0=cur[:, 1:L - 2], in1=cur[:, 3:L], op=ALU.add))(cur, oth))
                    cur, oth = oth, cur
                if w >= 8:
                    t = P.op("dve", (lambda cur, oth: lambda e: e.tensor_tensor(out=oth[:, 4:L - 3], in0=cur[:, 2:L - 5], in1=cur[:, 6:L - 1], op=ALU.add))(cur, oth))
                    cur, oth = oth, cur
                if w >= 16:
                    t = P.op("dve", (lambda cur, oth: lambda e: e.tensor_tensor(out=oth[:, 8:L - 7], in0=cur[:, 4:L - 11], in1=cur[:, 12:L - 3], op=ALU.add))(cur, oth))
                    cur, oth = oth, cur
                t = P.op("dve", (lambda cur, U, w: lambda e: e.scalar_tensor_tensor(
                    out=dpool, in0=cur[:, 8:8 + NQ], scalar=1.0 / w, in1=U[:, 8:8 + NQ], op0=ALU.mult, op1=ALU.subtract))(cur, U, w),
                    buf("dpool").wr())
                for (lo, ro) in ((0, 0), (NQ - 8, 8)):
                    t = P.op("dve", (lambda cur, oth, g, lo, ro: lambda e: e.tensor_tensor(
                        out=oth[:, 0:8], in0=cur[:, 8 + lo:16 + lo], in1=rcorr[:, g, ro:ro + 8], op=ALU.mult))(cur, oth, g, lo, ro),
                        buf("rcorr").rd())
                    t = P.op("dve", (lambda U, oth, lo: lambda e: e.tensor_tensor(
                        out=dpool[:, lo:lo + 8], in0=oth[:, 0:8], in1=U[:, 8 + lo:16 + lo], op=ALU.subtract))(U, oth, lo))
                buf("dpool").did_w(t)
                for ch in range(4):
                    bk = pk % 4
                    pk += 1
                    tm = P.op("pe", (lambda g, ch, bk: lambda e: e.matmul(banks[bk][:], lhsT=wpb[:, g, :], rhs=dpool[:, ch * 512:(ch + 1) * 512],
                                                                          start=True, stop=True))(g, ch, bk),
                              buf("dpool").rd() + buf("wpb").rd() + bankB[bk].wr())
                    bankB[bk].did_w(tm)
                    buf("dpool").did_r(tm)
                    t = P.op("act", (lambda g, ch, bk: lambda e: e.activation(out=pT[:, g, ch * 512:(ch + 1) * 512], in_=banks[bk][:],
                                                                              func=AF.Copy, scale=psc[:, g:g + 1]))(g, ch, bk),
                             bankB[bk].rd() + buf("psc").rd(), dr=False)
                    bankB[bk].did_r(t)
                    buf("pT").did_w(t)
            buf("X1").did_r(tm)
            buf("X1").did_r(t)

        acc_loc = {}
        k = 0
        for c in range(2):
            for qt in range(4):
                acc_loc[(c, qt)] = (4 + k // 2, (k % 2) * 160)
                k += 1
        sc_cnt = [0]
        pt_cnt = [0]

        att_state = {"zeroed": False, "deferred": None}

        def attention(hp):
            att_state["zeroed"] = False
            for hh in range(2):
                h = 2 * hp + hh
                for j in range(4):
                    pend = None
                    if not att_state["zeroed"]:
                        for bk_ in (4, 5, 6, 7):
                            tz = P.op("dve", (lambda bk_: lambda e: e.memset(banks[bk_][:, 0:320], 0.0))(bk_), bankB[bk_].wr(), dr=False)
                            bankB[bk_].did_w(tz)
                    att_state["zeroed"] = False
                    sl_ = SLOPES[h]
                    mlist = []
                    for m_ in range(NT):
                        if m_ < 16:
                            if m_ < 4 * j:
                                dmin = 512 * j - (128 * m_ + 127)
                            elif m_ > 4 * j + 3:
                                dmin = 128 * m_ - (512 * j + 511)
                            else:
                                dmin = 0
                        else:
                            dmin = min(128 * m_ - (512 * j + 511), 512 * j + S - (128 * m_ + 127))
                        if sl_ * dmin >= SKIP_T:
                            continue
                        mlist.append(m_)
                    for mi in range(len(mlist) + 1):
                        if mi == 3 and att_state["deferred"] is not None:
                            att_state["deferred"]()
                            att_state["deferred"] = None
                        cur = None
                        m = mlist[mi] if mi < len(mlist) else NT
                        if mi < len(mlist):
                            if m < 16:
                                i = m - 4 * j
                                if i < 0:
                                    kind, col, band = 0, 65, None
                                elif i > 3:
                                    kind, col, band = 1, 64, None
                                else:
                                    kind, col, band = None, None, i
                            else:
                                kind, col, band = 2, m, None
                            cur = []
                            bks = []
                            for c in range(2):
                                bk = sc_cnt[0] % 4
                                sc_cnt[0] += 1
                                bks.append(bk)
                                P.op("pe", (lambda c, hh, m, j, bk: lambda e: e.matmul(
                                    banks[bk][:], lhsT=KT[64 * c:64 * c + 64, hh, m * 128:(m + 1) * 128],
                                    rhs=QT[64 * c:64 * c + 64, hh, j * 512:(j + 1) * 512], start=True, stop=False))(c, hh, m, j, bk),
                                    bankB[bk].wr() + buf("KT").rd() + buf("QT").rd(), inc=False)
                            tqs = []
                            for c in range(2):
                                bk = bks[c]
                                if band is None:
                                    p0 = 32 * c
                                    tq = P.op("pe", (lambda h, col, j, bk, p0: lambda e: e.matmul(
                                        banks[bk][:], lhsT=csg[p0:p0 + 2, h, col:col + 1].to_broadcast([2, 128]),
                                        rhs=qpos[p0:p0 + 2, j * 512:(j + 1) * 512], start=False, stop=True))(h, col, j, bk, p0),
                                        buf("csg").rd() + buf("qpos").rd() + buf("csg2").rd() + buf("qpos2").rd())
                                else:
                                    P.op("pe", (lambda h, band, bk: lambda e: e.matmul(
                                        banks[bk][:], lhsT=sid[:, h, :], rhs=dband[:, 0, band, :], start=False, stop=False))(h, band, bk),
                                        buf("sid").rd() + buf("dband").rd(), inc=False)
                                    tq = P.op("pe", (lambda h, band, bk: lambda e: e.matmul(
                                        banks[bk][:], lhsT=sid[:, h, :], rhs=dband[:, 1, band, :], start=False, stop=True))(h, band, bk))
                                bankB[bk].did_w(tq)
                            for c in range(2):
                                bk = bks[c]
                                pi = pt_cnt[0] % 4
                                pt_cnt[0] += 1
                                pb = buf(f"PT{pi}")
                                if kind is None:
                                    te = P.op("act", (lambda bk, pi: lambda e: e.activation(
                                        out=PTb[pi][:], in_=banks[bk][:], func=AF.Exp, scale=0.125))(bk, pi),
                                        bankB[bk].rd() + pb.wr(), dr=False)
                                else:
                                    te = P.op("act", (lambda bk, pi, kind, h, m: lambda e: e.activation(
                                        out=PTb[pi][:], in_=banks[bk][:], func=AF.Exp, bias=kb[:, kind, h, m:m + 1], scale=0.125))(bk, pi, kind, h, m),
                                        bankB[bk].rd() + pb.wr() + buf("kb").rd(), dr=False)
                                bankB[bk].did_r(te)
                                pb.did_w(te)
                                cur.append((c, pi, m, mi == 0, mi == len(mlist) - 1))
                        if pend is not None:
                            for (c, pi, mm_, first_, last_) in pend:
                                pb = buf(f"PT{pi}")
                                tl = None
                                for qt in range(4):
                                    bk, off = acc_loc[(c, qt)]
                                    tl = P.op("pe", (lambda pi, qt, bk, off, mm_, hh, last_: lambda e: e.matmul(
                                        banks[bk][:, off:off + 129], lhsT=PTb[pi][:, qt * 128:(qt + 1) * 128],
                                        rhs=VA[:, mm_, hh, 0:129], start=False, stop=last_,
                                        skip_group_check=True))(pi, qt, bk, off, mm_, hh, last_),
                                        pb.rd() + buf("VA").rd() + (bankB[bk].wr() if first_ else []), inc=(qt == 3))
                                pb.did_r(tl)
                                if last_:
                                    for bk in (4, 5, 6, 7):
                                        bankB[bk].did_w(tl)
                        pend = cur
                    if att_state["deferred"] is not None:
                        att_state["deferred"]()
                        att_state["deferred"] = None
                    cps = []
                    for bi, bk in enumerate((4, 5, 6, 7)):
                        eng_ = "act" if bi % 2 == 0 else "dve"
                        if eng_ == "act":
                            tcp = P.op("act", (lambda bi, bk: lambda e: e.copy(out=accS[:, bi, 0:289], in_=banks[bk][:, 0:289]))(bi, bk),
                                       bankB[bk].rd() + buf("accS").wr(), dr=False)
                        else:
                            tcp = P.op("dve", (lambda bi, bk: lambda e: e.tensor_copy(out=accS[:, bi, 0:289], in_=banks[bk][:, 0:289]))(bi, bk),
                                       bankB[bk].rd() + buf("accS").wr(), dr=False)
                        bankB[bk].did_r(tcp)
                        cps.append(tcp)
                    for bk_ in (4, 5, 6, 7):
                        tz = P.op("dve", (lambda bk_: lambda e: e.memset(banks[bk_][:, 0:320], 0.0))(bk_), bankB[bk_].wr(), dr=False)
                        bankB[bk_].did_w(tz)
                    att_state["zeroed"] = True
                    td1 = None
                    for qt in range(4):
                        b0, o0 = acc_loc[(0, qt)]
                        b1, o1 = acc_loc[(1, qt)]
                        sc0 = 16 + 8 * qt
                        P.op("dve", (lambda b0, o0, sc0: lambda e: e.reciprocal(out=small[:, sc0:sc0 + 1], in_=accS[:, b0 - 4, o0 + 128:o0 + 129]))(b0, o0, sc0),
                             cps + buf("nrm").wr() + buf("tmpA").wr())
                        P.op("dve", (lambda b1, o1, sc0: lambda e: e.reciprocal(out=small[:, sc0 + 1:sc0 + 2], in_=accS[:, b1 - 4, o1 + 128:o1 + 129]))(b1, o1, sc0))
                        P.op("dve", (lambda sc0: lambda e: e.tensor_tensor(out=small[:, sc0 + 2:sc0 + 3], in0=small[:, sc0 + 1:sc0 + 2], in1=small[:, 0:1], op=ALU.mult))(sc0),
                             buf("neglam").rd())
                        P.op("dve", (lambda b0, o0, sc0, qt: lambda e: e.tensor_scalar(out=tmpA[:, 0, qt * 128:(qt + 1) * 128], in0=accS[:, b0 - 4, o0:o0 + 128],
                                                                                         scalar1=small[:, sc0:sc0 + 1], scalar2=None, op0=ALU.mult))(b0, o0, sc0, qt))
                        td1 = P.op("dve", (lambda b1, o1, sc0, qt: lambda e: e.scalar_tensor_tensor(
                            out=tmpA[:, 0, qt * 128:(qt + 1) * 128], in0=accS[:, b1 - 4, o1:o1 + 128], scalar=small[:, sc0 + 2:sc0 + 3],
                            in1=tmpA[:, 0, qt * 128:(qt + 1) * 128], op0=ALU.mult, op1=ALU.add))(b1, o1, sc0, qt))
                    buf("accS").did_w(cps[0])
                    buf("accS").did_r(cps[1])
                    buf("accS").did_r(cps[2])
                    buf("accS").did_r(cps[3])
                    buf("accS").did_r(td1)
                    buf("tmpA").did_w(td1)

                    def norm_tail(j=j, h=h, td1=td1):
                        ta1 = None
                        for qt in range(4):
                            sc0 = 16 + 8 * qt
                            P.op("act", (lambda qt, sc0: lambda e: e.activation(out=tmpA[:, 1, 0:128], in_=tmpA[:, 0, qt * 128:(qt + 1) * 128], func=AF.Square,
                                                                                accum_out=small[:, sc0 + 3:sc0 + 4]))(qt, sc0), [td1], dr=False)
                            ta1 = P.op("act", (lambda sc0: lambda e: e.activation(out=small[:, sc0 + 4:sc0 + 5], in_=small[:, sc0 + 3:sc0 + 4], func=AF.Sqrt,
                                                                                  bias=epsb[:, 0:1], scale=1.0 / 128))(sc0))
                        t4 = None
                        for qt in range(4):
                            sc0 = 16 + 8 * qt
                            P.op("dve", (lambda sc0: lambda e: e.reciprocal(out=small[:, sc0 + 4:sc0 + 5], in_=small[:, sc0 + 4:sc0 + 5]))(sc0), [ta1])
                            t4 = P.op("dve", (lambda j, qt, h, sc0: lambda e: e.scalar_tensor_tensor(
                                out=a_tok[:, 4 * j + qt, h * 128:(h + 1) * 128], in0=tmpA[:, 0, qt * 128:(qt + 1) * 128], scalar=small[:, sc0 + 4:sc0 + 5],
                                in1=sg8[:], op0=ALU.mult, op1=ALU.mult))(j, qt, h, sc0), buf("sg8").rd())
                        buf("tmpA").did_r(t4)
                        buf("tmpA").did_r(ta1)
                        buf("nrm").did_r(t4)
                        buf("nrm").did_w(t4)
                        buf("a_tok").did_w(t4)

                    att_state["deferred"] = norm_tail
            if att_state["deferred"] is not None:
                att_state["deferred"]()
                att_state["deferred"] = None

        if noattn:
            t = P.op("dve", lambda e: e.memset(X3[:, 0:8192], 0.0))
            buf("a_tok").did_w(t)
        if stage >= 2 and not noattn:
            for hp in range(2):
                dW = buf("W").wr() + buf("X1").wr()
                c0 = 2 * hp * 128
                tw = P.dma("pool", lambda e, c0=c0: e.dma_start(out=Wq2, in_=w_in[:, c0:c0 + 256].rearrange("(c p) n -> p c n", p=128)), "ld_w0", dW)
                tw = P.dma("pool", lambda e, c0=c0: e.dma_start(out=Wk2, in_=w_in[:, 512 + c0:512 + c0 + 256].rearrange("(c p) n -> p c n", p=128)), "ld_w0", dW)
                tw = P.dma("pool", lambda e, c0=c0: e.dma_start(out=Wv2, in_=w_in[:, 1024 + c0:1024 + c0 + 256].rearrange("(c p) n -> p c n", p=128)), "ld_w0", dW)
                buf("W").did_w(tw)
                t = P.op("dve", lambda e: e.memset(VA[:, :, :, 128:130], 1.0), buf("X1").wr() + buf("VA").wr() + buf("KT").wr())
                buf("VA").did_w(t)
                buf("KT").did_w(t)
                buf("QT").did_w(t)
                pk = 0
                last = None
                st_ = {"pk": 0, "last": None, "lastv": None, "tl": None}

                def proj_part(ch, part):
                    i = ch % 2
                    dB = buf(f"xTc{i}")
                    groups = [("k", 0), ("k", 1)] + ([("q", 0), ("q", 1)] if ch < 4 else [])
                    for gi_, (kind, hh) in enumerate(groups):
                        if gi_ % 4 != part:
                            continue
                        bk = st_["pk"] % 4
                        st_["pk"] += 1
                        Wt = Wk2 if kind == "k" else Wq2
                        tl = None
                        for c in range(8):
                            tl = P.op("pe", (lambda Wt, c, hh, i, bk: lambda e: e.matmul(
                                banks[bk][:], lhsT=Wt[:, c, hh * 128:(hh + 1) * 128], rhs=xTc[i][:, c, :],
                                start=(c == 0), stop=(c == 7)))(Wt, c, hh, i, bk),
                                dB.rd() + buf("W").rd() + bankB[bk].wr(), inc=(c == 7))
                        bankB[bk].did_w(tl)
                        dstT = KT if kind == "k" else QT
                        t = P.op("dve", (lambda dstT, hh, ch, bk: lambda e: e.tensor_copy(
                            out=dstT[:, hh, ch * 512:(ch + 1) * 512], in_=banks[bk][:]))(dstT, hh, ch, bk),
                            bankB[bk].rd() + buf("KT").rd(), dr=False)
                        bankB[bk].did_r(t)
                        st_["last"] = t
                        dB.did_r(tl)
                    tt = part
                    bk = st_["pk"] % 4
                    st_["pk"] += 1
                    m = 4 * ch + tt
                    tl = None
                    for c in range(8):
                        tl = P.op("pe", (lambda c, tt, i, bk: lambda e: e.matmul(
                            banks[bk][:, 0:256], lhsT=xTc[i][:, c, tt * 128:(tt + 1) * 128], rhs=Wv2[:, c, :],
                            start=(c == 0), stop=(c == 7)))(c, tt, i, bk),
                            dB.rd() + buf("W").rd() + bankB[bk].wr(), inc=(c == 7))
                    bankB[bk].did_w(tl)
                    t = P.op("act", (lambda m, bk: lambda e: e.copy(
                        out=VA[:, m, :, 0:128], in_=banks[bk][:, 0:256].rearrange("p (h e) -> p h e", e=128)))(m, bk),
                        bankB[bk].rd() + buf("VA").rd(), dr=False)
                    bankB[bk].did_r(t)
                    st_["lastv"] = t
                    st_["tl"] = tl
                    dB.did_r(tl)

                def load_chunk(ch_):
                    i2 = ch_ % 2
                    dB2 = buf(f"xTc{i2}")
                    tlx = P.dma("sp", (lambda ch_, i2: lambda e: e.dma_start(out=xTc_flat[i2], in_=XTs[ch_]))(ch_, i2), f"ld_xc{i2}",
                                dB2.wr() + buf("xts").rd())
                    dB2.did_w(tlx)

                if hp == 0:
                    load_chunk(0)
                    load_chunk(1)
                    ctxA = {16: fe_A(16), 17: fe_A(17)}

                    def feB_tile(n):
                        if n + 2 < 64:
                            ctxA[n + 2] = fe_A(n + 2)
                        i2 = (n // 4) % 2
                        dB2 = buf(f"xTc{i2}")
                        t5 = fe_B(ctxA.pop(n), xTc[i2][:, :, (n % 4) * 128:(n % 4 + 1) * 128], dB2)
                        if n % 4 == 3:
                            ch_ = n // 4
                            tsx = P.dma("pool", (lambda ch_, i2: lambda e: e.dma_start(out=XTs[ch_], in_=xTc_flat[i2]))(ch_, i2), "st_xts", [t5])
                            dB2.did_r(tsx)
                            buf("xts").did_w(tsx)

                    for ch in range(16):
                        for tt in range(4):
                            if 4 <= ch + 1 < 16:
                                feB_tile(4 * (ch + 1) + tt)
                            proj_part(ch, tt)
                        if ch + 2 < 4:
                            load_chunk(ch + 2)
                else:
                    load_chunk(0)
                    load_chunk(1)
                    for ch in range(16):
                        for tt in range(4):
                            proj_part(ch, tt)
                        if ch + 2 < 16:
                            load_chunk(ch + 2)
                tl, last, lastv = st_["tl"], st_["last"], st_["lastv"]
                buf("W").did_r(tl)
                buf("KT").did_w(last)
                buf("QT").did_w(last)
                buf("VA").did_w(lastv)
                if stage >= 6:
                    pdeps = [last, lastv]
                    if hp == 0:
                        zero_fill()
                        precast_weights(0, 8, pdeps)
                    else:
                        precast_weights(8, 32, pdeps)
                if stage >= 3:
                    attention(hp)
                    tk = (("pe", P.cnt["pe"]))
                    buf("X1").did_r(tk)
                    buf("KT").did_r(tk)
                    buf("VA").did_r(tk)
                    buf("QT").did_r(tk)

        if stage >= 4:
            dW = buf("W").wr() + [("pe", P.cnt["pe"])]
            for c_ in range(8):
                tw = P.dma("pool", (lambda c_: lambda e: e.dma_start(out=Wo[:, c_, :], in_=w_out[c_ * 128:(c_ + 1) * 128, :]))(c_), "ld_w0", dW)
            buf("W").did_w(tw)
            for t_ in range(16):
                i = t_ % 2
                bx = buf(f"xt{i}")
                tx = P.dma("sp", (lambda t_, i: lambda e: e.dma_start(out=xtb[i], in_=xr[t_ * 128:(t_ + 1) * 128, :]))(t_, i), f"ld_xt{i}", bx.wr())
                bx.did_w(tx)
                bk = 6 + i
                pv = banks[bk][:].bitcast(BF16).rearrange("p (c t) -> p c t", t=128)
                tl = None
                for c in range(4):
                    tl = P.op("pe", (lambda c, t_, pv: lambda e: e.transpose(out=pv[:, c, :], in_=a_tok[:, t_, c * 128:(c + 1) * 128], identity=idb[:]))(c, t_, pv),
                              buf("a_tok").rd() + bankB[bk].wr(), inc=(c == 3))
                bankB[bk].did_w(tl)
                ba = buf(f"aT{i}")
                t5 = P.op("act", (lambda i, pv: lambda e: e.copy(out=aTt[i], in_=pv[:, 0:4, :]))(i, pv), bankB[bk].rd() + ba.wr(), dr=False)
                bankB[bk].did_r(t5)
                ba.did_w(t5)
                for half in range(2):
                    bk2 = 2 * i + half
                    tl = None
                    for c in range(8):
                        lhs = aTt[i][:, c, :] if c < 4 else pT[:, c - 4, t_ * 128:(t_ + 1) * 128]
                        tl = P.op("pe", (lambda lhs, c, half, bk2: lambda e: e.matmul(
                            banks[bk2][:], lhsT=lhs, rhs=Wo[:, c, half * 512:(half + 1) * 512], start=(c == 0), stop=(c == 7)))(lhs, c, half, bk2),
                            ba.rd() + buf("pT").rd() + buf("W").rd() + bankB[bk2].wr(), inc=(c == 7))
                    bankB[bk2].did_w(tl)
                    t = P.op("dve", (lambda t_, half, bk2, i: lambda e: e.tensor_tensor(
                        out=h1[:, t_, half * 512:(half + 1) * 512], in0=banks[bk2][:], in1=xtb[i][:, half * 512:(half + 1) * 512], op=ALU.add))(t_, half, bk2, i),
                        bankB[bk2].rd() + bx.rd() + buf("X1").wr(), dr=False)
                    bankB[bk2].did_r(t)
                    bx.did_r(t)
                ba.did_r(tl)
                buf("h1").did_w(t)
            buf("W").did_r(tl)
            buf("X3").did_r(tl)

        def load_weights(ex):
            i = ex % 2
            bw = buf(f"we{i}")
            dW = bw.wr() + buf("W").wr() + buf("xt0").wr() + buf("xt1").wr() + buf("xt2").wr() + buf("xs0").wr() + buf("xs1").wr() + buf("xs2").wr()
            dW = dW + list(pc_last.values())
            tw = None
            tw = P.dma("pool", (lambda ex, i: lambda e: e.dma_start(out=wg_flat[i], in_=Wg_bf[ex].rearrange("(p c) f -> p (c f)", c=8)))(ex, i), f"ld_we{i}", dW)
            tw = P.dma("pool", (lambda ex, i: lambda e: e.dma_start(out=wu_flat[i], in_=Wu_bf[ex].rearrange("(p c) f -> p (c f)", c=8)))(ex, i), f"ld_we{i}", dW)
            for c0 in (0, 2):
                tw = P.dma("pool", (lambda ex, i, c0: lambda e: e.dma_start(
                    out=wdb[i][:, c0:c0 + 2, :], in_=Wd_bf[ex, c0 * 128:(c0 + 2) * 128, :].rearrange("(c p) n -> p c n", p=128)))(ex, i, c0), f"ld_we{i}", dW)
            bw.did_w(tw)

        if stage >= 5:
            if stage >= 6 and buf("zf").w is None:
                zero_fill()
                precast_weights(0, 32, [])
            if stage >= 6:
                load_weights(0)
                load_weights(1)
            t = P.op("dve", lambda e: e.memset(onesb[:], 1.0))
            buf("onesb").did_w(t)

            def prepA(t_):
                col = t_ % 4
                t1 = P.op("act", (lambda t_, col: lambda e: e.activation(out=junk[:], in_=h1[:, t_, :], func=AF.Square, accum_out=ss_t[:, col:col + 1]))(t_, col),
                          buf("h1").rd() + buf("junk").wr() + buf(f"ss{col}").wr(), dr=False)
                buf("junk").did_w(t1)
                t2 = P.op("act", (lambda col: lambda e: e.activation(out=rs_t[:, col:col + 1], in_=ss_t[:, col:col + 1], func=AF.Sqrt,
                                                                     bias=epsb[:, 0:1], scale=1.0 / DM))(col), buf(f"rs{col}").wr())
                t3 = P.op("dve", (lambda col: lambda e: e.reciprocal(out=rs_t[:, col:col + 1], in_=rs_t[:, col:col + 1]))(col), [t2], dr=False)
                buf(f"ss{col}").did_r(t2)
                t4 = P.op("dve", (lambda t_, col: lambda e: e.scalar_tensor_tensor(
                    out=xnAll[:, t_, :], in0=h1[:, t_, :], scalar=rs_t[:, col:col + 1], in1=gvb[:, 1, :], op0=ALU.mult, op1=ALU.mult))(t_, col),
                    buf("gv1").rd() + buf("X3").wr())
                buf(f"ss{col}").did_w(t1)
                buf(f"rs{col}").did_w(t3)
                buf(f"rs{col}").did_r(t4)
                buf(f"xn{t_}").did_w(t4)

            def prepB(t_):
                i = t_ % 2
                bn = buf(f"xn{t_}")
                bk = 0 + i
                pv = banks[bk][:].bitcast(BF16).rearrange("p (c t) -> p c t", t=128)
                tl = None
                for c in range(8):
                    tl = P.op("pe", (lambda c, pv, t_: lambda e: e.transpose(out=pv[:, c, :], in_=xnAll[:, t_, c * 128:(c + 1) * 128], identity=idb[:]))(c, pv, t_),
                              bn.rd() + bankB[bk].wr() + buf("idb").rd(), inc=(c == 7))
                bankB[bk].did_w(tl)
                bt = buf(f"xnT2{i}")
                ta = P.op("act", (lambda pv, i: lambda e: e.copy(out=xnT2[i], in_=pv))(pv, i),
                          bankB[bk].rd() + buf("X1").wr() + bt.wr(), dr=False)
                bankB[bk].did_r(ta)
                bt.did_w(ta)
                bk = 4 + i
                tl = None
                for c in range(8):
                    tl = P.op("pe", (lambda c, bk, i: lambda e: e.matmul(banks[bk][:, 0:36], lhsT=xnT2[i][:, c, :], rhs=wrtb[:, c, :],
                                                                         start=(c == 0), stop=(c == 7)))(c, bk, i),
                              bt.rd() + buf("wrtb").rd() + bankB[bk].wr(), inc=(c == 7))
                bt.did_r(tl)
                bankB[bk].did_w(tl)
                t = P.op("dve", (lambda bk, t_: lambda e: e.tensor_tensor(out=L_all[:, t_, :], in0=banks[bk][:, 0:36], in1=rbb[:], op=ALU.add))(bk, t_),
                         bankB[bk].rd() + buf("rbb").rd(), dr=False)
                bankB[bk].did_r(t)
                return t

            prepA(0)
            prepA(1)
            tL = None
            for t_ in range(16):
                if t_ + 2 < 16:
                    prepA(t_ + 2)
                tL = prepB(t_)

            def bc(v, n):
                return v.unsqueeze(2).to_broadcast([128, 16, n])

            D = lambda fn, deps=(): P.op("dve", fn, deps)
            GL = L_all[:, :, 0:4]
            D(lambda e: e.tensor_reduce(out=V_gmax, in_=GL, axis=AX.X, op=ALU.max), [tL])
            D(lambda e: e.tensor_tensor(out=gmask, in0=GL, in1=bc(V_gmax, 4), op=ALU.is_equal))
            t = D(lambda e: e.tensor_tensor(out=gdiff, in0=GL, in1=bc(V_gmax, 4), op=ALU.subtract))
            t = P.op("act", lambda e: e.activation(out=gdiff, in_=gdiff, func=AF.Exp), [t])
            D(lambda e: e.tensor_reduce(out=V_gsum, in_=gdiff, axis=AX.X, op=ALU.add), [t])
            D(lambda e: e.tensor_tensor(out=esel, in0=L_all[:, :, 4:12], in1=bc(gmask[:, :, 0], 8), op=ALU.mult))
            for g in range(1, 4):
                D((lambda g: lambda e: e.tensor_tensor(out=e2, in0=L_all[:, :, 4 + 8 * g:12 + 8 * g], in1=bc(gmask[:, :, g], 8), op=ALU.mult))(g))
                D(lambda e: e.tensor_tensor(out=esel, in0=esel, in1=e2, op=ALU.add))
            D(lambda e: e.tensor_reduce(out=V_m1, in_=esel, axis=AX.X, op=ALU.max))
            D(lambda e: e.tensor_tensor(out=mask1, in0=esel, in1=bc(V_m1, 8), op=ALU.is_equal))
            D(lambda e: e.scalar_tensor_tensor(out=e2, in0=mask1, scalar=-1.0e30, in1=esel, op0=ALU.mult, op1=ALU.add))
            D(lambda e: e.tensor_reduce(out=V_m2, in_=e2, axis=AX.X, op=ALU.max))
            D(lambda e: e.tensor_tensor(out=mask2, in0=e2, in1=bc(V_m2, 8), op=ALU.is_equal))
            t = D(lambda e: e.tensor_tensor(out=V_ex, in0=V_m2, in1=V_m1, op=ALU.subtract))
            t = P.op("act", lambda e: e.activation(out=V_ex, in_=V_ex, func=AF.Exp), [t])
            D(lambda e: e.scalar_tensor_tensor(out=V_den, in0=V_ex, scalar=1.0, in1=V_gsum, op0=ALU.add, op1=ALU.mult), [t])
            D(lambda e: e.reciprocal(out=gatesAll[:, :, 0], in_=V_den))
            D(lambda e: e.tensor_tensor(out=gatesAll[:, :, 1], in0=gatesAll[:, :, 0], in1=V_ex, op=ALU.mult))
            D(lambda e: e.tensor_tensor(out=msum, in0=mask1, in1=mask2, op=ALU.add))
            tmb = None
            for g in range(4):
                tmb = D((lambda g: lambda e: e.tensor_tensor(out=Mb_all[:, :, 8 * g:8 * g + 8], in0=msum, in1=bc(gmask[:, :, g], 8), op=ALU.mult))(g))
            prk = banks[2][:].rearrange("p (t e) -> p t e", e=32)
            tr_ = None
            for t_ in range(16):
                for tp_ in range(t_):
                    P.op("pe", (lambda t_, tp_: lambda e: e.matmul(prk[:, t_, :], lhsT=onesb[:], rhs=Mb_all[:, tp_, :], start=(tp_ == 0), stop=False))(t_, tp_),
                         [tmb] + buf("onesb").rd() + bankB[2].wr(), inc=False)
                tr_ = P.op("pe", (lambda t_: lambda e: e.matmul(prk[:, t_, :], lhsT=ltri[:], rhs=Mb_all[:, t_, :], start=(t_ == 0), stop=True))(t_),
                           [tmb] + buf("ltri").rd() + bankB[2].wr())
            bankB[2].did_w(tr_)
            D(lambda e: e.tensor_tensor(out=tmp32, in0=prk, in1=eoff[:].rearrange("p (t e) -> p t e", e=32), op=ALU.add), [tr_] + buf("eoff").rd())
            bankB[2].did_r(("dve", P.cnt["dve"]))
            D(lambda e: e.tensor_tensor(out=Rr, in0=tmp32[:, :, 0:8], in1=bc(gmask[:, :, 0], 8), op=ALU.mult))
            for g in range(1, 4):
                D((lambda g: lambda e: e.tensor_tensor(out=e2, in0=tmp32[:, :, 8 * g:8 * g + 8], in1=bc(gmask[:, :, g], 8), op=ALU.mult))(g))
                D(lambda e: e.tensor_tensor(out=Rr, in0=Rr, in1=e2, op=ALU.add))
            D(lambda e: e.tensor_tensor(out=e2, in0=Rr, in1=mask1, op=ALU.mult))
            D(lambda e: e.tensor_reduce(out=slf[:, :, 0], in_=e2, axis=AX.X, op=ALU.add))
            D(lambda e: e.tensor_tensor(out=e2, in0=Rr, in1=mask2, op=ALU.mult))
            D(lambda e: e.tensor_reduce(out=slf[:, :, 1], in_=e2, axis=AX.X, op=ALU.add))
            ti = D(lambda e: e.tensor_copy(out=idxAll[:], in_=slf))
            buf("idx").did_w(ti)
            sc_toks = []
            if stage >= 6:
                for t_ in range(16):
                    i = t_ % 2
                    tsc = None
                    for k_ in range(2):
                        tsc = P.dma("pool", (lambda t_, k_: lambda e: e.indirect_dma_start(
                            out=Xbuf[:, :], out_offset=bass.IndirectOffsetOnAxis(ap=idxAll[:, t_, k_:k_ + 1], axis=0),
                            in_=xnAll[:, t_, :], in_offset=None, bounds_check=None, oob_is_err=False))(t_, k_),
                            f"sc_x{i}", [ti] + buf("zf").rd())
                    sc_toks = [x for x in sc_toks if x[0] != f"sc_x{i}"] + [tsc]
            buf("X3").did_r(("pe", P.cnt["pe"]))
            for tk in sc_toks:
                buf("X3").did_r(tk)

        if stage >= 6:
            y_toks = {}

            def load_xe(ex):
                i = ex % 2
                bxe = buf(f"xe{i}")
                tld = P.dma("sp", (lambda ex, i: lambda e: e.dma_start(
                    out=xe[i], in_=Xbuf[ex * CAP:(ex + 1) * CAP, :].rearrange("(s p) d -> p s d", p=128)))(ex, i),
                    f"ld_xe{i}", sc_toks + bxe.wr() + buf("X3").wr())
                bxe.did_w(tld)

            def partA(ex):
                i = ex % 2
                bxe, bxt = buf(f"xe{i}"), buf(f"XeT{i}")
                tl = tcp = None
                for st_ in range(2):
                    bk = st_
                    pv = banks[bk][:].bitcast(BF16).rearrange("p (c t) -> p c t", t=128)
                    for c in range(8):
                        tl = P.op("pe", (lambda c, pv, i, st_: lambda e: e.transpose(out=pv[:, c, :], in_=xe[i].rearrange("p s (q c) -> p s c q", c=8)[:, st_, c, :], identity=idb[:]))(c, pv, i, st_),
                                  bxe.rd() + bankB[bk].wr(), inc=(c == 7))
                    bankB[bk].did_w(tl)
                    tcp = P.op("act", (lambda pv, i, st_: lambda e: e.copy(out=XeT[i][:, :, st_ * 128:(st_ + 1) * 128], in_=pv))(pv, i, st_),
                               bankB[bk].rd() + bxt.wr(), dr=False)
                    bankB[bk].did_r(tcp)
                bxe.did_r(tl)
                bxt.did_w(tcp)

            def gateup(ex):
                i = ex % 2
                bw = buf(f"we{i}")
                bxt, bh = buf(f"XeT{i}"), buf(f"HT{i}")
                tu = tm = None
                for fc in range(4):
                    bg, bu = (2, 3) if fc % 2 == 0 else (4, 5)
                    tg = None
                    for c in range(8):
                        tg = P.op("pe", (lambda c, fc, i, bg: lambda e: e.matmul(
                            banks[bg][:, 0:CAP], lhsT=wgb[i][:, c, fc * 128:(fc + 1) * 128], rhs=XeT[i][:, c, :],
                            start=(c == 0), stop=(c == 7)))(c, fc, i, bg),
                            bw.rd() + bxt.rd() + bankB[bg].wr(), inc=(c == 7))
                    bankB[bg].did_w(tg)
                    for c in range(8):
                        tu = P.op("pe", (lambda c, fc, i, bu: lambda e: e.matmul(
                            banks[bu][:, 0:CAP], lhsT=wub[i][:, c, fc * 128:(fc + 1) * 128], rhs=XeT[i][:, c, :],
                            start=(c == 0), stop=(c == 7)))(c, fc, i, bu),
                            bankB[bu].wr(), inc=(c == 7))
                    bankB[bu].did_w(tu)
                    sgi = fc % 2
                    ts = P.op("act", (lambda bg, sgi: lambda e: e.activation(out=tmpA[:, sgi, 0:CAP], in_=banks[bg][:, 0:CAP], func=AF.Silu))(bg, sgi),
                              bankB[bg].rd() + buf(f"sg{sgi}").wr(), dr=False)
                    bankB[bg].did_r(ts)
                    buf(f"sg{sgi}").did_w(ts)
                    tm = P.op("dve", (lambda i, fc, sgi, bu: lambda e: e.tensor_tensor(out=HT[i][:, fc, :], in0=banks[bu][:, 0:CAP], in1=tmpA[:, sgi, 0:CAP],
                                                                                       op=ALU.mult))(i, fc, sgi, bu),
                              bankB[bu].rd() + buf(f"sg{sgi}").rd() + bh.wr(), dr=False)
                    bankB[bu].did_r(tm)
                    buf(f"sg{sgi}").did_r(tm)
                bxt.did_r(tu)
                bh.did_w(tm)

            def down(ex):
                i = ex % 2
                bw, bh = buf(f"we{i}"), buf(f"HT{i}")
                tl = None
                for st_ in range(2):
                    yi = st_
                    by = buf(f"ye{yi}")
                    tcy_a = tcy = None
                    for half in range(2):
                        bk = 6 + half
                        for fc in range(4):
                            tl = P.op("pe", (lambda fc, st_, half, i, bk: lambda e: e.matmul(
                                banks[bk][:], lhsT=HT[i][:, fc, st_ * 128:(st_ + 1) * 128], rhs=wdb[i][:, fc, half * 512:(half + 1) * 512],
                                start=(fc == 0), stop=(fc == 3)))(fc, st_, half, i, bk),
                                bh.rd() + bw.rd() + bankB[bk].wr(), inc=(fc == 3))
                        bankB[bk].did_w(tl)
                        if half == 0:
                            tcy = P.op("act", (lambda yi, bk: lambda e: e.copy(out=ye[yi][:, 0:512], in_=banks[bk][:]))(yi, bk),
                                       bankB[bk].rd() + by.wr(), dr=False)
                            tcy_a = tcy
                        else:
                            tcy = P.op("dve", (lambda yi, bk: lambda e: e.tensor_copy(out=ye[yi][:, 512:1024], in_=banks[bk][:]))(yi, bk),
                                       bankB[bk].rd() + by.wr(), dr=False)
                        bankB[bk].did_r(tcy)
                    tst = P.dma("sp", (lambda ex, st_, yi: lambda e: e.dma_start(
                        out=Ybuf[ex * CAP + st_ * 128: ex * CAP + (st_ + 1) * 128, :], in_=ye[yi]))(ex, st_, yi),
                        f"st_ye{yi}", [tcy_a, tcy])
                    by.did_w(tcy)
                    by.did_r(tcy_a)
                    by.did_r(tst)
                    y_toks[yi] = tst
                bh.did_r(tl)
                bw.did_r(tl)

            load_xe(0)
            load_xe(1)
            partA(0)
            for ex in range(32):
                gateup(ex)
                if ex + 1 < 32:
                    partA(ex + 1)
                if ex + 2 < 32:
                    load_xe(ex + 2)
                down(ex)
                if ex + 2 < 32:
                    load_weights(ex + 2)
            allw = [("pe", P.cnt["pe"])] + buf("we0").wr() + buf("we1").wr()
            for t_ in range(16):
                gs = []
                for k_ in range(2):
                    gi = (2 * t_ + k_) % 8
                    bgb = buf(f"gb{gi}")
                    tg_ = P.dma("pool", (lambda t_, k_, gi: lambda e: e.indirect_dma_start(
                        out=gb[gi], out_offset=None, in_=Ybuf[:, :],
                        in_offset=bass.IndirectOffsetOnAxis(ap=idxAll[:, t_, k_:k_ + 1], axis=0),
                        bounds_check=None, oob_is_err=False))(t_, k_, gi),
                        f"ga{gi}", list(y_toks.values()) + bgb.wr() + allw + buf("idx").rd())
                    bgb.did_w(tg_)
                    gs.append((gi, bgb))
                for k_, (gi, bgb) in enumerate(gs):
                    ta = P.op("dve", (lambda t_, k_, gi: lambda e: e.scalar_tensor_tensor(
                        out=h1[:, t_, :], in0=gb[gi], scalar=gatesAll[:, t_, k_:k_ + 1], in1=h1[:, t_, :],
                        op0=ALU.mult, op1=ALU.add))(t_, k_, gi), bgb.rd() + buf("h1").rd())
                    bgb.did_r(ta)
            buf("h1").did_w(ta)

        if stage >= 4:
            for t_ in range(16):
                col = t_ % 4
                i = t_ % 2
                t1 = P.op("act", (lambda t_, col: lambda e: e.activation(out=junk[:], in_=h1[:, t_, :], func=AF.Square, accum_out=ss_t[:, col:col + 1]))(t_, col),
                          buf("h1").rd() + buf("junk").wr() + buf(f"ss{col}").wr(), dr=False)
                buf("junk").did_w(t1)
                t2 = P.op("act", (lambda col: lambda e: e.activation(out=rs_t[:, col:col + 1], in_=ss_t[:, col:col + 1], func=AF.Sqrt,
                                                                     bias=epsb[:, 0:1], scale=1.0 / DM))(col), buf(f"rs{col}").wr())
                t3 = P.op("dve", (lambda col: lambda e: e.reciprocal(out=rs_t[:, col:col + 1], in_=rs_t[:, col:col + 1]))(col), [t2], dr=False)
                buf(f"ss{col}").did_r(t2)
                t4 = P.op("dve", (lambda t_, col: lambda e: e.scalar_tensor_tensor(
                    out=h1[:, t_, :], in0=h1[:, t_, :], scalar=rs_t[:, col:col + 1], in1=gvb[:, 2, :], op0=ALU.mult, op1=ALU.mult))(t_, col),
                    buf("gv2").rd() + [t1])
                buf(f"ss{col}").did_w(t1)
                buf(f"rs{col}").did_w(t3)
                buf(f"rs{col}").did_r(t4)
                to = P.dma("sp", (lambda t_: lambda e: e.dma_start(out=out[t_ * 128:(t_ + 1) * 128, :], in_=h1[:, t_, :]))(t_), "st_out", [t4])
            P.wait("sp", [to])

        if dbg:
            deps_all = [(k, P.cnt[k]) for k in ("pe", "act", "dve")]
            def dump(name, ap_sb, shape, dt):
                d = nc.dram_tensor(name, list(shape), dt, kind="ExternalOutput").ap()
                tk = P.dma("sp", lambda e: e.dma_start(out=d, in_=ap_sb), "st_dbg", deps_all)
                P.wait("sp", [tk])
            if stage == 1:
                dump("d_pT", pT, [128, 4, NQ], BF16)
                dump("d_UT", UT, [128, 4, 2064], F32)
            if stage == 2:
                dump("d_KT", KT, [128, 2, S], BF16)
                dump("d_QT", QT, [128, 2, NQ], BF16)
                dump("d_VA", X1[:, 16384:16384 + 16640], [128, 16640], BF16)
            if stage == 3:
                dump("d_a", a_tok, [128, 16, 512], BF16)
            if stage == 5:
                dump("d_G", gatesAll[:], [128, 16, 2], F32)
                dump("d_idx", idxAll[:], [128, 16, 2], I32)
        P.emit()
    return nc


def host_consts(qs):
    T0 = NQ * qs
    r = np.arange(S)
    wrapped = (r + T0) >= S
    krel = np.where(wrapped, r - S, r).astype(np.float64)
    sig_tile = np.where(wrapped.reshape(NT, 128)[:, 0], -1.0, 1.0)
    kb = np.zeros((128, 3, 4, 64), np.float32)
    csg = np.zeros((2, 4, 66), np.float32)
    for h in range(4):
        sl = SLOPES[h]
        for m in range(64):
            kpos = 128 * m + np.arange(128)
            kb[:, 0, h, m] = sl * kpos
            kb[:, 1, h, m] = -sl * kpos
            kb[:, 2, h, m] = -sig_tile[m] * sl * krel[128 * m:128 * m + 128]
            csg[:, h, m] = 8 * sl * sig_tile[m]
        csg[:, h, 64] = 8 * sl
        csg[:, h, 65] = -8 * sl
    qpos = np.stack([128.0 * (np.arange(NQ) // 128), (np.arange(NQ) % 128).astype(np.float64)]).astype(np.float32)
    dband = np.zeros((128, 2, 4, 512), np.float32)
    kk = np.arange(128)[:, None]
    qq = np.arange(512)[None, :]
    for i in range(4):
        v = np.abs(qq - kk - 128 * i)
        dband[:, 0, i, :] = 256 * (v // 256)
        dband[:, 1, i, :] = v % 256
    sid = np.zeros((128, 4, 128), np.float32)
    for h in range(4):
        sid[:, h, :] = -8 * SLOPES[h] * np.eye(128)
    hv = np.zeros((128, 16), np.float32)
    hv[:, 0:8] = 1.0 if T0 > 0 else 0.0
    hv[:, 8:16] = 1.0 if T0 + NQ < S else 0.0
    rcorr = np.zeros((128, 4, 16), np.float32)
    for g, w in enumerate(WINS):
        for k_, rr in enumerate(list(range(8)) + list(range(NQ - 8, NQ))):
            t = T0 + rr
            lo = max(t - w // 2, 0)
            hi = min(t + w // 2, S)
            rcorr[:, g, k_] = 1.0 / (hi - lo)
    ltri = np.triu(np.ones((128, 128), np.float32), k=1)
    eoff = np.broadcast_to(np.tile((np.arange(32) * CAP).astype(np.float32), 16)[None, :], (128, 512)).copy()
    return dict(ltri=ltri, eoff=eoff, kb=kb.reshape(128, -1), csg=csg.reshape(2, -1), qpos=qpos, dband=dband.reshape(128, -1),
                sid=sid.reshape(128, -1), hv=hv, rcorr=rcorr.reshape(128, -1))


def make_in_maps(inputs, cores=range(8)):
    f = lambda a: np.ascontiguousarray(np.asarray(a, dtype=np.float32))
    x = f(inputs["x"])
    shared = dict(
        w_in=f(inputs["w_in"])[0], w_out=f(inputs["w_out"])[0], w_pool=f(inputs["w_pool"])[0],
        w_gate=f(inputs["w_gate"])[0], w_up=f(inputs["w_up"])[0], w_down=f(inputs["w_down"])[0],
        wr=np.ascontiguousarray(np.concatenate([f(inputs["w_group_router"])[0], f(inputs["w_expert_router"])[0].reshape(DM, 32)], axis=1)),
        vecs=np.ascontiguousarray(np.stack([f(inputs["norm1_g"])[0], f(inputs["norm2_g"])[0], f(inputs["final_g"])])),
        rbias=np.ascontiguousarray(np.concatenate([f(inputs["b_group_router"])[0], f(inputs["b_expert_router"])[0].reshape(32)])),
        lams=np.ascontiguousarray(np.stack([f(inputs["lambda_q1"])[0], f(inputs["lambda_k1"])[0], f(inputs["lambda_q2"])[0], f(inputs["lambda_k2"])[0]])),
        subg=f(inputs["subln_g"])[0],
        pscale=np.ascontiguousarray(f(inputs["pool_scale"])[0].reshape(4, 128).T),
        ident=np.eye(128, dtype=np.float32),
    )
    maps = []
    for c in cores:
        b, qs = c // 4, c % 4
        m = dict(shared)
        m["xr"] = np.ascontiguousarray(np.roll(x[b], -NQ * qs, axis=0))
        m.update(host_consts(qs))
        maps.append(m)
    return maps


_NC = None


def kernel(**inputs):
    global _NC
    if _NC is None:
        _NC = build()
    in_maps = make_in_maps(inputs)
    res = run_bass_kernel_spmd(_NC, in_maps, core_ids=list(range(8)))
    out = np.zeros((2, S, DM), np.float32)
    for c in range(8):
        b, qs = c // 4, c % 4
        out[b, NQ * qs:NQ * (qs + 1)] = res.results[c]["out"]
    return out
```

```python
import contextlib
import numpy as np
import concourse.bass as bass
import concourse.mybir as mybir
from concourse.bass_utils import run_bass_kernel_spmd

F32 = mybir.dt.float32
BF16 = mybir.dt.bfloat16
AF = mybir.ActivationFunctionType
ALU = mybir.AluOpType
AX = mybir.AxisListType

S = 8192
DM = 1024
NQ = 2048
NT = S // 128
EPS = 1e-6
SLOPES = [2.0 ** (-8.0 * (h + 1) / 4) for h in range(4)]
WINS = (2, 4, 8, 16)
ENGS = ("pe", "act", "dve", "pool", "sp")
CAP = 256
SKIP_T = 64.0
NR = 32 * CAP + 2048


class Prog:
    def __init__(self, nc):
        self.nc = nc
        self.ops = {e: [] for e in ENGS}
        self.cnt = {}
        self.sem_names = []
        for e in ENGS:
            self._newsem(e)

    def _newsem(self, key):
        self.cnt[key] = 0
        self.sem_names.append(key)

    def op(self, eng, fn, deps=(), inc=True, dr=None):
        d = tuple(x for x in deps if x is not None)
        tok = None
        if inc:
            self.cnt[eng] += 1
            tok = (eng, self.cnt[eng])
        if dr is None:
            dr = eng in ("dve", "act", "pool")
        self.ops[eng].append((fn, d, (eng, 1) if inc else None, dr))
        return tok

    def dma(self, queue, fn, semkey, deps=()):
        if semkey not in self.cnt:
            self._newsem(semkey)
        self.cnt[semkey] += 16
        tok = (semkey, self.cnt[semkey])
        d = tuple(x for x in deps if x is not None)
        self.ops[queue].append((fn, d, (semkey, 16), False))
        return tok

    def wait(self, eng, deps):
        d = tuple(x for x in deps if x is not None)
        self.ops[eng].append((None, d, None, False))

    def emit(self):
        nc = self.nc
        with contextlib.ExitStack() as st:
            sems = {k: st.enter_context(nc.semaphore("s_" + k)) for k in self.sem_names}
            block = st.enter_context(nc.Block())
            prog = self

            def run(engname, eng):
                waited = {}
                for fn, deps, inc, dr in prog.ops[engname]:
                    for (k, v) in deps:
                        if k == engname:
                            continue
                        if waited.get(k, 0) < v:
                            eng.wait_ge(sems[k], v)
                            waited[k] = v
                    if fn is None:
                        continue
                    if dr:
                        eng.drain()
                    ins = fn(eng)
                    if inc is not None:
                        ins.then_inc(sems[inc[0]], inc[1])

            @block.tensor
            def _(e):
                run("pe", e)

            @block.scalar
            def _(e):
                run("act", e)

            @block.vector
            def _(e):
                run("dve", e)

            @block.gpsimd
            def _(e):
                run("pool", e)

            @block.sync
            def _(e):
                run("sp", e)


class Buf:
    def __init__(self):
        self.w = None
        self.rs = {}

    def rd(self):
        return [self.w]

    def wr(self):
        return [self.w] + list(self.rs.values())

    def did_r(self, tok):
        if tok is None:
            return
        k, v = tok
        if k not in self.rs or self.rs[k][1] < v:
            self.rs[k] = tok

    def did_w(self, tok):
        if tok is None:
            return
        self.w = tok
        self.rs = {}


def build(stage=99, dbg=False, noattn=False):
    nc = bass.Bass("TRN2", target_bir_lowering=False)

    def DI(name, shape, dt=F32):
        return nc.dram_tensor(name, list(shape), dt, kind="ExternalInput").ap()

    xr = DI("xr", [S, DM])
    w_in = DI("w_in", [DM, 2048])
    w_out = DI("w_out", [DM, DM])
    w_pool = DI("w_pool", [4, 128, 128])
    w_gate = DI("w_gate", [32, DM, 512])
    w_up = DI("w_up", [32, DM, 512])
    w_down = DI("w_down", [32, 512, DM])
    wr_d = DI("wr", [DM, 36])
    vecs = DI("vecs", [3, DM])
    rbias = DI("rbias", [36])
    lams = DI("lams", [4, 64])
    subg = DI("subg", [128])
    pscale = DI("pscale", [128, 4])
    ident = DI("ident", [128, 128])
    kb_d = DI("kb", [128, 3 * 4 * 64])
    csg_d = DI("csg", [2, 4 * 66])
    qpos_d = DI("qpos", [2, NQ])
    dband_d = DI("dband", [128, 2 * 4 * 512])
    sid_d = DI("sid", [128, 4 * 128])
    hv_d = DI("hv", [128, 16])
    rcorr_d = DI("rcorr", [128, 4 * 16])
    ltri_d = DI("ltri", [128, 128])
    eoff_d = DI("eoff", [128, 512])
    out = nc.dram_tensor("out", [NQ, DM], F32, kind="ExternalOutput").ap()
    Xbuf = nc.dram_tensor("Xbuf", [NR, DM], BF16).ap()
    Ybuf = nc.dram_tensor("Ybuf", [NR, DM], F32).ap()
    XTs = nc.dram_tensor("XTs", [16, 128, 4096], BF16).ap()
    Wg_bf = nc.dram_tensor("Wg_bf", [32, DM, 512], BF16).ap()
    Wu_bf = nc.dram_tensor("Wu_bf", [32, DM, 512], BF16).ap()
    Wd_bf = nc.dram_tensor("Wd_bf", [32, 512, DM], BF16).ap()
    dbg_outs = {}

    with contextlib.ExitStack() as st:
        def sb(name, shape, dt):
            return st.enter_context(nc.sbuf_tensor("sb_" + name, list(shape), dt))

        def psb(name, shape, dt):
            return st.enter_context(nc.psum_tensor("ps_" + name, list(shape), dt))

        P = Prog(nc)

        X1 = sb("X1", [128, 37632], BF16)
        X2 = sb("X2", [128, 24576], BF16)
        X3 = sb("X3", [128, 16384], BF16)
        kb = sb("kb", [128, 3, 4, 64], F32)
        csg = sb("csg", [34, 4, 66], BF16)
        qpos = sb("qpos", [34, NQ], BF16)
        dband = sb("dband", [128, 2, 4, 512], BF16)
        sid = sb("sid", [128, 4, 128], BF16)
        idb = sb("idb", [128, 128], BF16)
        hv = sb("hv", [128, 16], F32)
        rcorr = sb("rcorr", [128, 4, 16], F32)
        gvb = sb("gvb", [128, 3, DM], F32)
        rbb = sb("rbb", [128, 36], F32)
        lamb = sb("lamb", [128, 4, 64], F32)
        sg8 = sb("sg8", [128, 128], F32)
        psc = sb("psc", [128, 4], F32)
        wpb = sb("wpb", [128, 4, 128], BF16)
        wrtb = sb("wrtb", [128, 8, 36], BF16)
        epsb = sb("epsb", [128, 1], F32)
        small = sb("small", [128, 64], F32)
        junk = sb("junk", [128, DM], BF16)
        PTb = [sb(f"PT{i}", [128, 512], BF16) for i in range(4)]
        I32 = mybir.dt.int32
        ltri = sb("ltri", [128, 128], BF16)
        onesb = sb("onesb", [128, 128], BF16)
        eoff = sb("eoff", [128, 512], F32)
        Mb_all = sb("Mb_all", [128, 16, 32], BF16)
        idxAll = sb("idxAll", [128, 16, 2], I32)
        gatesAll = sb("gatesAll", [128, 16, 2], F32)
        zt = sb("zt", [128, 1024], BF16)
        RW = sb("RW", [128, 2304], F32)
        tmpA = RW[:, 0:1024].rearrange("p (a b) -> p a b", b=512)
        accS = RW[:, 1024:2304].rearrange("p (b c) -> p b c", c=320)
        L_all = RW[:, 0:576].rearrange("p (t k) -> p t k", k=36)
        gmask = RW[:, 576:640].rearrange("p (t k) -> p t k", k=4)
        gdiff = RW[:, 640:704].rearrange("p (t k) -> p t k", k=4)
        esel = RW[:, 704:832].rearrange("p (t k) -> p t k", k=8)
        mask1 = RW[:, 832:960].rearrange("p (t k) -> p t k", k=8)
        e2 = RW[:, 960:1088].rearrange("p (t k) -> p t k", k=8)
        mask2 = RW[:, 1088:1216].rearrange("p (t k) -> p t k", k=8)
        msum = RW[:, 1216:1344].rearrange("p (t k) -> p t k", k=8)
        Rr = RW[:, 1344:1472].rearrange("p (t k) -> p t k", k=8)
        tmp32 = RW[:, 1472:1984].rearrange("p (t k) -> p t k", k=32)
        V_gmax, V_gsum, V_m1, V_m2, V_ex, V_den = [RW[:, 1984 + 16 * q: 2000 + 16 * q] for q in range(6)]
        slf = RW[:, 2080:2112].rearrange("p (t k) -> p t k", k=2)
        ss_t = sb("ss_t", [128, 4], F32)
        rs_t = sb("rs_t", [128, 4], F32)

        banks = [psb(f"bank{i}", [128, 512], F32) for i in range(8)]
        bankB = [Buf() for _ in range(8)]

        a_tok = X3[:, 0:8192].rearrange("p (t c) -> p t c", c=512)
        pT = X3[:, 8192:16384].rearrange("p (g t) -> p g t", t=NQ)
        xn2T = X3[:, :].rearrange("p (c t) -> p c t", t=NQ)
        KT = X1[:, 0:16384].rearrange("p (h t) -> p h t", t=S)
        VA = X1[:, 16384:16384 + 64 * 2 * 130].rearrange("p (m h e) -> p m h e", h=2, e=130)
        QT = X1[:, 33024:33024 + 4096].rearrange("p (h t) -> p h t", t=NQ)
        UT = X1[:, 0:4 * 2064 * 2].bitcast(F32).rearrange("p (g t) -> p g t", t=2064)
        Sa = X1[:, 16512:16512 + 4128].bitcast(F32)
        Sb = X1[:, 20640:20640 + 4128].bitcast(F32)
        dpool = X1[:, 24768:24768 + NQ]
        h1 = X1[:, 0:32768].bitcast(F32).rearrange("p (t d) -> p t d", d=DM)
        Wk2 = X2[:, 0:2048].rearrange("p (c n) -> p c n", n=256)
        Wv2 = X2[:, 2048:4096].rearrange("p (c n) -> p c n", n=256)
        Wq2 = X2[:, 4096:6144].rearrange("p (c n) -> p c n", n=256)
        Wu4 = X2[:, 0:4096].rearrange("p (c n) -> p c n", n=512)
        xtb = [X2[:, 6144 + i * 2048: 6144 + (i + 1) * 2048].bitcast(F32) for i in range(3)]
        xsb = [X2[:, 12288 + i * 1024: 12288 + (i + 1) * 1024] for i in range(3)]
        xTc = [X2[:, 15360 + i * 4096: 15360 + (i + 1) * 4096].rearrange("p (c t) -> p c t", t=512) for i in range(2)]
        xTc_flat = [X2[:, 15360 + i * 4096: 15360 + (i + 1) * 4096] for i in range(2)]
        Wo = X2[:, 12288:20480].rearrange("p (c n) -> p c n", n=DM)
        aTt = [X2[:, 20480 + i * 512: 20480 + (i + 1) * 512].rearrange("p (c t) -> p c t", t=128) for i in range(2)]
        xnAll = X3[:, :].rearrange("p (t d) -> p t d", d=DM)
        xnT2 = [X1[:, 32768 + i * 1024: 32768 + (i + 1) * 1024].rearrange("p (c t) -> p c t", t=128) for i in range(2)]
        XeT = [X3[:, 2048 + i * 2048: 2048 + (i + 1) * 2048].rearrange("p (c t) -> p c t", t=256) for i in range(2)]
        xe = [X3[:, 6144 + i * 2048: 6144 + (i + 1) * 2048].rearrange("p (s d) -> p s d", d=DM) for i in range(2)]
        ye = [X3[:, 10240 + i * 2048: 10240 + (i + 1) * 2048].bitcast(F32) for i in range(2)]
        HT = [X3[:, 14336 + i * 1024: 14336 + (i + 1) * 1024].rearrange("p (c t) -> p c t", t=256) for i in range(2)]
        gb = [X2[:, i * 2048:(i + 1) * 2048].bitcast(F32) for i in range(8)]
        wgb = [X2[:, i * 12288: i * 12288 + 4096].rearrange("p (c f) -> p c f", f=512) for i in range(2)]
        wub = [X2[:, i * 12288 + 4096: i * 12288 + 8192].rearrange("p (c f) -> p c f", f=512) for i in range(2)]
        wdb = [X2[:, i * 12288 + 8192: i * 12288 + 12288].rearrange("p (c n) -> p c n", n=DM) for i in range(2)]
        HTb = [X1[:, 32768 + i * 2048: 32768 + (i + 1) * 2048].rearrange("p (c t) -> p c t", t=512) for i in range(2)]

        B = {}

        def buf(name):
            if name not in B:
                B[name] = Buf()
            return B[name]

        def ld(queue, dst, src, name):
            t = P.dma(queue, lambda e: e.dma_start(out=dst, in_=src), "ld_" + name)
            buf(name).did_w(t)
            return t

        ld("pool", idb[:], ident[:, :], "idb")
        ld("sp", gvb[:, 0, :], vecs[0, :].partition_broadcast(128), "gv0")

        def late_loads():
            ld("sp", hv[:], hv_d[:, :], "hv")
            ld("sp", rcorr[:].rearrange("p a b -> p (a b)"), rcorr_d[:, :], "rcorr")
            ld("sp", psc[:], pscale[:, :], "psc")
            ld("pool", wpb[:], w_pool.rearrange("g c d -> c g d"), "wpb")
            ld("sp", kb[:].rearrange("p a b c -> p (a b c)"), kb_d[:, :], "kb")
            ld("pool", csg[0:2].rearrange("p a b -> p (a b)"), csg_d[:, :], "csg")
            ld("pool", qpos[0:2], qpos_d[:, :], "qpos")
            ld("pool", csg[32:34].rearrange("p a b -> p (a b)"), csg_d[:, :], "csg2")
            ld("pool", qpos[32:34], qpos_d[:, :], "qpos2")
            ld("pool", dband[:].rearrange("p a b c -> p (a b c)"), dband_d[:, :], "dband")
            ld("pool", sid[:].rearrange("p a b -> p (a b)"), sid_d[:, :], "sid")
            for i in range(1, 3):
                ld("sp", gvb[:, i, :], vecs[i, :].partition_broadcast(128), f"gv{i}")
            ld("sp", rbb[:], rbias.partition_broadcast(128), "rbb")
            ld("sp", lamb[:].rearrange("p a b -> p (a b)"), lams.rearrange("a b -> (a b)").partition_broadcast(128), "lamb")
            ld("sp", sg8[:], subg.partition_broadcast(128), "sg8")
            ld("pool", wrtb[:], wr_d.rearrange("(c p) n -> p c n", p=128), "wrtb")
            ld("pool", ltri[:], ltri_d[:, :], "ltri")
            ld("sp", eoff[:], eoff_d[:, :], "eoff")

        t = P.op("dve", lambda e: e.memset(epsb[:], EPS))
        buf("epsb").did_w(t)
        tz0 = P.op("dve", lambda e: e.memset(zt[:], 0.0))

        def zero_fill():
            tzf = None
            for r0 in range(0, NR, 128):
                tzf = P.dma("pool", (lambda r0: lambda e: e.dma_start(out=Xbuf[r0:r0 + 128, :], in_=zt[:]))(r0), "zf", [tz0])
            for r0 in range(32 * CAP, NR, 128):
                for h0 in (0, 512):
                    tzf = P.dma("pool", (lambda r0, h0: lambda e: e.dma_start(out=Ybuf[r0:r0 + 128, h0:h0 + 512], in_=zt[:].bitcast(F32)))(r0, h0), "zf", [tz0])
            buf("zf").did_w(tzf)

        pc_last = {}

        def precast_weights(lo, hi, deps):
            jobs = []
            for ex in range(lo, hi):
                for r0 in (0, 512):
                    jobs.append((Wg_bf[ex, r0:r0 + 512, :], w_gate[ex, r0:r0 + 512, :]))
                    jobs.append((Wu_bf[ex, r0:r0 + 512, :], w_up[ex, r0:r0 + 512, :]))
                for r0 in (0, 256):
                    jobs.append((Wd_bf[ex, r0:r0 + 256, :], w_down[ex, r0:r0 + 256, :]))
            for n_, (dst_, src_) in enumerate(jobs):
                key = f"pc{n_ % 2}"
                prev = [pc_last[key]] if key in pc_last else []
                pc_last[key] = P.dma("pool", (lambda dst_, src_: lambda e: e.dma_start(out=dst_, in_=src_))(dst_, src_), key, list(deps) + prev)
        def late_consts():
            t = P.op("dve", lambda e: e.tensor_scalar(out=sg8[:], in0=sg8[:], scalar1=0.8, scalar2=None, op0=ALU.mult),
                     buf("sg8").rd())
            buf("sg8").did_w(t)
            t = P.op("dve", lambda e: e.tensor_tensor(out=lamb[:, 0, :], in0=lamb[:, 0, :], in1=lamb[:, 1, :], op=ALU.mult),
                     buf("lamb").rd())
            t = P.op("dve", lambda e: e.tensor_tensor(out=lamb[:, 2, :], in0=lamb[:, 2, :], in1=lamb[:, 3, :], op=ALU.mult))
            t = P.op("dve", lambda e: e.reduce_sum(out=small[:, 1:2], in_=lamb[:, 0, :], axis=AX.X))
            t = P.op("dve", lambda e: e.reduce_sum(out=small[:, 2:3], in_=lamb[:, 2, :], axis=AX.X))
            t = P.op("act", lambda e: e.activation(out=small[:, 1:3], in_=small[:, 1:3], func=AF.Exp), [t])
            t = P.op("dve", lambda e: e.tensor_tensor(out=small[:, 0:1], in0=small[:, 2:3], in1=small[:, 1:2], op=ALU.subtract), [t])
            t = P.op("dve", lambda e: e.tensor_scalar(out=small[:, 0:1], in0=small[:, 0:1], scalar1=-0.2, scalar2=None, op0=ALU.add))
            buf("neglam").did_w(t)

        fe_cnt = [0]
        pTr_banks = [6, 7]

        def fe_A(m, gidx=0):
            i = fe_cnt[0] % 3
            ib = fe_cnt[0] % 2
            fe_cnt[0] += 1
            bx, bs = buf(f"xt{i}"), buf(f"xs{i}")
            tx = P.dma("sp", lambda e: e.dma_start(out=xtb[i], in_=xr[m * 128:(m + 1) * 128, :]), f"ld_xt{i}", bx.wr())
            bx.did_w(tx)
            col = (fe_cnt[0] % 4)
            t1 = P.op("act", lambda e: e.activation(out=junk[:], in_=xtb[i], func=AF.Square, accum_out=ss_t[:, col:col + 1]),
                      bx.rd() + buf("junk").wr() + buf(f"ss{col}").wr(), dr=False)
            buf("junk").did_w(t1)
            bx.did_r(t1)
            t2 = P.op("act", lambda e: e.activation(out=rs_t[:, col:col + 1], in_=ss_t[:, col:col + 1], func=AF.Sqrt,
                                                    bias=epsb[:, 0:1], scale=1.0 / DM), buf("epsb").rd() + buf(f"rs{col}").wr())
            t3 = P.op("dve", lambda e: e.reciprocal(out=rs_t[:, col:col + 1], in_=rs_t[:, col:col + 1]), [t2], dr=False)
            buf(f"ss{col}").did_r(t2)
            t4 = P.op("dve", lambda e: e.scalar_tensor_tensor(out=xsb[i], in0=xtb[i], scalar=rs_t[:, col:col + 1],
                                                               in1=gvb[:, gidx, :], op0=ALU.mult, op1=ALU.mult),
                      bx.rd() + bs.wr() + buf(f"gv{gidx}").rd())
            buf(f"ss{col}").did_w(t1)
            buf(f"rs{col}").did_w(t3)
            buf(f"rs{col}").did_r(t4)
            bx.did_r(t4)
            bs.did_w(t4)
            return (i, ib)

        def fe_B(ctx, dst, dstB):
            i, ib = ctx
            bs = buf(f"xs{i}")
            bk = pTr_banks[ib]
            pv = banks[bk][:].bitcast(BF16).rearrange("p (c t) -> p c t", t=128)
            tl = None
            for c in range(8):
                tl = P.op("pe", (lambda c: lambda e: e.transpose(out=pv[:, c, :], in_=xsb[i][:, c * 128:(c + 1) * 128],
                                                                 identity=idb[:]))(c),
                          bs.rd() + bankB[bk].wr() + buf("idb").rd(), inc=(c == 7))
            bs.did_r(tl)
            bankB[bk].did_w(tl)
            t5 = P.op("act", lambda e: e.copy(out=dst, in_=pv), bankB[bk].rd() + dstB.wr(), dr=False)
            bankB[bk].did_r(t5)
            dstB.did_w(t5)
            return t5

        def front_end(m, dst, dstB, gidx=0):
            return fe_B(fe_A(m, gidx), dst, dstB)

        if stage >= 1:
            tw = P.dma("pool", lambda e: e.dma_start(out=Wu4, in_=w_in[:, 1536:2048].rearrange("(c p) n -> p c n", p=128)), "ld_w0")
            buf("W").did_w(tw)
            tiles = [63] + list(range(16)) + [16]
            pk = 0
            ctxP = {0: fe_A(tiles[0]), 1: fe_A(tiles[1])}
            late_loads()
            for idx, m in enumerate(tiles):
                i = idx % 2
                dst = xTc[i][:, :, 0:128]
                dB = buf(f"xTc{i}")
                if idx + 2 < len(tiles):
                    ctxP[idx + 2] = fe_A(tiles[idx + 2])
                t5p = fe_B(ctxP[idx], dst, dB)
                if 1 <= idx <= 16 and stage >= 2:
                    tsx = P.dma("pool", (lambda m, i: lambda e: e.dma_start(
                        out=XTs[m // 4].rearrange("p (c t) -> p c t", t=512)[:, :, (m % 4) * 128:(m % 4 + 1) * 128],
                        in_=xTc[i][:, :, 0:128]))(m, i), "st_xts", [t5p])
                    dB.did_r(tsx)
                    buf("xts").did_w(tsx)
                if idx == 0:
                    c0, n, uo = 120, 8, 0
                elif idx == 17:
                    c0, n, uo = 0, 8, 8 + NQ
                else:
                    c0, n, uo = 0, 128, 8 + (idx - 1) * 128
                bk = pk % 4
                pk += 1
                pu = banks[bk][:].rearrange("p (g t) -> p g t", t=128)
                tl = None
                for g in range(4):
                    for c in range(8):
                        tl = P.op("pe", (lambda g, c, c0, n, i, pu: lambda e: e.matmul(
                            pu[:, g, 0:n], lhsT=Wu4[:, c, g * 128:(g + 1) * 128], rhs=xTc[i][:, c, c0:c0 + n],
                            start=(c == 0), stop=(c == 7)))(g, c, c0, n, i, pu),
                            dB.rd() + buf("W").rd() + bankB[bk].wr(), inc=(g == 3 and c == 7))
                dB.did_r(tl)
                bankB[bk].did_w(tl)
                t = P.op("dve", (lambda n, uo, pu: lambda e: e.tensor_copy(out=UT[:, :, uo:uo + n], in_=pu[:, :, 0:n]))(n, uo, pu),
                         bankB[bk].rd() + buf("UT").wr(), dr=False)
                bankB[bk].did_r(t)
                buf("UTw").did_w(t)
            buf("W").did_r(tl)
            late_consts()
            for g in range(4):
                t = P.op("dve", (lambda g: lambda e: e.tensor_tensor(out=UT[:, g, 0:8], in0=UT[:, g, 0:8], in1=hv[:, 0:8], op=ALU.mult))(g),
                         buf("hv").rd() + buf("UTw").rd())
                t = P.op("dve", (lambda g: lambda e: e.tensor_tensor(out=UT[:, g, 8 + NQ:16 + NQ], in0=UT[:, g, 8 + NQ:16 + NQ],
                                                                     in1=hv[:, 8:16], op=ALU.mult))(g))
            L = 2064
            for g in range(4):
                w = WINS[g]
                U = UT[:, g, :]
                t = P.op("dve", (lambda U: lambda e: e.tensor_tensor(out=Sa[:, 1:L], in0=U[:, 0:L - 1], in1=U[:, 1:L], op=ALU.add))(U),
                         buf("dpool").rd())
                cur, oth = Sa, Sb
                if w >= 4:
                    t = P.op("dve", (lambda cur, oth: lambda e: e.tensor_tensor(out=oth[:, 2:L - 1], in0=cur[:, 1:L - 2], in1=cur[:, 3:L], op=ALU.add))(cur, oth))
                    cur, oth = oth, cur
                if w >= 8:
                    t = P.op("dve", (lambda cur, oth: lambda e: e.tensor_tensor(out=oth[:, 4:L - 3], in0=cur[:, 2:L - 5], in1=cur[:, 6:L - 1], op=ALU.add))(cur, oth))
                    cur, oth = oth, cur
                if w >= 16:
                    t = P.op("dve", (lambda cur, oth: lambda e: e.tensor_tensor(out=oth[:, 8:L - 7], in0=cur[:, 4:L - 11], in1=cur[:, 12:L - 3], op=ALU.add))(cur, oth))
                    cur, oth = oth, cur
                t = P.op("dve", (lambda cur, U, w: lambda e: e.scalar_tensor_tensor(
                    out=dpool, in0=cur[:, 8:8 + NQ], scalar=1.0 / w, in1=U[:, 8:8 + NQ], op0=ALU.mult, op1=ALU.subtract))(cur, U, w),
                    buf("dpool").wr())
                for (lo, ro) in ((0, 0), (NQ - 8, 8)):
                    t = P.op("dve", (lambda cur, oth, g, lo, ro: lambda e: e.tensor_tensor(
                        out=oth[:, 0:8], in0=cur[:, 8 + lo:16 + lo], in1=rcorr[:, g, ro:ro + 8], op=ALU.mult))(cur, oth, g, lo, ro),
                        buf("rcorr").rd())
                    t = P.op("dve", (lambda U, oth, lo: lambda e: e.tensor_tensor(
                        out=dpool[:, lo:lo + 8], in0=oth[:, 0:8], in1=U[:, 8 + lo:16 + lo], op=ALU.subtract))(U, oth, lo))
                buf("dpool").did_w(t)
                for ch in range(4):
                    bk = pk % 4
                    pk += 1
                    tm = P.op("pe", (lambda g, ch, bk: lambda e: e.matmul(banks[bk][:], lhsT=wpb[:, g, :], rhs=dpool[:, ch * 512:(ch + 1) * 512],
                                                                          start=True, stop=True))(g, ch, bk),
                              buf("dpool").rd() + buf("wpb").rd() + bankB[bk].wr())
                    bankB[bk].did_w(tm)
                    buf("dpool").did_r(tm)
                    t = P.op("act", (lambda g, ch, bk: lambda e: e.activation(out=pT[:, g, ch * 512:(ch + 1) * 512], in_=banks[bk][:],
                                                                              func=AF.Copy, scale=psc[:, g:g + 1]))(g, ch, bk),
                             bankB[bk].rd() + buf("psc").rd(), dr=False)
                    bankB[bk].did_r(t)
                    buf("pT").did_w(t)
            buf("X1").did_r(tm)
            buf("X1").did_r(t)

        acc_loc = {}
        k = 0
        for c in range(2):
            for qt in range(4):
                acc_loc[(c, qt)] = (4 + k // 2, (k % 2) * 160)
                k += 1
        sc_cnt = [0]
        pt_cnt = [0]

        att_state = {"zeroed": False, "deferred": None}

        def attention(hp):
            att_state["zeroed"] = False
            for hh in range(2):
                h = 2 * hp + hh
                for j in range(4):
                    pend = None
                    if not att_state["zeroed"]:
                        for bk_ in (4, 5, 6, 7):
                            tz = P.op("dve", (lambda bk_: lambda e: e.memset(banks[bk_][:, 0:320], 0.0))(bk_), bankB[bk_].wr(), dr=False)
                            bankB[bk_].did_w(tz)
                    att_state["zeroed"] = False
                    sl_ = SLOPES[h]
                    mlist = []
                    for m_ in range(NT):
                        if m_ < 16:
                            if m_ < 4 * j:
                                dmin = 512 * j - (128 * m_ + 127)
                            elif m_ > 4 * j + 3:
                                dmin = 128 * m_ - (512 * j + 511)
                            else:
                                dmin = 0
                        else:
                            dmin = min(128 * m_ - (512 * j + 511), 512 * j + S - (128 * m_ + 127))
                        if sl_ * dmin >= SKIP_T:
                            continue
                        mlist.append(m_)
                    for mi in range(len(mlist) + 1):
                        if mi == 3 and att_state["deferred"] is not None:
                            att_state["deferred"]()
                            att_state["deferred"] = None
                        cur = None
                        m = mlist[mi] if mi < len(mlist) else NT
                        if mi < len(mlist):
                            if m < 16:
                                i = m - 4 * j
                                if i < 0:
                                    kind, col, band = 0, 65, None
                                elif i > 3:
                                    kind, col, band = 1, 64, None
                                else:
                                    kind, col, band = None, None, i
                            else:
                                kind, col, band = 2, m, None
                            cur = []
                            bks = []
                            for c in range(2):
                                bk = sc_cnt[0] % 4
                                sc_cnt[0] += 1
                                bks.append(bk)
                                P.op("pe", (lambda c, hh, m, j, bk: lambda e: e.matmul(
                                    banks[bk][:], lhsT=KT[64 * c:64 * c + 64, hh, m * 128:(m + 1) * 128],
                                    rhs=QT[64 * c:64 * c + 64, hh, j * 512:(j + 1) * 512], start=True, stop=False))(c, hh, m, j, bk),
                                    bankB[bk].wr() + buf("KT").rd() + buf("QT").rd(), inc=False)
                            tqs = []
                            for c in range(2):
                                bk = bks[c]
                                if band is None:
                                    p0 = 32 * c
                                    tq = P.op("pe", (lambda h, col, j, bk, p0: lambda e: e.matmul(
                                        banks[bk][:], lhsT=csg[p0:p0 + 2, h, col:col + 1].to_broadcast([2, 128]),
                                        rhs=qpos[p0:p0 + 2, j * 512:(j + 1) * 512], start=False, stop=True))(h, col, j, bk, p0),
                                        buf("csg").rd() + buf("qpos").rd() + buf("csg2").rd() + buf("qpos2").rd())
                                else:
                                    P.op("pe", (lambda h, band, bk: lambda e: e.matmul(
                                        banks[bk][:], lhsT=sid[:, h, :], rhs=dband[:, 0, band, :], start=False, stop=False))(h, band, bk),
                                        buf("sid").rd() + buf("dband").rd(), inc=False)
                                    tq = P.op("pe", (lambda h, band, bk: lambda e: e.matmul(
                                        banks[bk][:], lhsT=sid[:, h, :], rhs=dband[:, 1, band, :], start=False, stop=True))(h, band, bk))
                                bankB[bk].did_w(tq)
                            for c in range(2):
                                bk = bks[c]
                                pi = pt_cnt[0] % 4
                                pt_cnt[0] += 1
                                pb = buf(f"PT{pi}")
                                if kind is None:
                                    te = P.op("act", (lambda bk, pi: lambda e: e.activation(
                                        out=PTb[pi][:], in_=banks[bk][:], func=AF.Exp, scale=0.125))(bk, pi),
                                        bankB[bk].rd() + pb.wr(), dr=False)
                                else:
                                    te = P.op("act", (lambda bk, pi, kind, h, m: lambda e: e.activation(
                                        out=PTb[pi][:], in_=banks[bk][:], func=AF.Exp, bias=kb[:, kind, h, m:m + 1], scale=0.125))(bk, pi, kind, h, m),
                                        bankB[bk].rd() + pb.wr() + buf("kb").rd(), dr=False)
                                bankB[bk].did_r(te)
                                pb.did_w(te)
                                cur.append((c, pi, m, mi == 0, mi == len(mlist) - 1))
                        if pend is not None:
                            for (c, pi, mm_, first_, last_) in pend:
                                pb = buf(f"PT{pi}")
                                tl = None
                                for qt in range(4):
                                    bk, off = acc_loc[(c, qt)]
                                    tl = P.op("pe", (lambda pi, qt, bk, off, mm_, hh, last_: lambda e: e.matmul(
                                        banks[bk][:, off:off + 129], lhsT=PTb[pi][:, qt * 128:(qt + 1) * 128],
                                        rhs=VA[:, mm_, hh, 0:129], start=False, stop=last_,
                                        skip_group_check=True))(pi, qt, bk, off, mm_, hh, last_),
                                        pb.rd() + buf("VA").rd() + (bankB[bk].wr() if first_ else []), inc=(qt == 3))
                                pb.did_r(tl)
                                if last_:
                                    for bk in (4, 5, 6, 7):
                                        bankB[bk].did_w(tl)
                        pend = cur
                    if att_state["deferred"] is not None:
                        att_state["deferred"]()
                        att_state["deferred"] = None
                    cps = []
                    for bi, bk in enumerate((4, 5, 6, 7)):
                        eng_ = "act" if bi % 2 == 0 else "dve"
                        if eng_ == "act":
                            tcp = P.op("act", (lambda bi, bk: lambda e: e.copy(out=accS[:, bi, 0:289], in_=banks[bk][:, 0:289]))(bi, bk),
                                       bankB[bk].rd() + buf("accS").wr(), dr=False)
                        else:
                            tcp = P.op("dve", (lambda bi, bk: lambda e: e.tensor_copy(out=accS[:, bi, 0:289], in_=banks[bk][:, 0:289]))(bi, bk),
                                       bankB[bk].rd() + buf("accS").wr(), dr=False)
                        bankB[bk].did_r(tcp)
                        cps.append(tcp)
                    for bk_ in (4, 5, 6, 7):
                        tz = P.op("dve", (lambda bk_: lambda e: e.memset(banks[bk_][:, 0:320], 0.0))(bk_), bankB[bk_].wr(), dr=False)
                        bankB[bk_].did_w(tz)
                    att_state["zeroed"] = True
                    td1 = None
                    for qt in range(4):
                        b0, o0 = acc_loc[(0, qt)]
                        b1, o1 = acc_loc[(1, qt)]
                        sc0 = 16 + 8 * qt
                        P.op("dve", (lambda b0, o0, sc0: lambda e: e.reciprocal(out=small[:, sc0:sc0 + 1], in_=accS[:, b0 - 4, o0 + 128:o0 + 129]))(b0, o0, sc0),
                             cps + buf("nrm").wr() + buf("tmpA").wr())
                        P.op("dve", (lambda b1, o1, sc0: lambda e: e.reciprocal(out=small[:, sc0 + 1:sc0 + 2], in_=accS[:, b1 - 4, o1 + 128:o1 + 129]))(b1, o1, sc0))
                        P.op("dve", (lambda sc0: lambda e: e.tensor_tensor(out=small[:, sc0 + 2:sc0 + 3], in0=small[:, sc0 + 1:sc0 + 2], in1=small[:, 0:1], op=ALU.mult))(sc0),
                             buf("neglam").rd())
                        P.op("dve", (lambda b0, o0, sc0, qt: lambda e: e.tensor_scalar(out=tmpA[:, 0, qt * 128:(qt + 1) * 128], in0=accS[:, b0 - 4, o0:o0 + 128],
                                                                                         scalar1=small[:, sc0:sc0 + 1], scalar2=None, op0=ALU.mult))(b0, o0, sc0, qt))
                        td1 = P.op("dve", (lambda b1, o1, sc0, qt: lambda e: e.scalar_tensor_tensor(
                            out=tmpA[:, 0, qt * 128:(qt + 1) * 128], in0=accS[:, b1 - 4, o1:o1 + 128], scalar=small[:, sc0 + 2:sc0 + 3],
                            in1=tmpA[:, 0, qt * 128:(qt + 1) * 128], op0=ALU.mult, op1=ALU.add))(b1, o1, sc0, qt))
                    buf("accS").did_w(cps[0])
                    buf("accS").did_r(cps[1])
                    buf("accS").did_r(cps[2])
                    buf("accS").did_r(cps[3])
                    buf("accS").did_r(td1)
                    buf("tmpA").did_w(td1)

                    def norm_tail(j=j, h=h, td1=td1):
                        ta1 = None
                        for qt in range(4):
                            sc0 = 16 + 8 * qt
                            P.op("act", (lambda qt, sc0: lambda e: e.activation(out=tmpA[:, 1, 0:128], in_=tmpA[:, 0, qt * 128:(qt + 1) * 128], func=AF.Square,
                                                                                accum_out=small[:, sc0 + 3:sc0 + 4]))(qt, sc0), [td1], dr=False)
                            ta1 = P.op("act", (lambda sc0: lambda e: e.activation(out=small[:, sc0 + 4:sc0 + 5], in_=small[:, sc0 + 3:sc0 + 4], func=AF.Sqrt,
                                                                                  bias=epsb[:, 0:1], scale=1.0 / 128))(sc0))
                        t4 = None
                        for qt in range(4):
                            sc0 = 16 + 8 * qt
                            P.op("dve", (lambda sc0: lambda e: e.reciprocal(out=small[:, sc0 + 4:sc0 + 5], in_=small[:, sc0 + 4:sc0 + 5]))(sc0), [ta1])
                            t4 = P.op("dve", (lambda j, qt, h, sc0: lambda e: e.scalar_tensor_tensor(
                                out=a_tok[:, 4 * j + qt, h * 128:(h + 1) * 128], in0=tmpA[:, 0, qt * 128:(qt + 1) * 128], scalar=small[:, sc0 + 4:sc0 + 5],
                                in1=sg8[:], op0=ALU.mult, op1=ALU.mult))(j, qt, h, sc0), buf("sg8").rd())
                        buf("tmpA").did_r(t4)
                        buf("tmpA").did_r(ta1)
                        buf("nrm").did_r(t4)
                        buf("nrm").did_w(t4)
                        buf("a_tok").did_w(t4)

                    att_state["deferred"] = norm_tail
            if att_state["deferred"] is not None:
                att_state["deferred"]()
                att_state["deferred"] = None

        if noattn:
            t = P.op("dve", lambda e: e.memset(X3[:, 0:8192], 0.0))
            buf("a_tok").did_w(t)
        if stage >= 2 and not noattn:
            for hp in range(2):
                dW = buf("W").wr() + buf("X1").wr()
                c0 = 2 * hp * 128
                tw = P.dma("pool", lambda e, c0=c0: e.dma_start(out=Wq2, in_=w_in[:, c0:c0 + 256].rearrange("(c p) n -> p c n", p=128)), "ld_w0", dW)
                tw = P.dma("pool", lambda e, c0=c0: e.dma_start(out=Wk2, in_=w_in[:, 512 + c0:512 + c0 + 256].rearrange("(c p) n -> p c n", p=128)), "ld_w0", dW)
                tw = P.dma("pool", lambda e, c0=c0: e.dma_start(out=Wv2, in_=w_in[:, 1024 + c0:1024 + c0 + 256].rearrange("(c p) n -> p c n", p=128)), "ld_w0", dW)
                buf("W").did_w(tw)
                t = P.op("dve", lambda e: e.memset(VA[:, :, :, 128:130], 1.0), buf("X1").wr() + buf("VA").wr() + buf("KT").wr())
                buf("VA").did_w(t)
                buf("KT").did_w(t)
                buf("QT").did_w(t)
                pk = 0
                last = None
                st_ = {"pk": 0, "last": None, "lastv": None, "tl": None}

                def proj_part(ch, part):
                    i = ch % 2
                    dB = buf(f"xTc{i}")
                    groups = [("k", 0), ("k", 1)] + ([("q", 0), ("q", 1)] if ch < 4 else [])
                    for gi_, (kind, hh) in enumerate(groups):
                        if gi_ % 4 != part:
                            continue
                        bk = st_["pk"] % 4
                        st_["pk"] += 1
                        Wt = Wk2 if kind == "k" else Wq2
                        tl = None
                        for c in range(8):
                            tl = P.op("pe", (lambda Wt, c, hh, i, bk: lambda e: e.matmul(
                                banks[bk][:], lhsT=Wt[:, c, hh * 128:(hh + 1) * 128], rhs=xTc[i][:, c, :],
                                start=(c == 0), stop=(c == 7)))(Wt, c, hh, i, bk),
                                dB.rd() + buf("W").rd() + bankB[bk].wr(), inc=(c == 7))
                        bankB[bk].did_w(tl)
                        dstT = KT if kind == "k" else QT
                        t = P.op("dve", (lambda dstT, hh, ch, bk: lambda e: e.tensor_copy(
                            out=dstT[:, hh, ch * 512:(ch + 1) * 512], in_=banks[bk][:]))(dstT, hh, ch, bk),
                            bankB[bk].rd() + buf("KT").rd(), dr=False)
                        bankB[bk].did_r(t)
                        st_["last"] = t
                        dB.did_r(tl)
                    tt = part
                    bk = st_["pk"] % 4
                    st_["pk"] += 1
                    m = 4 * ch + tt
                    tl = None
                    for c in range(8):
                        tl = P.op("pe", (lambda c, tt, i, bk: lambda e: e.matmul(
                            banks[bk][:, 0:256], lhsT=xTc[i][:, c, tt * 128:(tt + 1) * 128], rhs=Wv2[:, c, :],
                            start=(c == 0), stop=(c == 7)))(c, tt, i, bk),
                            dB.rd() + buf("W").rd() + bankB[bk].wr(), inc=(c == 7))
                    bankB[bk].did_w(tl)
                    t = P.op("act", (lambda m, bk: lambda e: e.copy(
                        out=VA[:, m, :, 0:128], in_=banks[bk][:, 0:256].rearrange("p (h e) -> p h e", e=128)))(m, bk),
                        bankB[bk].rd() + buf("VA").rd(), dr=False)
                    bankB[bk].did_r(t)
                    st_["lastv"] = t
                    st_["tl"] = tl
                    dB.did_r(tl)

                def load_chunk(ch_):
                    i2 = ch_ % 2
                    dB2 = buf(f"xTc{i2}")
                    tlx = P.dma("sp", (lambda ch_, i2: lambda e: e.dma_start(out=xTc_flat[i2], in_=XTs[ch_]))(ch_, i2), f"ld_xc{i2}",
                                dB2.wr() + buf("xts").rd())
                    dB2.did_w(tlx)

                if hp == 0:
                    load_chunk(0)
                    load_chunk(1)
                    ctxA = {16: fe_A(16), 17: fe_A(17)}

                    def feB_tile(n):
                        if n + 2 < 64:
                            ctxA[n + 2] = fe_A(n + 2)
                        i2 = (n // 4) % 2
                        dB2 = buf(f"xTc{i2}")
                        t5 = fe_B(ctxA.pop(n), xTc[i2][:, :, (n % 4) * 128:(n % 4 + 1) * 128], dB2)
                        if n % 4 == 3:
                            ch_ = n // 4
                            tsx = P.dma("pool", (lambda ch_, i2: lambda e: e.dma_start(out=XTs[ch_], in_=xTc_flat[i2]))(ch_, i2), "st_xts", [t5])
                            dB2.did_r(tsx)
                            buf("xts").did_w(tsx)

                    for ch in range(16):
                        for tt in range(4):
                            if 4 <= ch + 1 < 16:
                                feB_tile(4 * (ch + 1) + tt)
                            proj_part(ch, tt)
                        if ch + 2 < 4:
                            load_chunk(ch + 2)
                else:
                    load_chunk(0)
                    load_chunk(1)
                    for ch in range(16):
                        for tt in range(4):
                            proj_part(ch, tt)
                        if ch + 2 < 16:
                            load_chunk(ch + 2)
                tl, last, lastv = st_["tl"], st_["last"], st_["lastv"]
                buf("W").did_r(tl)
                buf("KT").did_w(last)
                buf("QT").did_w(last)
                buf("VA").did_w(lastv)
                if stage >= 6:
                    pdeps = [last, lastv]
                    if hp == 0:
                        zero_fill()
                        precast_weights(0, 8, pdeps)
                    else:
                        precast_weights(8, 32, pdeps)
                if stage >= 3:
                    attention(hp)
                    tk = (("pe", P.cnt["pe"]))
                    buf("X1").did_r(tk)
                    buf("KT").did_r(tk)
                    buf("VA").did_r(tk)
                    buf("QT").did_r(tk)

        if stage >= 4:
            dW = buf("W").wr() + [("pe", P.cnt["pe"])]
            for c_ in range(8):
                tw = P.dma("pool", (lambda c_: lambda e: e.dma_start(out=Wo[:, c_, :], in_=w_out[c_ * 128:(c_ + 1) * 128, :]))(c_), "ld_w0", dW)
            buf("W").did_w(tw)
            for t_ in range(16):
                i = t_ % 2
                bx = buf(f"xt{i}")
                tx = P.dma("sp", (lambda t_, i: lambda e: e.dma_start(out=xtb[i], in_=xr[t_ * 128:(t_ + 1) * 128, :]))(t_, i), f"ld_xt{i}", bx.wr())
                bx.did_w(tx)
                bk = 6 + i
                pv = banks[bk][:].bitcast(BF16).rearrange("p (c t) -> p c t", t=128)
                tl = None
                for c in range(4):
                    tl = P.op("pe", (lambda c, t_, pv: lambda e: e.transpose(out=pv[:, c, :], in_=a_tok[:, t_, c * 128:(c + 1) * 128], identity=idb[:]))(c, t_, pv),
                              buf("a_tok").rd() + bankB[bk].wr(), inc=(c == 3))
                bankB[bk].did_w(tl)
                ba = buf(f"aT{i}")
                t5 = P.op("act", (lambda i, pv: lambda e: e.copy(out=aTt[i], in_=pv[:, 0:4, :]))(i, pv), bankB[bk].rd() + ba.wr(), dr=False)
                bankB[bk].did_r(t5)
                ba.did_w(t5)
                for half in range(2):
                    bk2 = 2 * i + half
                    tl = None
                    for c in range(8):
                        lhs = aTt[i][:, c, :] if c < 4 else pT[:, c - 4, t_ * 128:(t_ + 1) * 128]
                        tl = P.op("pe", (lambda lhs, c, half, bk2: lambda e: e.matmul(
                            banks[bk2][:], lhsT=lhs, rhs=Wo[:, c, half * 512:(half + 1) * 512], start=(c == 0), stop=(c == 7)))(lhs, c, half, bk2),
                            ba.rd() + buf("pT").rd() + buf("W").rd() + bankB[bk2].wr(), inc=(c == 7))
                    bankB[bk2].did_w(tl)
                    t = P.op("dve", (lambda t_, half, bk2, i: lambda e: e.tensor_tensor(
                        out=h1[:, t_, half * 512:(half + 1) * 512], in0=banks[bk2][:], in1=xtb[i][:, half * 512:(half + 1) * 512], op=ALU.add))(t_, half, bk2, i),
                        bankB[bk2].rd() + bx.rd() + buf("X1").wr(), dr=False)
                    bankB[bk2].did_r(t)
                    bx.did_r(t)
                ba.did_r(tl)
                buf("h1").did_w(t)
            buf("W").did_r(tl)
            buf("X3").did_r(tl)

        def load_weights(ex):
            i = ex % 2
            bw = buf(f"we{i}")
            dW = bw.wr() + buf("W").wr() + buf("xt0").wr() + buf("xt1").wr() + buf("xt2").wr() + buf("xs0").wr() + buf("xs1").wr() + buf("xs2").wr()
            dW = dW + list(pc_last.values())
            tw = None
            for c0 in (0, 4):
                tw = P.dma("pool", (lambda ex, i, c0: lambda e: e.dma_start(
                    out=wgb[i][:, c0:c0 + 4, :], in_=Wg_bf[ex, c0 * 128:(c0 + 4) * 128, :].rearrange("(c p) f -> p c f", p=128)))(ex, i, c0), f"ld_we{i}", dW)
                tw = P.dma("pool", (lambda ex, i, c0: lambda e: e.dma_start(
                    out=wub[i][:, c0:c0 + 4, :], in_=Wu_bf[ex, c0 * 128:(c0 + 4) * 128, :].rearrange("(c p) f -> p c f", p=128)))(ex, i, c0), f"ld_we{i}", dW)
            for c0 in (0, 2):
                tw = P.dma("pool", (lambda ex, i, c0: lambda e: e.dma_start(
                    out=wdb[i][:, c0:c0 + 2, :], in_=Wd_bf[ex, c0 * 128:(c0 + 2) * 128, :].rearrange("(c p) n -> p c n", p=128)))(ex, i, c0), f"ld_we{i}", dW)
            bw.did_w(tw)

        if stage >= 5:
            if stage >= 6 and buf("zf").w is None:
                zero_fill()
                precast_weights(0, 32, [])
            if stage >= 6:
                load_weights(0)
                load_weights(1)
            t = P.op("dve", lambda e: e.memset(onesb[:], 1.0))
            buf("onesb").did_w(t)

            def prepA(t_):
                col = t_ % 4
                t1 = P.op("act", (lambda t_, col: lambda e: e.activation(out=junk[:], in_=h1[:, t_, :], func=AF.Square, accum_out=ss_t[:, col:col + 1]))(t_, col),
                          buf("h1").rd() + buf("junk").wr() + buf(f"ss{col}").wr(), dr=False)
                buf("junk").did_w(t1)
                t2 = P.op("act", (lambda col: lambda e: e.activation(out=rs_t[:, col:col + 1], in_=ss_t[:, col:col + 1], func=AF.Sqrt,
                                                                     bias=epsb[:, 0:1], scale=1.0 / DM))(col), buf(f"rs{col}").wr())
                t3 = P.op("dve", (lambda col: lambda e: e.reciprocal(out=rs_t[:, col:col + 1], in_=rs_t[:, col:col + 1]))(col), [t2], dr=False)
                buf(f"ss{col}").did_r(t2)
                t4 = P.op("dve", (lambda t_, col: lambda e: e.scalar_tensor_tensor(
                    out=xnAll[:, t_, :], in0=h1[:, t_, :], scalar=rs_t[:, col:col + 1], in1=gvb[:, 1, :], op0=ALU.mult, op1=ALU.mult))(t_, col),
                    buf("gv1").rd() + buf("X3").wr())
                buf(f"ss{col}").did_w(t1)
                buf(f"rs{col}").did_w(t3)
                buf(f"rs{col}").did_r(t4)
                buf(f"xn{t_}").did_w(t4)

            def prepB(t_):
                i = t_ % 2
                bn = buf(f"xn{t_}")
                bk = 0 + i
                pv = banks[bk][:].bitcast(BF16).rearrange("p (c t) -> p c t", t=128)
                tl = None
                for c in range(8):
                    tl = P.op("pe", (lambda c, pv, t_: lambda e: e.transpose(out=pv[:, c, :], in_=xnAll[:, t_, c * 128:(c + 1) * 128], identity=idb[:]))(c, pv, t_),
                              bn.rd() + bankB[bk].wr() + buf("idb").rd(), inc=(c == 7))
                bankB[bk].did_w(tl)
                bt = buf(f"xnT2{i}")
                ta = P.op("act", (lambda pv, i: lambda e: e.copy(out=xnT2[i], in_=pv))(pv, i),
                          bankB[bk].rd() + buf("X1").wr() + bt.wr(), dr=False)
                bankB[bk].did_r(ta)
                bt.did_w(ta)
                bk = 4 + i
                tl = None
                for c in range(8):
                    tl = P.op("pe", (lambda c, bk, i: lambda e: e.matmul(banks[bk][:, 0:36], lhsT=xnT2[i][:, c, :], rhs=wrtb[:, c, :],
                                                                         start=(c == 0), stop=(c == 7)))(c, bk, i),
                              bt.rd() + buf("wrtb").rd() + bankB[bk].wr(), inc=(c == 7))
                bt.did_r(tl)
                bankB[bk].did_w(tl)
                t = P.op("dve", (lambda bk, t_: lambda e: e.tensor_tensor(out=L_all[:, t_, :], in0=banks[bk][:, 0:36], in1=rbb[:], op=ALU.add))(bk, t_),
                         bankB[bk].rd() + buf("rbb").rd(), dr=False)
                bankB[bk].did_r(t)
                return t

            prepA(0)
            prepA(1)
            tL = None
            for t_ in range(16):
                if t_ + 2 < 16:
                    prepA(t_ + 2)
                tL = prepB(t_)

            def bc(v, n):
                return v.unsqueeze(2).to_broadcast([128, 16, n])

            D = lambda fn, deps=(): P.op("dve", fn, deps)
            GL = L_all[:, :, 0:4]
            D(lambda e: e.tensor_reduce(out=V_gmax, in_=GL, axis=AX.X, op=ALU.max), [tL])
            D(lambda e: e.tensor_tensor(out=gmask, in0=GL, in1=bc(V_gmax, 4), op=ALU.is_equal))
            t = D(lambda e: e.tensor_tensor(out=gdiff, in0=GL, in1=bc(V_gmax, 4), op=ALU.subtract))
            t = P.op("act", lambda e: e.activation(out=gdiff, in_=gdiff, func=AF.Exp), [t])
            D(lambda e: e.tensor_reduce(out=V_gsum, in_=gdiff, axis=AX.X, op=ALU.add), [t])
            D(lambda e: e.tensor_tensor(out=esel, in0=L_all[:, :, 4:12], in1=bc(gmask[:, :, 0], 8), op=ALU.mult))
            for g in range(1, 4):
                D((lambda g: lambda e: e.tensor_tensor(out=e2, in0=L_all[:, :, 4 + 8 * g:12 + 8 * g], in1=bc(gmask[:, :, g], 8), op=ALU.mult))(g))
                D(lambda e: e.tensor_tensor(out=esel, in0=esel, in1=e2, op=ALU.add))
            D(lambda e: e.tensor_reduce(out=V_m1, in_=esel, axis=AX.X, op=ALU.max))
            D(lambda e: e.tensor_tensor(out=mask1, in0=esel, in1=bc(V_m1, 8), op=ALU.is_equal))
            D(lambda e: e.scalar_tensor_tensor(out=e2, in0=mask1, scalar=-1.0e30, in1=esel, op0=ALU.mult, op1=ALU.add))
            D(lambda e: e.tensor_reduce(out=V_m2, in_=e2, axis=AX.X, op=ALU.max))
            D(lambda e: e.tensor_tensor(out=mask2, in0=e2, in1=bc(V_m2, 8), op=ALU.is_equal))
            t = D(lambda e: e.tensor_tensor(out=V_ex, in0=V_m2, in1=V_m1, op=ALU.subtract))
            t = P.op("act", lambda e: e.activation(out=V_ex, in_=V_ex, func=AF.Exp), [t])
            D(lambda e: e.scalar_tensor_tensor(out=V_den, in0=V_ex, scalar=1.0, in1=V_gsum, op0=ALU.add, op1=ALU.mult), [t])
            D(lambda e: e.reciprocal(out=gatesAll[:, :, 0], in_=V_den))
            D(lambda e: e.tensor_tensor(out=gatesAll[:, :, 1], in0=gatesAll[:, :, 0], in1=V_ex, op=ALU.mult))
            D(lambda e: e.tensor_tensor(out=msum, in0=mask1, in1=mask2, op=ALU.add))
            tmb = None
            for g in range(4):
                tmb = D((lambda g: lambda e: e.tensor_tensor(out=Mb_all[:, :, 8 * g:8 * g + 8], in0=msum, in1=bc(gmask[:, :, g], 8), op=ALU.mult))(g))
            prk = banks[2][:].rearrange("p (t e) -> p t e", e=32)
            tr_ = None
            for t_ in range(16):
                for tp_ in range(t_):
                    P.op("pe", (lambda t_, tp_: lambda e: e.matmul(prk[:, t_, :], lhsT=onesb[:], rhs=Mb_all[:, tp_, :], start=(tp_ == 0), stop=False))(t_, tp_),
                         [tmb] + buf("onesb").rd() + bankB[2].wr(), inc=False)
                tr_ = P.op("pe", (lambda t_: lambda e: e.matmul(prk[:, t_, :], lhsT=ltri[:], rhs=Mb_all[:, t_, :], start=(t_ == 0), stop=True))(t_),
                           [tmb] + buf("ltri").rd() + bankB[2].wr())
            bankB[2].did_w(tr_)
            D(lambda e: e.tensor_tensor(out=tmp32, in0=prk, in1=eoff[:].rearrange("p (t e) -> p t e", e=32), op=ALU.add), [tr_] + buf("eoff").rd())
            bankB[2].did_r(("dve", P.cnt["dve"]))
            D(lambda e: e.tensor_tensor(out=Rr, in0=tmp32[:, :, 0:8], in1=bc(gmask[:, :, 0], 8), op=ALU.mult))
            for g in range(1, 4):
                D((lambda g: lambda e: e.tensor_tensor(out=e2, in0=tmp32[:, :, 8 * g:8 * g + 8], in1=bc(gmask[:, :, g], 8), op=ALU.mult))(g))
                D(lambda e: e.tensor_tensor(out=Rr, in0=Rr, in1=e2, op=ALU.add))
            D(lambda e: e.tensor_tensor(out=e2, in0=Rr, in1=mask1, op=ALU.mult))
            D(lambda e: e.tensor_reduce(out=slf[:, :, 0], in_=e2, axis=AX.X, op=ALU.add))
            D(lambda e: e.tensor_tensor(out=e2, in0=Rr, in1=mask2, op=ALU.mult))
            D(lambda e: e.tensor_reduce(out=slf[:, :, 1], in_=e2, axis=AX.X, op=ALU.add))
            ti = D(lambda e: e.tensor_copy(out=idxAll[:], in_=slf))
            buf("idx").did_w(ti)
            sc_toks = []
            if stage >= 6:
                for t_ in range(16):
                    i = t_ % 2
                    tsc = None
                    for k_ in range(2):
                        tsc = P.dma("pool", (lambda t_, k_: lambda e: e.indirect_dma_start(
                            out=Xbuf[:, :], out_offset=bass.IndirectOffsetOnAxis(ap=idxAll[:, t_, k_:k_ + 1], axis=0),
                            in_=xnAll[:, t_, :], in_offset=None, bounds_check=None, oob_is_err=False))(t_, k_),
                            f"sc_x{i}", [ti] + buf("zf").rd())
                    sc_toks = [x for x in sc_toks if x[0] != f"sc_x{i}"] + [tsc]
            buf("X3").did_r(("pe", P.cnt["pe"]))
            for tk in sc_toks:
                buf("X3").did_r(tk)

        if stage >= 6:
            y_toks = {}

            def load_xe(ex):
                i = ex % 2
                bxe = buf(f"xe{i}")
                tld = P.dma("sp", (lambda ex, i: lambda e: e.dma_start(
                    out=xe[i], in_=Xbuf[ex * CAP:(ex + 1) * CAP, :].rearrange("(s p) d -> p s d", p=128)))(ex, i),
                    f"ld_xe{i}", sc_toks + bxe.wr() + buf("X3").wr())
                bxe.did_w(tld)

            def partA(ex):
                i = ex % 2
                bxe, bxt = buf(f"xe{i}"), buf(f"XeT{i}")
                tl = tcp = None
                for st_ in range(2):
                    bk = st_
                    pv = banks[bk][:].bitcast(BF16).rearrange("p (c t) -> p c t", t=128)
                    for c in range(8):
                        tl = P.op("pe", (lambda c, pv, i, st_: lambda e: e.transpose(out=pv[:, c, :], in_=xe[i][:, st_, c * 128:(c + 1) * 128], identity=idb[:]))(c, pv, i, st_),
                                  bxe.rd() + bankB[bk].wr(), inc=(c == 7))
                    bankB[bk].did_w(tl)
                    tcp = P.op("act", (lambda pv, i, st_: lambda e: e.copy(out=XeT[i][:, :, st_ * 128:(st_ + 1) * 128], in_=pv))(pv, i, st_),
                               bankB[bk].rd() + bxt.wr(), dr=False)
                    bankB[bk].did_r(tcp)
                bxe.did_r(tl)
                bxt.did_w(tcp)

            def gateup(ex):
                i = ex % 2
                bw = buf(f"we{i}")
                bxt, bh = buf(f"XeT{i}"), buf(f"HT{i}")
                tu = tm = None
                for fc in range(4):
                    bg, bu = (2, 3) if fc % 2 == 0 else (4, 5)
                    tg = None
                    for c in range(8):
                        tg = P.op("pe", (lambda c, fc, i, bg: lambda e: e.matmul(
                            banks[bg][:, 0:CAP], lhsT=wgb[i][:, c, fc * 128:(fc + 1) * 128], rhs=XeT[i][:, c, :],
                            start=(c == 0), stop=(c == 7)))(c, fc, i, bg),
                            bw.rd() + bxt.rd() + bankB[bg].wr(), inc=(c == 7))
                    bankB[bg].did_w(tg)
                    for c in range(8):
                        tu = P.op("pe", (lambda c, fc, i, bu: lambda e: e.matmul(
                            banks[bu][:, 0:CAP], lhsT=wub[i][:, c, fc * 128:(fc + 1) * 128], rhs=XeT[i][:, c, :],
                            start=(c == 0), stop=(c == 7)))(c, fc, i, bu),
                            bankB[bu].wr(), inc=(c == 7))
                    bankB[bu].did_w(tu)
                    sgi = fc % 2
                    ts = P.op("act", (lambda bg, sgi: lambda e: e.activation(out=tmpA[:, sgi, 0:CAP], in_=banks[bg][:, 0:CAP], func=AF.Silu))(bg, sgi),
                              bankB[bg].rd() + buf(f"sg{sgi}").wr(), dr=False)
                    bankB[bg].did_r(ts)
                    buf(f"sg{sgi}").did_w(ts)
                    tm = P.op("dve", (lambda i, fc, sgi, bu: lambda e: e.tensor_tensor(out=HT[i][:, fc, :], in0=banks[bu][:, 0:CAP], in1=tmpA[:, sgi, 0:CAP],
                                                                                       op=ALU.mult))(i, fc, sgi, bu),
                              bankB[bu].rd() + buf(f"sg{sgi}").rd() + bh.wr(), dr=False)
                    bankB[bu].did_r(tm)
                    buf(f"sg{sgi}").did_r(tm)
                bxt.did_r(tu)
                bh.did_w(tm)

            def down(ex):
                i = ex % 2
                bw, bh = buf(f"we{i}"), buf(f"HT{i}")
                tl = None
                for st_ in range(2):
                    yi = st_
                    by = buf(f"ye{yi}")
                    tcy_a = tcy = None
                    for half in range(2):
                        bk = 6 + half
                        for fc in range(4):
                            tl = P.op("pe", (lambda fc, st_, half, i, bk: lambda e: e.matmul(
                                banks[bk][:], lhsT=HT[i][:, fc, st_ * 128:(st_ + 1) * 128], rhs=wdb[i][:, fc, half * 512:(half + 1) * 512],
                                start=(fc == 0), stop=(fc == 3)))(fc, st_, half, i, bk),
                                bh.rd() + bw.rd() + bankB[bk].wr(), inc=(fc == 3))
                        bankB[bk].did_w(tl)
                        if half == 0:
                            tcy = P.op("act", (lambda yi, bk: lambda e: e.copy(out=ye[yi][:, 0:512], in_=banks[bk][:]))(yi, bk),
                                       bankB[bk].rd() + by.wr(), dr=False)
                            tcy_a = tcy
                        else:
                            tcy = P.op("dve", (lambda yi, bk: lambda e: e.tensor_copy(out=ye[yi][:, 512:1024], in_=banks[bk][:]))(yi, bk),
                                       bankB[bk].rd() + by.wr(), dr=False)
                        bankB[bk].did_r(tcy)
                    tst = P.dma("sp", (lambda ex, st_, yi: lambda e: e.dma_start(
                        out=Ybuf[ex * CAP + st_ * 128: ex * CAP + (st_ + 1) * 128, :], in_=ye[yi]))(ex, st_, yi),
                        f"st_ye{yi}", [tcy_a, tcy])
                    by.did_w(tcy)
                    by.did_r(tcy_a)
                    by.did_r(tst)
                    y_toks[yi] = tst
                bh.did_r(tl)
                bw.did_r(tl)

            load_xe(0)
            load_xe(1)
            partA(0)
            for ex in range(32):
                gateup(ex)
                if ex + 1 < 32:
                    partA(ex + 1)
                if ex + 2 < 32:
                    load_xe(ex + 2)
                down(ex)
                if ex + 2 < 32:
                    load_weights(ex + 2)
            allw = [("pe", P.cnt["pe"])] + buf("we0").wr() + buf("we1").wr()
            for t_ in range(16):
                gs = []
                for k_ in range(2):
                    gi = (2 * t_ + k_) % 8
                    bgb = buf(f"gb{gi}")
                    tg_ = P.dma("pool", (lambda t_, k_, gi: lambda e: e.indirect_dma_start(
                        out=gb[gi], out_offset=None, in_=Ybuf[:, :],
                        in_offset=bass.IndirectOffsetOnAxis(ap=idxAll[:, t_, k_:k_ + 1], axis=0),
                        bounds_check=None, oob_is_err=False))(t_, k_, gi),
                        f"ga{gi}", list(y_toks.values()) + bgb.wr() + allw + buf("idx").rd())
                    bgb.did_w(tg_)
                    gs.append((gi, bgb))
                for k_, (gi, bgb) in enumerate(gs):
                    ta = P.op("dve", (lambda t_, k_, gi: lambda e: e.scalar_tensor_tensor(
                        out=h1[:, t_, :], in0=gb[gi], scalar=gatesAll[:, t_, k_:k_ + 1], in1=h1[:, t_, :],
                        op0=ALU.mult, op1=ALU.add))(t_, k_, gi), bgb.rd() + buf("h1").rd())
                    bgb.did_r(ta)
            buf("h1").did_w(ta)

        if stage >= 4:
            for t_ in range(16):
                col = t_ % 4
                i = t_ % 2
                t1 = P.op("act", (lambda t_, col: lambda e: e.activation(out=junk[:], in_=h1[:, t_, :], func=AF.Square, accum_out=ss_t[:, col:col + 1]))(t_, col),
                          buf("h1").rd() + buf("junk").wr() + buf(f"ss{col}").wr(), dr=False)
                buf("junk").did_w(t1)
                t2 = P.op("act", (lambda col: lambda e: e.activation(out=rs_t[:, col:col + 1], in_=ss_t[:, col:col + 1], func=AF.Sqrt,
                                                                     bias=epsb[:, 0:1], scale=1.0 / DM))(col), buf(f"rs{col}").wr())
                t3 = P.op("dve", (lambda col: lambda e: e.reciprocal(out=rs_t[:, col:col + 1], in_=rs_t[:, col:col + 1]))(col), [t2], dr=False)
                buf(f"ss{col}").did_r(t2)
                t4 = P.op("dve", (lambda t_, col: lambda e: e.scalar_tensor_tensor(
                    out=h1[:, t_, :], in0=h1[:, t_, :], scalar=rs_t[:, col:col + 1], in1=gvb[:, 2, :], op0=ALU.mult, op1=ALU.mult))(t_, col),
                    buf("gv2").rd() + [t1])
                buf(f"ss{col}").did_w(t1)
                buf(f"rs{col}").did_w(t3)
                buf(f"rs{col}").did_r(t4)
                to = P.dma("sp", (lambda t_: lambda e: e.dma_start(out=out[t_ * 128:(t_ + 1) * 128, :], in_=h1[:, t_, :]))(t_), "st_out", [t4])
            P.wait("sp", [to])

        if dbg:
            deps_all = [(k, P.cnt[k]) for k in ("pe", "act", "dve")]
            def dump(name, ap_sb, shape, dt):
                d = nc.dram_tensor(name, list(shape), dt, kind="ExternalOutput").ap()
                tk = P.dma("sp", lambda e: e.dma_start(out=d, in_=ap_sb), "st_dbg", deps_all)
                P.wait("sp", [tk])
            if stage == 1:
                dump("d_pT", pT, [128, 4, NQ], BF16)
                dump("d_UT", UT, [128, 4, 2064], F32)
            if stage == 2:
                dump("d_KT", KT, [128, 2, S], BF16)
                dump("d_QT", QT, [128, 2, NQ], BF16)
                dump("d_VA", X1[:, 16384:16384 + 16640], [128, 16640], BF16)
            if stage == 3:
                dump("d_a", a_tok, [128, 16, 512], BF16)
            if stage == 5:
                dump("d_G", gatesAll[:], [128, 16, 2], F32)
                dump("d_idx", idxAll[:], [128, 16, 2], I32)
        P.emit()
    return nc


def host_consts(qs):
    T0 = NQ * qs
    r = np.arange(S)
    wrapped = (r + T0) >= S
    krel = np.where(wrapped, r - S, r).astype(np.float64)
    sig_tile = np.where(wrapped.reshape(NT, 128)[:, 0], -1.0, 1.0)
    kb = np.zeros((128, 3, 4, 64), np.float32)
    csg = np.zeros((2, 4, 66), np.float32)
    for h in range(4):
        sl = SLOPES[h]
        for m in range(64):
            kpos = 128 * m + np.arange(128)
            kb[:, 0, h, m] = sl * kpos
            kb[:, 1, h, m] = -sl * kpos
            kb[:, 2, h, m] = -sig_tile[m] * sl * krel[128 * m:128 * m + 128]
            csg[:, h, m] = 8 * sl * sig_tile[m]
        csg[:, h, 64] = 8 * sl
        csg[:, h, 65] = -8 * sl
    qpos = np.stack([128.0 * (np.arange(NQ) // 128), (np.arange(NQ) % 128).astype(np.float64)]).astype(np.float32)
    dband = np.zeros((128, 2, 4, 512), np.float32)
    kk = np.arange(128)[:, None]
    qq = np.arange(512)[None, :]
    for i in range(4):
        v = np.abs(qq - kk - 128 * i)
        dband[:, 0, i, :] = 256 * (v // 256)
        dband[:, 1, i, :] = v % 256
    sid = np.zeros((128, 4, 128), np.float32)
    for h in range(4):
        sid[:, h, :] = -8 * SLOPES[h] * np.eye(128)
    hv = np.zeros((128, 16), np.float32)
    hv[:, 0:8] = 1.0 if T0 > 0 else 0.0
    hv[:, 8:16] = 1.0 if T0 + NQ < S else 0.0
    rcorr = np.zeros((128, 4, 16), np.float32)
    for g, w in enumerate(WINS):
        for k_, rr in enumerate(list(range(8)) + list(range(NQ - 8, NQ))):
            t = T0 + rr
            lo = max(t - w // 2, 0)
            hi = min(t + w // 2, S)
            rcorr[:, g, k_] = 1.0 / (hi - lo)
    ltri = np.triu(np.ones((128, 128), np.float32), k=1)
    eoff = np.broadcast_to(np.tile((np.arange(32) * CAP).astype(np.float32), 16)[None, :], (128, 512)).copy()
    return dict(ltri=ltri, eoff=eoff, kb=kb.reshape(128, -1), csg=csg.reshape(2, -1), qpos=qpos, dband=dband.reshape(128, -1),
                sid=sid.reshape(128, -1), hv=hv, rcorr=rcorr.reshape(128, -1))


def make_in_maps(inputs, cores=range(8)):
    f = lambda a: np.ascontiguousarray(np.asarray(a, dtype=np.float32))
    x = f(inputs["x"])
    shared = dict(
        w_in=f(inputs["w_in"])[0], w_out=f(inputs["w_out"])[0], w_pool=f(inputs["w_pool"])[0],
        w_gate=f(inputs["w_gate"])[0], w_up=f(inputs["w_up"])[0], w_down=f(inputs["w_down"])[0],
        wr=np.ascontiguousarray(np.concatenate([f(inputs["w_group_router"])[0], f(inputs["w_expert_router"])[0].reshape(DM, 32)], axis=1)),
        vecs=np.ascontiguousarray(np.stack([f(inputs["norm1_g"])[0], f(inputs["norm2_g"])[0], f(inputs["final_g"])])),
        rbias=np.ascontiguousarray(np.concatenate([f(inputs["b_group_router"])[0], f(inputs["b_expert_router"])[0].reshape(32)])),
        lams=np.ascontiguousarray(np.stack([f(inputs["lambda_q1"])[0], f(inputs["lambda_k1"])[0], f(inputs["lambda_q2"])[0], f(inputs["lambda_k2"])[0]])),
        subg=f(inputs["subln_g"])[0],
        pscale=np.ascontiguousarray(f(inputs["pool_scale"])[0].reshape(4, 128).T),
        ident=np.eye(128, dtype=np.float32),
    )
    maps = []
    for c in cores:
        b, qs = c // 4, c % 4
        m = dict(shared)
        m["xr"] = np.ascontiguousarray(np.roll(x[b], -NQ * qs, axis=0))
        m.update(host_consts(qs))
        maps.append(m)
    return maps


_NC = None


def kernel(**inputs):
    global _NC
    if _NC is None:
        _NC = build()
    in_maps = make_in_maps(inputs)
    res = run_bass_kernel_spmd(_NC, in_maps, core_ids=list(range(8)))
    out = np.zeros((2, S, DM), np.float32)
    for c in range(8):
        b, qs = c // 4, c % 4
        out[b, NQ * qs:NQ * (qs + 1)] = res.results[c]["out"]
    return out
```
